# Optimizing a Trainium2 kernel written in Bass

```python
import math
import jax
import jax.numpy as jnp
from jax import lax
import numpy as np

D_MODEL = 1024
BATCH = 4
SEQ = 8192
DEPTH = 2

CHUNK = 64
S5_WIDTH = D_MODEL // 4
S5_GROUP_CH = 16
S5_GROUPS = S5_WIDTH // S5_GROUP_CH
S5_STATE = 64
SB_HEADS = 4
SB_HEAD_DIM = 64
SB_WIDTH = SB_HEADS * SB_HEAD_DIM
SB_BLOCK = 128
RET_HEADS = 4
RET_DK = 64
RET_DV = 128
RET_QK_WIDTH = RET_HEADS * RET_DK
RET_V_WIDTH = RET_HEADS * RET_DV
ROPE_BASE = 10000.0
N_BRANCHES = 3
IN_WIDTH = S5_WIDTH + 3 * SB_WIDTH + 2 * RET_QK_WIDTH + 2 * RET_V_WIDTH + N_BRANCHES * D_MODEL
N_EXPERTS = 16
N_EXPERT_GROUPS = 4
EXPERTS_PER_GROUP = N_EXPERTS // N_EXPERT_GROUPS
TOP_K = 2
D_EXPERT = 1024
MOE_BLOCK = 128
ALPHA = (2 * DEPTH) ** 0.25
BETA = (8 * DEPTH) ** -0.25
LN_EPS = 1e-5

kernel_name = 'hybrid_streaming_encoder'


def layer_norm(x, g, b):
    xf = x.astype(jnp.float32)
    mu = jnp.mean(xf, axis=-1, keepdims=True)
    var = jnp.mean(jnp.square(xf - mu), axis=-1, keepdims=True)
    return ((xf - mu) * lax.rsqrt(var + LN_EPS) * g + b).astype(x.dtype)


def rope(t, pos):
    half = t.shape[-1] // 2
    inv_freq = ROPE_BASE ** (-jnp.arange(half, dtype=jnp.float32) / half)
    ang = pos[:, None] * inv_freq[None, :]
    cos = jnp.cos(ang)[None, :, None, :]
    sin = jnp.sin(ang)[None, :, None, :]
    t1, t2 = t[..., :half], t[..., half:]
    return jnp.concatenate([t1 * cos - t2 * sin, t1 * sin + t2 * cos], axis=-1)


def s5_mixer(u, lam_re, lam_im, log_dt, b_re, b_im, c_re, c_im, d_skip, w_glu, b_glu):
    Bsz, L, _ = u.shape
    uf = u.astype(jnp.float32).reshape(Bsz, L, S5_GROUPS, S5_GROUP_CH)
    lam = lax.complex(lam_re.astype(jnp.float32), lam_im.astype(jnp.float32))
    dt = jnp.exp(log_dt.astype(jnp.float32))[:, None]
    lam_bar = jnp.exp(lam * dt)
    zoh = (lam_bar - 1.0) / lam
    b_bar = lax.complex(b_re.astype(jnp.float32), b_im.astype(jnp.float32)) * zoh[..., None]
    bu = lax.complex(jnp.einsum('blgc,gpc->blgp', uf, jnp.real(b_bar)),
                     jnp.einsum('blgc,gpc->blgp', uf, jnp.imag(b_bar)))
    a = jnp.broadcast_to(lam_bar, (L,) + lam_bar.shape)

    def combine(e1, e2):
        a1, s1 = e1
        a2, s2 = e2
        return a1 * a2, a2 * s1 + s2

    def scan_one(bu_b):
        return lax.associative_scan(combine, (a, bu_b))[1]

    states = jax.vmap(scan_one)(bu)
    y = (jnp.einsum('blgp,gcp->blgc', jnp.real(states), c_re.astype(jnp.float32))
         - jnp.einsum('blgp,gcp->blgc', jnp.imag(states), c_im.astype(jnp.float32))
         + d_skip.astype(jnp.float32).reshape(S5_GROUPS, S5_GROUP_CH) * uf)
    y = jax.nn.gelu(y.reshape(Bsz, L, S5_WIDTH))
    y = y * jax.nn.sigmoid(y @ w_glu.astype(jnp.float32) + b_glu.astype(jnp.float32))
    return y.astype(u.dtype)


def stick_breaking_attention(q, k, v):
    Bsz, L, H, d = q.shape
    qf = q.astype(jnp.float32) * (d ** -0.5)
    kf = k.astype(jnp.float32)
    vf = v.astype(jnp.float32)
    nqb = L // SB_BLOCK
    qb = qf.reshape(Bsz, nqb, SB_BLOCK, H, d).transpose(1, 0, 3, 2, 4)
    key_pos = jnp.arange(L)

    def block(args):
        i, qblk = args
        qpos = i * SB_BLOCK + jnp.arange(SB_BLOCK)
        z = jnp.einsum('bhqd,bkhd->bhqk', qblk, kf)
        mask = key_pos[None, :] < qpos[:, None]
        log_1m = jnp.where(mask, jax.nn.log_sigmoid(-z), 0.0)
        after = lax.cumsum(log_1m, axis=3, reverse=True) - log_1m
        w = jnp.where(mask, jnp.exp(jax.nn.log_sigmoid(z) + after), 0.0)
        return jnp.einsum('bhqk,bkhd->bqhd', w, vf)

    out = lax.map(block, (jnp.arange(nqb), qb))
    return out.transpose(1, 0, 2, 3, 4).reshape(Bsz, L, H * d)


def chunkwise_retention(q, k, v, g, gn_g):
    Bsz, L, _ = q.shape
    n_chunks = L // CHUNK
    pos = jnp.arange(L, dtype=jnp.float32)
    qf = rope(q.astype(jnp.float32).reshape(Bsz, L, RET_HEADS, RET_DK), pos)
    kf = rope(k.astype(jnp.float32).reshape(Bsz, L, RET_HEADS, RET_DK), pos) * (RET_DK ** -0.5)
    vf = v.astype(jnp.float32).reshape(Bsz, L, RET_HEADS, RET_DV)

    def to_chunks(t):
        return t.reshape(Bsz, n_chunks, CHUNK, RET_HEADS, t.shape[-1]).transpose(0, 3, 1, 2, 4)

    qc, kc, vc = to_chunks(qf), to_chunks(kf), to_chunks(vf)
    log_gamma = jnp.log(1.0 - 2.0 ** (-5.0 - jnp.arange(RET_HEADS, dtype=jnp.float32)))
    idx = jnp.arange(CHUNK, dtype=jnp.float32)
    intra_decay = jnp.exp(log_gamma[:, None, None] * jnp.abs(idx[:, None] - idx[None, :]))
    scores = jnp.einsum('bhncd,bhnsd->bhncs', qc, kc) * intra_decay[None, :, None]
    intra = jnp.einsum('bhncs,bhnse->bhnce', scores, vc)
    k_dec = kc * jnp.exp(log_gamma[:, None] * (CHUNK - 1.0 - idx))[None, :, None, :, None]
    kv = jnp.einsum('bhnsd,bhnse->nbhde', k_dec, vc)
    chunk_decay = jnp.exp(log_gamma * CHUNK)[None, :, None, None]

    def step(state, kv_i):
        return chunk_decay * state + kv_i, state

    _, state_prev = lax.scan(step, jnp.zeros_like(kv[0]), kv)
    q_dec = qc * jnp.exp(log_gamma[:, None] * (idx + 1.0))[None, :, None, :, None]
    cross = jnp.einsum('bhncd,nbhde->bhnce', q_dec, state_prev)
    o = (intra + cross).transpose(0, 2, 3, 1, 4).reshape(Bsz, L, RET_HEADS, RET_DV)
    mu = jnp.mean(o, axis=-1, keepdims=True)
    var = jnp.mean(jnp.square(o - mu), axis=-1, keepdims=True)
    o = ((o - mu) * lax.rsqrt(var + LN_EPS)).reshape(Bsz, L, RET_V_WIDTH) * gn_g.astype(jnp.float32)
    return jax.nn.silu(g.astype(jnp.float32)) * o


def hybrid_mixer(x, w_in, lam_re, lam_im, log_dt, b_re, b_im, c_re, c_im, d_skip, w_glu, b_glu,
                 gn_g, w_up_a, w_up_b, w_up_c, w_out):
    Bsz, L, _ = x.shape
    h = x @ w_in
    sizes = (S5_WIDTH, SB_WIDTH, SB_WIDTH, SB_WIDTH, RET_QK_WIDTH, RET_QK_WIDTH,
             RET_V_WIDTH, RET_V_WIDTH, D_MODEL, D_MODEL, D_MODEL)
    cuts = np.cumsum(sizes)[:-1].tolist()
    u_a, q_b, k_b, v_b, q_c, k_c, v_c, g_c, gate_a, gate_b, gate_c = jnp.split(h, cuts, axis=-1)
    y_a = s5_mixer(u_a, lam_re, lam_im, log_dt, b_re, b_im, c_re, c_im, d_skip, w_glu, b_glu)
    hd = (Bsz, L, SB_HEADS, SB_HEAD_DIM)
    y_b = stick_breaking_attention(q_b.reshape(hd), k_b.reshape(hd), v_b.reshape(hd)).astype(x.dtype)
    y_c = chunkwise_retention(q_c, k_c, v_c, g_c, gn_g).astype(x.dtype)
    merged = (jax.nn.sigmoid(gate_a) * (y_a @ w_up_a)
              + jax.nn.sigmoid(gate_b) * (y_b @ w_up_b)
              + jax.nn.sigmoid(gate_c) * (y_c @ w_up_c))
    return merged @ w_out


def grouped_moe(x, router_w, router_b, w1, w3, w2):
    Bsz, L, D = x.shape
    T = Bsz * L
    xt = x.reshape(T, D)
    logits = (xt @ router_w).astype(jnp.float32) + router_b.astype(jnp.float32)
    scores = jax.nn.softmax(logits, axis=-1)
    grp_score = lax.top_k(scores.reshape(T, N_EXPERT_GROUPS, EXPERTS_PER_GROUP), 2)[0].sum(-1)
    best = jnp.argmax(grp_score, axis=-1)
    in_group = (jnp.arange(N_EXPERTS) // EXPERTS_PER_GROUP)[None, :] == best[:, None]
    top_w, top_e = lax.top_k(jnp.where(in_group, scores, -1.0), TOP_K)
    gates = top_w / jnp.sum(top_w, axis=-1, keepdims=True)
    n_assign = T * TOP_K
    flat_e = top_e.reshape(-1)
    flat_tok = jnp.arange(n_assign) // TOP_K
    flat_g = gates.reshape(-1)
    order = jnp.argsort(flat_e)
    sorted_e = flat_e[order]
    counts = jnp.bincount(flat_e, length=N_EXPERTS)
    starts = jnp.cumsum(counts) - counts
    padded = (counts + MOE_BLOCK - 1) // MOE_BLOCK * MOE_BLOCK
    pends = jnp.cumsum(padded)
    pstarts = pends - padded
    dest = pstarts[sorted_e] + (jnp.arange(n_assign) - starts[sorted_e])
    n_rows = n_assign + N_EXPERTS * MOE_BLOCK
    buf_tok = jnp.full((n_rows,), T, dtype=jnp.int32).at[dest].set(flat_tok[order].astype(jnp.int32))
    buf_gate = jnp.zeros((n_rows,), jnp.float32).at[dest].set(flat_g[order])
    n_blocks = n_rows // MOE_BLOCK
    block_e = jnp.clip(jnp.searchsorted(pends, jnp.arange(n_blocks) * MOE_BLOCK, side='right'), 0, N_EXPERTS - 1)
    x_pad = jnp.concatenate([xt, jnp.zeros((1, D), xt.dtype)], axis=0)
    xs = x_pad[buf_tok].reshape(n_blocks, MOE_BLOCK, D)

    def expert_block(args):
        xb, e = args
        hb = jax.nn.silu(xb @ w1[e]) * (xb @ w3[e])
        return hb @ w2[e]

    ys = lax.map(expert_block, (xs, block_e)).reshape(n_rows, D)
    ys = ys * buf_gate[:, None].astype(ys.dtype)
    out = jax.ops.segment_sum(ys, buf_tok, num_segments=T + 1)[:T]
    return out.reshape(Bsz, L, D).astype(x.dtype)


def setup_inputs(seed: int = 0) -> dict:
    key = jax.random.key(seed)
    ks = jax.random.split(key, 32)
    f32 = jnp.float32

    def nrm(k, shape, scale):
        return jax.random.normal(k, shape, f32) * scale

    Lr, D = DEPTH, D_MODEL
    G, P, Cg = S5_GROUPS, S5_STATE, S5_GROUP_CH
    E, F = N_EXPERTS, D_EXPERT
    return {
        'x': nrm(ks[0], (BATCH, SEQ, D), 1.0),
        'ln0_g': 1.0 + nrm(ks[1], (D,), 0.02),
        'ln0_b': nrm(ks[2], (D,), 0.02),
        'w_in': nrm(ks[3], (Lr, D, IN_WIDTH), D ** -0.5),
        's5_lambda_re': -0.5 * (1.0 + nrm(ks[4], (Lr, G, P), 0.02)),
        's5_lambda_im': math.pi * jnp.arange(P, dtype=f32) + nrm(ks[5], (Lr, G, P), 0.02),
        's5_log_dt': jax.random.uniform(ks[6], (Lr, G), f32, math.log(1e-3), math.log(1e-1)),
        's5_b_re': nrm(ks[7], (Lr, G, P, Cg), (2 * Cg) ** -0.5),
        's5_b_im': nrm(ks[8], (Lr, G, P, Cg), (2 * Cg) ** -0.5),
        's5_c_re': nrm(ks[9], (Lr, G, Cg, P), P ** -0.5),
        's5_c_im': nrm(ks[10], (Lr, G, Cg, P), P ** -0.5),
        's5_d': nrm(ks[11], (Lr, S5_WIDTH), 1.0),
        's5_w_glu': nrm(ks[12], (Lr, S5_WIDTH, S5_WIDTH), S5_WIDTH ** -0.5),
        's5_b_glu': nrm(ks[13], (Lr, S5_WIDTH), 0.02),
        'ret_gn_g': 1.0 + nrm(ks[14], (Lr, RET_V_WIDTH), 0.02),
        'w_up_a': nrm(ks[15], (Lr, S5_WIDTH, D), S5_WIDTH ** -0.5),
        'w_up_b': nrm(ks[16], (Lr, SB_WIDTH, D), SB_WIDTH ** -0.5),
        'w_up_c': nrm(ks[17], (Lr, RET_V_WIDTH, D), RET_V_WIDTH ** -0.5),
        'w_out': nrm(ks[18], (Lr, D, D), BETA * D ** -0.5),
        'ln1_g': 1.0 + nrm(ks[19], (Lr, D), 0.02),
        'ln1_b': nrm(ks[20], (Lr, D), 0.02),
        'router_w': nrm(ks[21], (D, E), D ** -0.5),
        'router_b': nrm(ks[22], (E,), 0.01),
        'moe_w1': nrm(ks[23], (Lr, E, D, F), D ** -0.5),
        'moe_w3': nrm(ks[24], (Lr, E, D, F), D ** -0.5),
        'moe_w2': nrm(ks[25], (Lr, E, F, D), BETA * F ** -0.5),
        'ln2_g': 1.0 + nrm(ks[26], (Lr, D), 0.02),
        'ln2_b': nrm(ks[27], (Lr, D), 0.02),
    }


def reference(x, ln0_g, ln0_b, w_in, s5_lambda_re, s5_lambda_im, s5_log_dt, s5_b_re, s5_b_im,
              s5_c_re, s5_c_im, s5_d, s5_w_glu, s5_b_glu, ret_gn_g, w_up_a, w_up_b, w_up_c, w_out,
              ln1_g, ln1_b, router_w, router_b, moe_w1, moe_w3, moe_w2, ln2_g, ln2_b):
    h = layer_norm(x, ln0_g, ln0_b)
    for l in range(DEPTH):
        y = hybrid_mixer(h, w_in[l], s5_lambda_re[l], s5_lambda_im[l], s5_log_dt[l], s5_b_re[l], s5_b_im[l],
                         s5_c_re[l], s5_c_im[l], s5_d[l], s5_w_glu[l], s5_b_glu[l], ret_gn_g[l],
                         w_up_a[l], w_up_b[l], w_up_c[l], w_out[l])
        h = layer_norm(ALPHA * h + y, ln1_g[l], ln1_b[l])
        y = grouped_moe(h, router_w, router_b, moe_w1[l], moe_w3[l], moe_w2[l])
        h = layer_norm(ALPHA * h + y, ln2_g[l], ln2_b[l])
    return h
```

```python
import math
from contextlib import ExitStack

import numpy as np
import concourse.bass as bass
import concourse.mybir as mybir
from concourse.bass_utils import run_bass_kernel_spmd

F32 = mybir.dt.float32
BF16 = mybir.dt.bfloat16
I32 = mybir.dt.int32
AF = mybir.ActivationFunctionType
ALU = mybir.AluOpType
AX = mybir.AxisListType

N_CORES = 8
D_MODEL = 1024
BATCH = 4
SEQ = 8192
DEPTH = 2
ALPHA = (2 * DEPTH) ** 0.25
LN_EPS = 1e-5


class Prog:
    COMPUTE = ("pe", "act", "dve", "pool")
    QUEUES = ("sp", "act", "pool")
    ENGS = ("pe", "act", "dve", "pool", "sp")
    RING = 6

    def __init__(self, same_engine_sync=True):
        self.nc = bass.Bass("TRN2", target_bir_lowering=False)
        self.ges = ExitStack()
        self.es = ExitStack()
        self.ops = []
        self.same_engine_sync = same_engine_sync
        self._n = 0
        nc = self.nc
        self.semobj = {}
        for e in self.COMPUTE:
            self.semobj[("c", e)] = self.ges.enter_context(nc.semaphore(f"c_{e}"))
        for q in self.QUEUES:
            for i in range(self.RING):
                self.semobj[("r", q, i)] = self.ges.enter_context(nc.semaphore(f"r_{q}{i}"))
        self.semobj[("cc",)] = self.ges.enter_context(nc.semaphore("cc"))
        self.cnt = {e: 0 for e in self.COMPUTE}
        self.ccv = 0
        self.ring_pos = {q: 0 for q in self.QUEUES}
        self.ring_val = {q: [0] * self.RING for q in self.QUEUES}
        self.known = {e: {} for e in self.ENGS}
        self.last_w = {}
        self.readers = {}
        self.pending_barrier = {e: [] for e in self.ENGS}
        self.n_emitted = 0
        self.real = {e: 0 for e in self.COMPUTE}
        self.tokmap = {}

    def _name(self, base):
        self._n += 1
        return f"{base}_{self._n}"

    def sb(self, shape, dt, name="sb"):
        return self.es.enter_context(self.nc.sbuf_tensor(self._name(name), list(shape), dt))

    def ps(self, shape, dt=F32, name="ps"):
        return self.es.enter_context(self.nc.psum_tensor(self._name(name), list(shape), dt))

    def dram_in(self, name, shape, dt=F32):
        return self.nc.dram_tensor(name, list(shape), dt, kind="ExternalInput")

    def dram_out(self, name, shape, dt=F32):
        return self.nc.dram_tensor(name, list(shape), dt, kind="ExternalOutput")

    def dram_tmp(self, name, shape, dt=F32):
        return self.nc.dram_tensor(name, list(shape), dt, kind="Internal")

    def op(self, eng, fn, reads=(), writes=()):
        self.ops.append(dict(eng=eng, fn=fn, reads=tuple(reads), writes=tuple(writes), kind="c"))

    def dma(self, queue, out, in_, reads=(), writes=(), **kw):
        def fn(e, out=out, in_=in_, kw=kw):
            return e.dma_start(out=out, in_=in_, **kw)
        self.ops.append(dict(eng=queue, fn=fn, reads=tuple(reads), writes=tuple(writes), kind="dma"))

    def coll(self, kind, in_ap, out_ap, groups, reads=(), writes=()):
        def fn(e):
            return e.collective_compute(kind, ALU.add if kind in ("AllReduce", "ReduceScatter") else ALU.bypass, replica_groups=groups, ins=[in_ap], outs=[out_ap])
        self.ops.append(dict(eng="pool", fn=fn, reads=tuple(reads), writes=tuple(writes), kind="cc"))

    def end_phase(self, final=False):
        nc = self.nc
        streams = {e: [] for e in self.ENGS}
        for o in self.ops:
            e = o["eng"]
            need = {}

            def req(tok):
                if tok is None:
                    return
                k, v = tok
                if need.get(k, 0) < v:
                    need[k] = v

            for r in o["reads"]:
                req(self.last_w.get(r))
            for w in o["writes"]:
                req(self.last_w.get(w))
                for t in self.readers.get(w, ()):
                    req(t)
            for t in self.pending_barrier[e]:
                req(t)
            self.pending_barrier[e] = []
            if o["kind"] == "dma":
                i = self.ring_pos[e] % self.RING
                self.ring_pos[e] += 1
                key = ("r", e, i)
                if self.ring_val[e][i] > 0:
                    req((key, self.ring_val[e][i]))
                self.ring_val[e][i] += 16
                tok = (key, self.ring_val[e][i])
                inc = (key, 16)
            elif o["kind"] == "cc":
                if self.ccv > 0:
                    req((("cc",), self.ccv))
                self.ccv += CC_INC
                tok = (("cc",), self.ccv)
                inc = (("cc",), CC_INC)
            else:
                self.cnt[e] += 1
                tok = (("c", e), self.cnt[e])
                inc = (("c", e), 1)
            waits = []
            for k, v in need.items():
                if k == ("c", e) and o["kind"] == "c":
                    if e == "pe" or not self.same_engine_sync:
                        continue
                if self.known[e].get(k, 0) >= v:
                    continue
                self.known[e][k] = v
                waits.append((k, v))
            streams[e].append((waits, o["fn"], inc, tok))
            for r in o["reads"]:
                self.readers.setdefault(r, []).append(tok)
            for w in o["writes"]:
                self.last_w[w] = tok
                self.readers[w] = []
        self.n_emitted += len(self.ops)
        self.ops = []
        all_done = []
        for q in self.QUEUES:
            for i in range(self.RING):
                if self.ring_val[q][i] > 0:
                    all_done.append((("r", q, i), self.ring_val[q][i]))
        for e in self.COMPUTE:
            if self.cnt[e] > 0:
                all_done.append((("c", e), self.cnt[e]))
        if self.ccv > 0:
            all_done.append((("cc",), self.ccv))
        for e in self.ENGS:
            self.pending_barrier[e] = list(all_done)
        targets = set(all_done)
        for e in self.ENGS:
            for waits, _, _, _ in streams[e]:
                targets.update(waits)
        for e in self.COMPUTE:
            for waits, fn, inc, tok in streams[e]:
                if inc[0] == ("c", e) and tok in targets:
                    self.real[e] += 1
                    self.tokmap[tok] = self.real[e]
        semobj = self.semobj
        tokmap = self.tokmap

        def real(k, v):
            return tokmap[(k, v)] if k[0] == "c" else v

        def emit(engobj, stream, extra_waits=()):
            for waits, fn, inc, tok in stream:
                for k, v in waits:
                    engobj.wait_ge(semobj[k], real(k, v))
                ins = fn(engobj)
                if inc[0][0] != "c" or tok in tokmap:
                    ins.then_inc(semobj[inc[0]], inc[1])
            for k, v in extra_waits:
                engobj.wait_ge(semobj[k], real(k, v))

        with nc.Block() as block:
            @block.tensor
            def _(eng):
                emit(eng, streams["pe"])

            @block.scalar
            def _(eng):
                emit(eng, streams["act"])

            @block.vector
            def _(eng):
                emit(eng, streams["dve"])

            @block.gpsimd
            def _(eng):
                emit(eng, streams["pool"])

            @block.sync
            def _(eng):
                emit(eng, streams["sp"], all_done if final else ())

        self.es.close()
        self.es = ExitStack()

    def build(self):
        self.end_phase(final=True)
        self.ges.close()
        return self.nc


CC_INC = 1


def run_spmd(nc, in_maps):
    res = run_bass_kernel_spmd(nc, in_maps, core_ids=list(range(N_CORES)))
    return res.results


def load_weight_bf16(p, w_dram_ap, ncols, tag, queue="sp"):
    w32 = p.sb([128, 8, ncols], F32, tag + "32")
    wb = p.sb([128, 8, ncols], BF16, tag + "b")
    p.dma(queue, w32[:], w_dram_ap.rearrange("(kc p) n -> p kc n", p=128), writes=[tag + "32"])
    p.op("pool", lambda e: e.tensor_copy(out=wb[:], in_=w32[:]), reads=[tag + "32"], writes=[tag])
    return wb


class HStream:
    def __init__(self, p, blk_ap, nbuf=2, src_res="hT_full"):
        self.p = p
        self.blk_ap = blk_ap
        self.nbuf = nbuf
        self.src_res = src_res
        self.hb = [p.sb([128, 8, 512], BF16, "hb") for _ in range(nbuf)]
        self.i = 0

    def load(self, blk, cast_eng="dve"):
        k = self.i % self.nbuf
        self.i += 1
        self.p.dma("sp", self.hb[k][:], self.blk_ap(blk), reads=[self.src_res], writes=[f"hb_{k}"])
        return self.hb[k], f"hb_{k}"


class YWriter:
    def __init__(self, p, yin, mk, mk_res, Tc):
        self.p, self.yin, self.mk, self.mk_res, self.Tc = p, yin, mk, mk_res, Tc
        self.tmp = [p.sb([128, 512], BF16, "ywt") for _ in range(4)]
        self.i = 0

    def write(self, row0, psl, src, src_res, blk, engs=("pool", "pool")):
        p = self.p
        nb_half = self.Tc // 512
        th, tl = blk // nb_half, blk % nb_half
        n = psl.stop - psl.start
        for rb in range(2):
            k = self.i % 4
            self.i += 1
            tmp = self.tmp[k]
            p.op(engs[rb], lambda e, tmp=tmp, rb=rb, src=src, psl=psl: e.tensor_scalar(
                out=tmp[psl, :], in0=src, scalar1=self.mk[psl, rb:rb + 1], scalar2=None, op0=ALU.mult),
                reads=[src_res, self.mk_res], writes=[f"ywt{k}"])
            p.dma("act" if rb == 0 else "pool", self.yin[th, rb, row0:row0 + n, tl * 512:(tl + 1) * 512], tmp[psl, :],
                  reads=[f"ywt{k}"], writes=["yin"])


def emit_sb(p, io, L, yw_fn):
    NB = L // 512
    NT = L // 128
    wq_d, wk_d, wv_d, tri_d, msk_d = io["wq"], io["wk"], io["wv"], io["tri"], io["masks"]
    yw = yw_fn()

    wq = load_weight_bf16(p, wq_d, 128, "wq")
    wk = load_weight_bf16(p, wk_d, 128, "wk")
    wv = load_weight_bf16(p, wv_d, 128, "wv")
    tri32 = p.sb([128, 128], F32, "tri32")
    tri = p.sb([128, 128], BF16, "tri")
    ones = p.sb([128, 128], BF16, "ones")
    masks = p.sb([128, 4, 512], F32, "masks")
    p.dma("act", tri32[:], tri_d, writes=["tri32"])
    p.dma("act", masks[:], msk_d, writes=["masks"])
    p.op("pool", lambda e: e.tensor_copy(out=tri[:], in_=tri32[:]), reads=["tri32"], writes=["tri"])
    p.op("pool", lambda e: e.memset(ones[:], 1.0), writes=["ones"])

    QT = p.sb([128, L], BF16, "QT")
    KT = p.sb([128, L], BF16, "KT")
    V = p.sb([128, NT, 128], BF16, "V")

    hs = HStream(p, io["hblk"])
    zps = [p.ps([128, 512], F32, "zps") for _ in range(3)]
    cps = [p.ps([128, 512], F32, "cps") for _ in range(3)]
    pq = zps
    pv = [c[:, 0:128] for c in cps]
    for blk in range(NB):
        hb, hres = hs.load(blk)
        for wi, (w, wres, dst, scale) in enumerate(((wq, "wq", QT, 0.125), (wk, "wk", KT, 1.0))):
            k = wi
            for kc in range(8):
                p.op("pe", lambda e, k=k, w=w, kc=kc, hb=hb: e.matmul(pq[k][:], lhsT=w[:, kc, :], rhs=hb[:, kc, :],
                                                                     start=(kc == 0), stop=(kc == 7)),
                     reads=[wres, hres], writes=[f"z{k}"])
            p.op("act", lambda e, k=k, dst=dst, blk=blk, scale=scale: e.activation(
                out=dst[:, blk * 512:(blk + 1) * 512], in_=pq[k][:], func=AF.Copy, scale=scale),
                reads=[f"z{k}"], writes=[("QK", wi, blk)])
        for sub in range(4):
            k = sub % 2
            t = blk * 4 + sub
            for kc in range(8):
                p.op("pe", lambda e, k=k, kc=kc, hb=hb, sub=sub: e.matmul(
                    pv[k], lhsT=hb[:, kc, sub * 128:(sub + 1) * 128], rhs=wv[:, kc, :],
                    start=(kc == 0), stop=(kc == 7)), reads=["wv", hres], writes=[f"c{k}"])
            p.op("dve", lambda e, k=k, t=t: e.tensor_copy(out=V[:, t, :], in_=pv[k]),
                 reads=[f"c{k}"], writes=[("V", t)])

    _ops1 = [p.ps([128, 512], F32, "ops") for _ in range(2)]
    ops_ = [[_ops1[h], _ops1[h]] for h in range(2)]
    NE = 7
    e32 = [p.sb([128, 512], F32, "e32") for _ in range(NE)]
    spb = [p.sb([128, 512], BF16, "spb") for _ in range(3)]
    wb = [p.sb([128, 512], BF16, "w") for _ in range(3)]
    S32 = [[p.sb([128, 512], F32, "S32") for _ in range(2)] for _ in range(2)]
    Sb = [[p.sb([128, 512], BF16, "Sb") for _ in range(2)] for _ in range(2)]
    yo = [[p.sb([128, 512], F32, "yo") for _ in range(2)] for _ in range(2)]
    tiles = []
    for I in range(NB):
        njb = 4 * I + 4
        for idx, j in enumerate(range(njb - 1, -1, -1)):
            for h in range(2):
                tiles.append(dict(I=I, h=h, j=j, idx=idx, first=(idx == 0), last=(j == 0), r=j - 4 * I))
    n = len(tiles)

    def stA(t, T):
        k = t % 3
        hp = slice(T["h"] * 64, (T["h"] + 1) * 64)
        j, I = T["j"], T["I"]
        p.op("pe", lambda e: e.matmul(zps[k][:], lhsT=KT[hp, j * 128:(j + 1) * 128], rhs=QT[hp, I * 512:(I + 1) * 512],
                                      start=True, stop=True), reads=[("QK", 0, I), ("QK", 1, j // 4)], writes=[f"z{k}"])

    def stB1(t, T):
        k, ke = t % 3, t % NE
        p.op("act", lambda e: e.activation(out=e32[ke][:], in_=zps[k][:], func=AF.Exp), reads=[f"z{k}"], writes=[f"e{ke}"])
        if T["r"] >= 0:
            r = T["r"]
            p.op("dve", lambda e: e.tensor_tensor(out=e32[ke][:], in0=e32[ke][:], in1=masks[:, r, :], op=ALU.mult),
                 reads=[f"e{ke}", "masks"], writes=[f"e{ke}"])

    def stB2(t, T):
        ke, ks, h = t % NE, t % 3, T["h"]
        p.op("act", lambda e: e.activation(out=spb[ks][:], in_=e32[ke][:], func=AF.Ln, bias=1.0),
             reads=[f"e{ke}"], writes=[f"sp{ks}"])
        if not T["last"]:
            ver = (T["idx"] + 1) % 2
            old = T["idx"] % 2
            if T["first"]:
                p.op("dve", lambda e: e.tensor_copy(out=S32[h][ver][:], in_=spb[ks][:]), reads=[f"sp{ks}"], writes=[f"S32{h}{ver}"])
                p.op("pool", lambda e: e.tensor_copy(out=Sb[h][ver][:], in_=spb[ks][:]), reads=[f"sp{ks}"], writes=[f"Sb{h}{ver}"])
            else:
                p.op("dve", lambda e: e.tensor_tensor(out=S32[h][ver][:], in0=S32[h][old][:], in1=spb[ks][:], op=ALU.add),
                     reads=[f"sp{ks}", f"S32{h}{old}"], writes=[f"S32{h}{ver}"])
                p.op("dve" if h == 0 else "pool",
                     lambda e: e.tensor_tensor(out=Sb[h][ver][:], in0=S32[h][old][:], in1=spb[ks][:], op=ALU.add),
                     reads=[f"sp{ks}", f"S32{h}{old}"], writes=[f"Sb{h}{ver}"])

    def stC1(t, T):
        k, ks, h = t % 3, t % 3, T["h"]
        first = T["first"]
        p.op("pe", lambda e: e.matmul(cps[k][:], lhsT=tri[:], rhs=spb[ks][:], start=True, stop=first),
             reads=["tri", f"sp{ks}"], writes=[f"c{k}"])
        if not first:
            ver = T["idx"] % 2
            p.op("pe", lambda e: e.matmul(cps[k][:], lhsT=ones[:], rhs=Sb[h][ver][:], start=False, stop=True),
                 reads=["ones", f"Sb{h}{ver}"], writes=[f"c{k}"])

    def stC2(t, T):
        k = t % 3
        p.op("act", lambda e: e.activation(out=cps[k][:], in_=cps[k][:], func=AF.Exp, scale=-1.0),
             reads=[f"c{k}"], writes=[f"c{k}"])

    def stD(t, T):
        k, kw, ke, h, I, j = t % 3, t % 3, t % NE, T["h"], T["I"], T["j"]
        par = I % 2
        hp = slice(h * 64, (h + 1) * 64)
        p.op("dve", lambda e: e.tensor_tensor(out=wb[kw][:], in0=e32[ke][:], in1=cps[k][:], op=ALU.mult),
             reads=[f"e{ke}", f"c{k}"], writes=[f"w{kw}"])
        p.op("pe", lambda e: e.matmul(ops_[h][par][hp, :], lhsT=V[:, j, hp], rhs=wb[kw][:], start=T["first"], stop=T["last"]),
             reads=[("V", j), f"w{kw}"], writes=[f"o{h}"])
        if T["last"]:
            p.op("dve", lambda e: e.tensor_copy(out=yo[h][par][hp, :], in_=ops_[h][par][hp, :]),
                 reads=[f"o{h}"], writes=[f"yo{h}{par}"])
            yw.write(128 + h * 64, hp, yo[h][par][hp, :], f"yo{h}{par}", I)

    for step in range(n + 5):
        for off, st in enumerate((stA, stB1, stB2, stC1, stC2, stD)):
            t = step - off
            if 0 <= t < n:
                st(t, tiles[t])
    p.end_phase()


def sb_consts():
    tri = (np.arange(128)[:, None] >= np.arange(128)[None, :]).astype(np.float32)
    masks = np.zeros((128, 4, 512), np.float32)
    for r in range(4):
        masks[:, r, :] = ((128 * r + np.arange(128))[:, None] < np.arange(512)[None, :]).astype(np.float32)
    return tri, masks


RET_GAMMA = [1.0 - 2.0 ** (-5.0 - h) for h in range(4)]


def emit_ret(p, io, L, yw_fn):
    NB = L // 512
    NT = L // 128
    wqk_d, wvg_d, cos_d, sin_d, dm_d, dec_d, gn_d, id_d = (io[k] for k in ("wqk", "wvg", "cos", "sin", "dmask", "dec", "gn",
                                                                          "ident"))
    yw = yw_fn()

    wqk = load_weight_bf16(p, wqk_d, 256, "wqk")
    wvg = load_weight_bf16(p, wvg_d, 512, "wvg", queue="act")
    cos = p.sb([128, NT, 4, 32], F32, "cos")
    sin = p.sb([128, NT, 4, 32], F32, "sin")
    dmask = p.sb([128, 2, 128], F32, "dmask")
    dec = p.sb([128, 6], F32, "dec")
    gn = p.sb([128, 256], F32, "gn")
    id32 = p.sb([128, 128], F32, "id32")
    ident = p.sb([128, 128], BF16, "ident")
    for t_, d_, n_ in ((cos, cos_d, "cos"), (sin, sin_d, "sin"), (dmask, dm_d, "dmask"), (dec, dec_d, "dec"),
                       (gn, gn_d, "gn"), (id32, id_d, "id32")):
        p.dma("act", t_[:], d_, writes=[n_])
    p.op("pool", lambda e: e.tensor_copy(out=ident[:], in_=id32[:]), reads=["id32"], writes=["ident"])

    st32 = p.sb([128, 128], F32, "st32")
    stb = p.sb([128, 128], BF16, "stb")
    p.op("pool", lambda e: e.memset(st32[:], 0.0), writes=["st32"])
    p.op("pool", lambda e: e.memset(stb[:], 0.0), writes=["stb"])

    qkps = p.ps([128, 256], F32, "qkps")
    vgps = p.ps([128, 512], F32, "vgps")
    trps = p.ps([128, 3, 128], BF16, "trps")
    scps = p.ps([128, 128], F32, "scps")
    ops_ = p.ps([128, 128], F32, "ops")
    kvps = p.ps([128, 128], F32, "kvps")

    ta = p.sb([128, 4, 32], F32, "ta")
    tb = p.sb([128, 4, 32], F32, "tb")
    tc_ = p.sb([128, 4, 32], F32, "tc")
    td = p.sb([128, 4, 32], F32, "td")
    R32 = p.sb([128, 4, 2, 32], F32, "R32")
    QKb = p.sb([128, 256], BF16, "QKb")
    QDb = p.sb([128, 128], BF16, "QDb")
    KDb = p.sb([128, 128], BF16, "KDb")
    TR = p.sb([128, 3, 128], BF16, "TR")
    Vb = p.sb([128, 256], BF16, "Vb")
    sg = p.sb([128, 256], F32, "sg")
    scb = p.sb([128, 128], BF16, "scb")
    junk = p.sb([128, 128], F32, "junk")
    stt = p.sb([128, 8], F32, "stt")
    on = p.sb([128, 128], F32, "on")
    yc = [p.sb([128, 256], F32, "yc") for _ in range(2)]
    ycT = [p.sb([128, 512], F32, "ycT") for _ in range(2)]
    typs = p.ps([128, 2, 128], F32, "typs")

    hs = HStream(p, io["hblk"])
    R32f = R32[:].rearrange("p a b c -> p (a b c)")
    for blk in range(NB):
        hb, hres = hs.load(blk)
        for sub in range(4):
            t = blk * 4 + sub
            ts_ = slice(sub * 128, (sub + 1) * 128)
            for kc in range(8):
                p.op("pe", lambda e, kc=kc, hb=hb, ts_=ts_: e.matmul(qkps[:], lhsT=hb[:, kc, ts_], rhs=wqk[:, kc, :],
                                                                     start=(kc == 0), stop=(kc == 7)),
                     reads=["wqk", hres], writes=["qkps"])
            for kc in range(8):
                p.op("pe", lambda e, kc=kc, hb=hb, ts_=ts_: e.matmul(vgps[:], lhsT=hb[:, kc, ts_], rhs=wvg[:, kc, :],
                                                                     start=(kc == 0), stop=(kc == 7)),
                     reads=["wvg", hres], writes=["vgps"])
            qk4 = qkps[:].rearrange("p (a b c) -> p a b c", a=4, b=2)
            t1 = qk4[:, :, 0, :]
            t2 = qk4[:, :, 1, :]
            p.op("dve", lambda e, t=t, t1=t1: e.tensor_tensor(out=ta[:], in0=t1, in1=cos[:, t, :, :], op=ALU.mult),
                 reads=["qkps", "cos"], writes=["ta"])
            p.op("dve", lambda e, t=t, t2=t2: e.tensor_tensor(out=tb[:], in0=t2, in1=sin[:, t, :, :], op=ALU.mult),
                 reads=["qkps", "sin"], writes=["tb"])
            p.op("dve", lambda e, t=t, t1=t1: e.tensor_tensor(out=tc_[:], in0=t1, in1=sin[:, t, :, :], op=ALU.mult),
                 reads=["qkps", "sin"], writes=["tc"])
            p.op("dve", lambda e, t=t, t2=t2: e.tensor_tensor(out=td[:], in0=t2, in1=cos[:, t, :, :], op=ALU.mult),
                 reads=["qkps", "cos"], writes=["td"])
            p.op("pool", lambda e: e.tensor_tensor(out=R32[:, :, 0, :], in0=ta[:], in1=tb[:], op=ALU.subtract),
                 reads=["ta", "tb"], writes=["R32"])
            p.op("pool", lambda e: e.tensor_tensor(out=R32[:, :, 1, :], in0=tc_[:], in1=td[:], op=ALU.add),
                 reads=["tc", "td"], writes=["R32"])
            p.op("act", lambda e: e.activation(out=QKb[:, 0:128], in_=R32f[:, 0:128], func=AF.Copy),
                 reads=["R32"], writes=["QKb"])
            p.op("act", lambda e: e.activation(out=QKb[:, 128:256], in_=R32f[:, 128:256], func=AF.Copy, scale=0.125),
                 reads=["R32"], writes=["QKb"])
            for h in range(2):
                p.op("dve", lambda e, h=h: e.tensor_scalar(out=QDb[:, h * 64:(h + 1) * 64], in0=R32f[:, h * 64:(h + 1) * 64],
                                                           scalar1=dec[:, h:h + 1], scalar2=None, op0=ALU.mult),
                     reads=["R32", "dec"], writes=["QDb"])
                p.op("dve", lambda e, h=h: e.tensor_scalar(out=KDb[:, h * 64:(h + 1) * 64],
                                                           in0=R32f[:, 128 + h * 64:128 + (h + 1) * 64],
                                                           scalar1=dec[:, 2 + h:3 + h], scalar2=None, op0=ALU.mult),
                     reads=["R32", "dec"], writes=["KDb"])
            for i_, (src, sres) in enumerate(((QKb[:, 0:128], "QKb"), (QKb[:, 128:256], "QKb"), (QDb[:], "QDb"))):
                p.op("pe", lambda e, i_=i_, src=src: e.transpose(trps[:, i_, :], src, ident[:]),
                     reads=[sres, "ident"], writes=["trps"])
            p.op("act", lambda e: e.activation(out=TR[:], in_=trps[:], func=AF.Copy), reads=["trps"], writes=["TR"])
            p.op("act", lambda e: e.activation(out=Vb[:], in_=vgps[:, 0:256], func=AF.Copy), reads=["vgps"], writes=["Vb"])
            p.op("act", lambda e: e.activation(out=sg[:], in_=vgps[:, 256:512], func=AF.Silu), reads=["vgps"], writes=["sg"])
            yk = t % 2
            for h in range(2):
                hp = slice(h * 64, (h + 1) * 64)
                vs = slice(h * 128, (h + 1) * 128)
                p.op("pe", lambda e, hp=hp: e.matmul(scps[:], lhsT=TR[hp, 1, :], rhs=TR[hp, 0, :], start=True, stop=True),
                     reads=["TR"], writes=["scps"])
                p.op("dve", lambda e, h=h: e.tensor_tensor(out=scb[:], in0=scps[:], in1=dmask[:, h, :], op=ALU.mult),
                     reads=["scps", "dmask"], writes=["scb"])
                p.op("pe", lambda e, vs=vs, t=t: e.matmul(ops_[:], lhsT=scb[:], rhs=Vb[:, vs], start=True, stop=(t == 0)),
                     reads=["scb", "Vb"], writes=["ops"])
                if t > 0:
                    p.op("pe", lambda e, hp=hp: e.matmul(ops_[:], lhsT=TR[hp, 2, :], rhs=stb[hp, :], start=False, stop=True),
                         reads=["TR", "stb"], writes=["ops"])
                p.op("pe", lambda e, h=h, hp=hp, vs=vs: e.matmul(kvps[hp, :], lhsT=KDb[:, h * 64:(h + 1) * 64], rhs=Vb[:, vs],
                                                                 start=True, stop=True),
                     reads=["KDb", "Vb"], writes=["kvps"])
                p.op("dve", lambda e, h=h, hp=hp: e.scalar_tensor_tensor(out=st32[hp, :], in0=st32[hp, :],
                                                                          scalar=dec[hp, 4 + h:5 + h], in1=kvps[hp, :],
                                                                          op0=ALU.mult, op1=ALU.add),
                     reads=["st32", "kvps", "dec"], writes=["st32"])
                p.op("pool", lambda e, hp=hp: e.tensor_copy(out=stb[hp, :], in_=st32[hp, :]), reads=["st32"], writes=["stb"])
                p.op("act", lambda e: e.activation(out=junk[:], in_=ops_[:], func=AF.Identity, accum_out=stt[:, 0:1]),
                     reads=["ops"], writes=["junk", "stt"])
                p.op("act", lambda e: e.activation(out=junk[:], in_=ops_[:], func=AF.Square, accum_out=stt[:, 1:2]),
                     reads=["ops"], writes=["junk", "stt"])
                p.op("dve", lambda e: e.tensor_scalar(out=stt[:, 2:3], in0=stt[:, 0:1], scalar1=1.0 / 128, scalar2=None,
                                                      op0=ALU.mult), reads=["stt"], writes=["stt"])
                p.op("dve", lambda e: e.tensor_tensor(out=stt[:, 3:4], in0=stt[:, 2:3], in1=stt[:, 2:3], op=ALU.mult),
                     reads=["stt"], writes=["stt"])
                p.op("dve", lambda e: e.scalar_tensor_tensor(out=stt[:, 4:5], in0=stt[:, 1:2], scalar=1.0 / 128,
                                                             in1=stt[:, 3:4], op0=ALU.mult, op1=ALU.subtract),
                     reads=["stt"], writes=["stt"])
                p.op("act", lambda e: e.activation(out=stt[:, 6:7], in_=stt[:, 4:5], func=AF.Sqrt, bias=LN_EPS),
                     reads=["stt"], writes=["stt"])
                p.op("dve", lambda e: e.reciprocal(out=stt[:, 5:6], in_=stt[:, 6:7]), reads=["stt"], writes=["stt"])
                p.op("dve", lambda e: e.tensor_scalar(out=on[:], in0=ops_[:], scalar1=stt[:, 2:3], scalar2=stt[:, 5:6],
                                                      op0=ALU.subtract, op1=ALU.mult), reads=["ops", "stt"], writes=["on"])
                p.op("pool", lambda e, vs=vs: e.tensor_tensor(out=on[:], in0=on[:], in1=gn[:, vs], op=ALU.mult),
                     reads=["on", "gn"], writes=["on"])
                p.op("pool", lambda e, vs=vs, yk=yk: e.tensor_tensor(out=yc[yk][:, vs], in0=on[:], in1=sg[:, vs], op=ALU.mult),
                     reads=["on", "sg"], writes=[f"yc{yk}"])
            for cch in range(2):
                p.op("pe", lambda e, cch=cch, yk=yk: e.transpose(typs[:, cch, :], yc[yk][:, cch * 128:(cch + 1) * 128], id32[:]),
                     reads=[f"yc{yk}", "id32"], writes=["typs"])
            p.op("act", lambda e, sub=sub: e.activation(out=ycT[0][:, sub * 128:(sub + 1) * 128], in_=typs[:, 0, :], func=AF.Copy),
                 reads=["typs"], writes=["ycT0"])
            p.op("act", lambda e, sub=sub: e.activation(out=ycT[1][:, sub * 128:(sub + 1) * 128], in_=typs[:, 1, :], func=AF.Copy),
                 reads=["typs"], writes=["ycT1"])
        for cch in range(2):
            yw.write(256 + cch * 128, slice(0, 128), ycT[cch][:], f"ycT{cch}", blk)
    p.end_phase()


def ret_consts(L, hf):
    NT = L // 128
    half = 32
    inv_freq = (10000.0 ** (-np.arange(half, dtype=np.float32) / half)).astype(np.float32)
    pos = np.arange(L, dtype=np.float32)
    ang = (pos[:, None] * inv_freq[None, :]).astype(np.float32)
    c = np.cos(ang).astype(np.float32).reshape(NT, 128, 32).transpose(1, 0, 2)
    s = np.sin(ang).astype(np.float32).reshape(NT, 128, 32).transpose(1, 0, 2)
    cos = np.ascontiguousarray(np.broadcast_to(c[:, :, None, :], (128, NT, 4, 32)))
    sin = np.ascontiguousarray(np.broadcast_to(s[:, :, None, :], (128, NT, 4, 32)))
    idx = np.arange(128)
    dmask = np.zeros((128, 2, 128), np.float32)
    dec = np.zeros((128, 6), np.float32)
    for h in range(2):
        g = RET_GAMMA[2 * hf + h]
        lg = math.log(g)
        sI = idx[:, None]
        cI = idx[None, :]
        same = (sI // 64) == (cI // 64)
        m = np.where(same, np.exp(lg * np.abs(cI - sI)), np.where(cI > sI, np.exp(lg * (cI - sI)), 0.0))
        dmask[:, h, :] = m
        dec[:, h] = np.exp(lg * (idx + 1.0))
        dec[:, 2 + h] = 0.125 * np.exp(lg * (127.0 - idx))
        dec[:, 4 + h] = math.exp(lg * 128.0)
    return cos, sin, dmask, dec, np.eye(128, dtype=np.float32)

def emit_lambar(p, lre, lim, ldt, shape, tag):
    def T(n):
        return p.sb(shape, F32, tag + n)
    dt, a, th, mag, c, s, t1, t2, c2, s2, lbr, lbi = (T(n) for n in
                                                     ("dt", "a", "th", "mag", "c", "s", "t1", "t2", "c2", "s2", "lbr", "lbi"))
    R = tag + "res"
    p.op("act", lambda e: e.activation(out=dt[:], in_=ldt[:], func=AF.Exp), reads=[tag + "in"], writes=[R])
    p.op("dve", lambda e: e.tensor_tensor(out=a[:], in0=lre[:], in1=dt[:], op=ALU.mult), reads=[tag + "in", R], writes=[R])
    p.op("dve", lambda e: e.tensor_tensor(out=th[:], in0=lim[:], in1=dt[:], op=ALU.mult), reads=[tag + "in", R], writes=[R])
    p.op("act", lambda e: e.activation(out=mag[:], in_=a[:], func=AF.Exp), reads=[R], writes=[R])
    p.op("act", lambda e: e.activation(out=s[:], in_=th[:], func=AF.Sin, scale=1.0 / 16), reads=[R], writes=[R])
    p.op("dve", lambda e: e.tensor_scalar(out=t1[:], in0=th[:], scalar1=-1.0 / 16, scalar2=math.pi / 2, op0=ALU.mult,
                                          op1=ALU.add), reads=[R], writes=[R])
    p.op("act", lambda e: e.activation(out=c[:], in_=t1[:], func=AF.Sin), reads=[R], writes=[R])
    cur = (c, s)
    nxt = (c2, s2)
    for _ in range(4):
        cc, ss = cur
        nc_, ns_ = nxt
        p.op("dve", lambda e, cc=cc: e.tensor_tensor(out=t1[:], in0=cc[:], in1=cc[:], op=ALU.mult), reads=[R], writes=[R])
        p.op("dve", lambda e, ss=ss: e.tensor_tensor(out=t2[:], in0=ss[:], in1=ss[:], op=ALU.mult), reads=[R], writes=[R])
        p.op("dve", lambda e, cc=cc, ss=ss, ns_=ns_: e.scalar_tensor_tensor(out=ns_[:], in0=cc[:], scalar=2.0, in1=ss[:],
                                                                            op0=ALU.mult, op1=ALU.mult), reads=[R], writes=[R])
        p.op("dve", lambda e, nc_=nc_: e.tensor_tensor(out=nc_[:], in0=t1[:], in1=t2[:], op=ALU.subtract), reads=[R], writes=[R])
        cur, nxt = nxt, cur
    cc, ss = cur
    p.op("dve", lambda e: e.tensor_tensor(out=lbr[:], in0=mag[:], in1=cc[:], op=ALU.mult), reads=[R], writes=[R])
    p.op("dve", lambda e: e.tensor_tensor(out=lbi[:], in0=mag[:], in1=ss[:], op=ALU.mult), reads=[R], writes=[R])
    return lbr, lbi, R, (t1, t2, a, th, c, s)


def emit_s5(p, io, L, yw_fn, SEG=1024):
    stop = 0
    SEG = min(SEG, L)
    NSEG = L // SEG
    NBS = SEG // 512
    NK = int(math.log2(SEG))
    wu_d, rep_d, col_d, bp_d, cp_d, dcol_d = (io[k] for k in ("wu", "lam_rep", "lam_col", "bp", "cp", "dcol"))
    yw = yw_fn()

    wu = load_weight_bf16(p, wu_d, 128, "wu")
    rep = p.sb([128, 3, 512], F32, "rep")
    col = p.sb([128, 3, 4], F32, "col")
    bp = p.sb([128, 2, 4, 128], F32, "bp")
    cp = p.sb([128, 2, 4, 128], F32, "cp")
    dcol = p.sb([128, 1], F32, "dcol")
    p.dma("act", rep[:], rep_d, writes=["repin"])
    p.dma("act", col[:], col_d, writes=["colin"])
    p.dma("act", bp[:], bp_d, writes=["bp"])
    p.dma("act", cp[:], cp_d, writes=["cp"])
    p.dma("act", dcol[:], dcol_d, writes=["dcol"])

    lbr, lbi, R, (t1, t2, t3, t4, t5, t6) = emit_lambar(p, rep[:, 0, :], rep[:, 1, :], rep[:, 2, :], [128, 512], "rep")
    lr, li = rep[:, 0, :], rep[:, 1, :]
    zr = p.sb([128, 512], F32, "zr")
    zi = p.sb([128, 512], F32, "zi")
    nr = p.sb([128, 512], F32, "nr")
    p.op("dve", lambda e: e.tensor_scalar(out=nr[:], in0=lbr[:], scalar1=-1.0, scalar2=None, op0=ALU.add), reads=[R], writes=[R])
    p.op("dve", lambda e: e.tensor_tensor(out=t1[:], in0=lr, in1=lr, op=ALU.mult), reads=["repin", R], writes=[R])
    p.op("dve", lambda e: e.tensor_tensor(out=t2[:], in0=li, in1=li, op=ALU.mult), reads=["repin", R], writes=[R])
    p.op("dve", lambda e: e.tensor_tensor(out=t3[:], in0=t1[:], in1=t2[:], op=ALU.add), reads=[R], writes=[R])
    p.op("dve", lambda e: e.reciprocal(out=t4[:], in_=t3[:]), reads=[R], writes=[R])
    p.op("dve", lambda e: e.tensor_tensor(out=t1[:], in0=nr[:], in1=lr, op=ALU.mult), reads=[R], writes=[R])
    p.op("dve", lambda e: e.tensor_tensor(out=t2[:], in0=lbi[:], in1=li, op=ALU.mult), reads=[R], writes=[R])
    p.op("dve", lambda e: e.tensor_tensor(out=t1[:], in0=t1[:], in1=t2[:], op=ALU.add), reads=[R], writes=[R])
    p.op("dve", lambda e: e.tensor_tensor(out=zr[:], in0=t1[:], in1=t4[:], op=ALU.mult), reads=[R], writes=[R])
    p.op("dve", lambda e: e.tensor_tensor(out=t1[:], in0=lbi[:], in1=lr, op=ALU.mult), reads=[R], writes=[R])
    p.op("dve", lambda e: e.tensor_tensor(out=t2[:], in0=nr[:], in1=li, op=ALU.mult), reads=[R], writes=[R])
    p.op("dve", lambda e: e.tensor_tensor(out=t1[:], in0=t1[:], in1=t2[:], op=ALU.subtract), reads=[R], writes=[R])
    p.op("dve", lambda e: e.tensor_tensor(out=zi[:], in0=t1[:], in1=t4[:], op=ALU.mult), reads=[R], writes=[R])
    bb = p.sb([128, 2, 4, 128], BF16, "bb")
    ncp = p.sb([128, 2, 4, 128], BF16, "cpb")
    for pr in range(4):
        ps_ = slice(pr * 128, (pr + 1) * 128)
        u1 = t5[:, 0:128]
        u2 = t6[:, 0:128]
        p.op("dve", lambda e, pr=pr, ps_=ps_: e.tensor_tensor(out=u1, in0=bp[:, 0, pr, :], in1=zr[:, ps_], op=ALU.mult),
             reads=["bp", R], writes=[R])
        p.op("dve", lambda e, pr=pr, ps_=ps_: e.tensor_tensor(out=u2, in0=bp[:, 1, pr, :], in1=zi[:, ps_], op=ALU.mult),
             reads=["bp", R], writes=[R])
        p.op("dve", lambda e, pr=pr: e.tensor_tensor(out=bb[:, 0, pr, :], in0=u1, in1=u2, op=ALU.subtract),
             reads=[R], writes=[R, "bb"])
        p.op("dve", lambda e, pr=pr, ps_=ps_: e.tensor_tensor(out=u1, in0=bp[:, 0, pr, :], in1=zi[:, ps_], op=ALU.mult),
             reads=["bp", R], writes=[R])
        p.op("dve", lambda e, pr=pr, ps_=ps_: e.tensor_tensor(out=u2, in0=bp[:, 1, pr, :], in1=zr[:, ps_], op=ALU.mult),
             reads=["bp", R], writes=[R])
        p.op("dve", lambda e, pr=pr: e.tensor_tensor(out=bb[:, 1, pr, :], in0=u1, in1=u2, op=ALU.add),
             reads=[R], writes=[R, "bb"])
    p.op("act", lambda e: e.activation(out=ncp[:, 0, :, :], in_=cp[:, 0, :, :], func=AF.Copy), reads=["cp"], writes=["cpb"])
    p.op("act", lambda e: e.activation(out=ncp[:, 1, :, :], in_=cp[:, 1, :, :], func=AF.Copy, scale=-1.0),
         reads=["cp"], writes=["cpb"])

    cbr, cbi, RC, (c1, c2, _, _, _, _) = emit_lambar(p, col[:, 0, :], col[:, 1, :], col[:, 2, :], [128, 4], "col")
    AR = p.sb([128, NK + 1, 4], F32, "AR")
    AI = p.sb([128, NK + 1, 4], F32, "AI")
    NAI = p.sb([128, NK + 1, 4], F32, "NAI")
    p.op("dve", lambda e: e.tensor_copy(out=AR[:, 0, :], in_=cbr[:]), reads=[RC], writes=["A"])
    p.op("dve", lambda e: e.tensor_copy(out=AI[:, 0, :], in_=cbi[:]), reads=[RC], writes=["A"])
    for k in range(1, NK + 1):
        p.op("dve", lambda e, k=k: e.tensor_tensor(out=c1[:], in0=AR[:, k - 1, :], in1=AR[:, k - 1, :], op=ALU.mult),
             reads=["A", RC], writes=[RC])
        p.op("dve", lambda e, k=k: e.tensor_tensor(out=c2[:], in0=AI[:, k - 1, :], in1=AI[:, k - 1, :], op=ALU.mult),
             reads=["A", RC], writes=[RC])
        p.op("dve", lambda e, k=k: e.tensor_tensor(out=AR[:, k, :], in0=c1[:], in1=c2[:], op=ALU.subtract),
             reads=[RC, "A"], writes=["A"])
        p.op("dve", lambda e, k=k: e.scalar_tensor_tensor(out=AI[:, k, :], in0=AR[:, k - 1, :], scalar=2.0, in1=AI[:, k - 1, :],
                                                          op0=ALU.mult, op1=ALU.mult), reads=["A"], writes=["A"])
    p.op("dve", lambda e: e.tensor_scalar(out=NAI[:], in0=AI[:], scalar1=-1.0, scalar2=None, op0=ALU.mult),
         reads=["A"], writes=["A"])

    UT = p.sb([128, SEG], BF16, "UT")
    U32 = p.sb([128, SEG], F32, "U32")
    RE = [[p.sb([128, SEG], F32, "RE") for _ in range(2)] for _ in range(2)]
    IM = [[p.sb([128, SEG], F32, "IM") for _ in range(2)] for _ in range(2)]
    REb = p.sb([128, 4, SEG], BF16, "REb")
    IMb = p.sb([128, 4, SEG], BF16, "IMb")
    carry = p.sb([128, 4, 2], F32, "carry")
    tmpB = [p.sb([128, SEG], F32, "tmpB") for _ in range(2)]
    ctmp = p.sb([128, 4], F32, "ctmp")
    ups = [p.ps([128, 512], F32, "ups") for _ in range(2)]
    bps = [p.ps([128, 512], F32, "bps") for _ in range(2)]
    yps = [p.ps([128, 512], F32, "yps") for _ in range(2)]
    g1 = p.sb([128, 512], F32, "g1")
    g2 = p.sb([128, 512], F32, "g2")
    g3 = p.sb([128, 512], F32, "g3")
    yo = [p.sb([128, 512], F32, "yo") for _ in range(2)]
    hs = HStream(p, io["hblk"])
    for sg_ in range(NSEG):
        for b_ in range(NBS):
            blk = sg_ * NBS + b_
            hb, hres = hs.load(blk)
            k = b_ % 2
            for kc in range(8):
                p.op("pe", lambda e, k=k, kc=kc, hb=hb: e.matmul(ups[k][:], lhsT=wu[:, kc, :], rhs=hb[:, kc, :],
                                                                 start=(kc == 0), stop=(kc == 7)),
                     reads=["wu", hres], writes=[f"ups{k}"])
            cs = slice(b_ * 512, (b_ + 1) * 512)
            p.op("dve", lambda e, k=k, cs=cs: e.tensor_copy(out=U32[:, cs], in_=ups[k][:]),
                 reads=[f"ups{k}"], writes=[("U32", b_)])
            p.op("act", lambda e, cs=cs: e.activation(out=UT[:, cs], in_=U32[:, cs], func=AF.Copy),
                 reads=[("U32", b_)], writes=[("UT", b_)])
        for pg in range(2):
            lanes = ((0, 2 * pg), (1, 2 * pg + 1))
            for ln, pr in lanes:
                for b_ in range(NBS):
                    cs = slice(b_ * 512, (b_ + 1) * 512)
                    for ri, dst in enumerate((RE[ln][0], IM[ln][0])):
                        k = (b_ * 2 + ri) % 2
                        p.op("pe", lambda e, k=k, ri=ri, pr=pr, cs=cs: e.matmul(bps[k][:], lhsT=bb[:, ri, pr, :], rhs=UT[:, cs],
                                                                                start=True, stop=True),
                             reads=["bb", ("UT", b_)], writes=[f"bps{k}"])
                        p.op("act", lambda e, k=k, dst=dst, cs=cs: e.activation(out=dst[:, cs], in_=bps[k][:], func=AF.Copy),
                             reads=[f"bps{k}"], writes=[("RI", ln, 0, ri)])
                if sg_ > 0:
                    cr = carry[:, pr, 0:1]
                    ci = carry[:, pr, 1:2]
                    R0, I0 = RE[ln][0], IM[ln][0]
                    for dstT, ri, pairA, pairB in ((R0, 0, (AR, cr), (NAI, ci)), (I0, 1, (AI, cr), (AR, ci))):
                        for sc, cc in (pairA, pairB):
                            p.op("dve", lambda e, pr=pr, dstT=dstT, sc=sc, cc=cc: e.scalar_tensor_tensor(
                                out=dstT[:, 0:1], in0=cc, scalar=sc[:, 0, pr:pr + 1], in1=dstT[:, 0:1], op0=ALU.mult, op1=ALU.add),
                                reads=["carry", "A", ("RI", ln, 0, ri)], writes=[("RI", ln, 0, ri)])
            src = 0
            for k in range(NK):
                d = 1 << k
                dst = 1 - src
                for ln, pr in lanes:
                    sR, sI, dR, dI = RE[ln][src], IM[ln][src], RE[ln][dst], IM[ln][dst]
                    ar = AR[:, k, pr:pr + 1]
                    ai = AI[:, k, pr:pr + 1]
                    nai = NAI[:, k, pr:pr + 1]
                    tB = tmpB[ln]
                    rs = [("RI", ln, src, 0), ("RI", ln, src, 1), "A"]
                    p.op("act", lambda e, sR=sR, dR=dR, d=d: e.activation(out=dR[:, 0:d], in_=sR[:, 0:d], func=AF.Copy),
                         reads=rs, writes=[("RI", ln, dst, 0)])
                    p.op("act", lambda e, sI=sI, dI=dI, d=d: e.activation(out=dI[:, 0:d], in_=sI[:, 0:d], func=AF.Copy),
                         reads=rs, writes=[("RI", ln, dst, 1)])
                    p.op("dve", lambda e, sR=sR, dR=dR, d=d, ar=ar: e.scalar_tensor_tensor(
                        out=dR[:, d:SEG], in0=sR[:, 0:SEG - d], scalar=ar, in1=sR[:, d:SEG], op0=ALU.mult, op1=ALU.add),
                        reads=rs, writes=[("RI", ln, dst, 0)])
                    p.op("act", lambda e, sI=sI, d=d, ar=ar, tB=tB: e.activation(out=tB[:, d:SEG], in_=sI[:, 0:SEG - d],
                                                                                 func=AF.Identity, scale=ar),
                         reads=rs, writes=[f"tmpB{ln}"])
                    p.op("dve", lambda e, sR=sR, sI=sI, dI=dI, d=d, ai=ai: e.scalar_tensor_tensor(
                        out=dI[:, d:SEG], in0=sR[:, 0:SEG - d], scalar=ai, in1=sI[:, d:SEG], op0=ALU.mult, op1=ALU.add),
                        reads=rs, writes=[("RI", ln, dst, 1)])
                    p.op("dve", lambda e, sI=sI, dR=dR, d=d, nai=nai: e.scalar_tensor_tensor(
                        out=dR[:, d:SEG], in0=sI[:, 0:SEG - d], scalar=nai, in1=dR[:, d:SEG], op0=ALU.mult, op1=ALU.add),
                        reads=rs + [("RI", ln, dst, 0)], writes=[("RI", ln, dst, 0)])
                    p.op("pool", lambda e, dI=dI, d=d, tB=tB: e.tensor_tensor(out=dI[:, d:SEG], in0=tB[:, d:SEG], in1=dI[:, d:SEG],
                                                                              op=ALU.add),
                         reads=[f"tmpB{ln}", ("RI", ln, dst, 1)], writes=[("RI", ln, dst, 1)])
                src = dst
            for ln, pr in lanes:
                fR, fI = RE[ln][src], IM[ln][src]
                p.op("act", lambda e, fR=fR, pr=pr: e.activation(out=REb[:, pr, :], in_=fR[:], func=AF.Copy),
                     reads=[("RI", ln, src, 0)], writes=[("Sb", pr)])
                p.op("act", lambda e, fI=fI, pr=pr: e.activation(out=IMb[:, pr, :], in_=fI[:], func=AF.Copy),
                     reads=[("RI", ln, src, 1)], writes=[("Sb", pr)])
                p.op("dve", lambda e, fR=fR, pr=pr: e.tensor_copy(out=carry[:, pr, 0:1], in_=fR[:, SEG - 1:SEG]),
                     reads=[("RI", ln, src, 0)], writes=["carry"])
                p.op("dve", lambda e, fI=fI, pr=pr: e.tensor_copy(out=carry[:, pr, 1:2], in_=fI[:, SEG - 1:SEG]),
                     reads=[("RI", ln, src, 1)], writes=["carry"])
        for b_ in range(NBS):
            blk = sg_ * NBS + b_
            k = b_ % 2
            cs = slice(b_ * 512, (b_ + 1) * 512)
            n = 0
            for pr in range(4):
                for ri, Sx in enumerate((REb, IMb)):
                    p.op("pe", lambda e, k=k, pr=pr, ri=ri, Sx=Sx, cs=cs, n=n: e.matmul(
                        yps[k][:], lhsT=ncp[:, ri, pr, :], rhs=Sx[:, pr, cs], start=(n == 0), stop=(n == 7)),
                        reads=["cpb", ("Sb", pr)], writes=[f"yps{k}"])
                    n += 1
            p.op("dve", lambda e, k=k, cs=cs: e.scalar_tensor_tensor(out=g1[:], in0=U32[:, cs], scalar=dcol[:, 0:1], in1=yps[k][:],
                                                                     op0=ALU.mult, op1=ALU.add),
                 reads=[f"yps{k}", ("U32", b_), "dcol"], writes=["g1"])
            p.op("act", lambda e: e.activation(out=g2[:], in_=g1[:], func=AF.Square), reads=["g1"], writes=["g2"])
            p.op("dve", lambda e: e.tensor_scalar(out=g2[:], in0=g2[:], scalar1=0.044715, scalar2=1.0, op0=ALU.mult, op1=ALU.add),
                 reads=["g2"], writes=["g2"])
            p.op("dve", lambda e: e.tensor_tensor(out=g2[:], in0=g2[:], in1=g1[:], op=ALU.mult), reads=["g2", "g1"], writes=["g2"])
            p.op("act", lambda e: e.activation(out=g3[:], in_=g2[:], func=AF.Tanh, scale=math.sqrt(2.0 / math.pi)),
                 reads=["g2"], writes=["g3"])
            p.op("pool", lambda e: e.tensor_scalar(out=g1[:], in0=g1[:], scalar1=0.5, scalar2=None, op0=ALU.mult),
                 reads=["g1", "g2"], writes=["g1"])
            p.op("dve", lambda e, k=k: e.scalar_tensor_tensor(out=yo[k][:], in0=g3[:], scalar=1.0, in1=g1[:], op0=ALU.add,
                                                              op1=ALU.mult), reads=["g3", "g1"], writes=[f"yo{k}"])
            yw.write(0, slice(0, 128), yo[k][:], f"yo{k}", blk)
    p.end_phase()


def s5_host_layout(hf, lam_re, lam_im, log_dt, b_re, b_im, c_re, c_im, d_skip):
    G0 = 8 * hf
    f32 = np.float32
    rep = np.zeros((128, 3, 512), f32)
    rep[:, 0, :] = lam_re[G0:G0 + 8].reshape(1, 512)
    rep[:, 1, :] = lam_im[G0:G0 + 8].reshape(1, 512)
    rep[:, 2, :] = np.repeat(log_dt[G0:G0 + 8], 64)[None, :]
    col = np.zeros((128, 3, 4), f32)
    bp = np.zeros((128, 2, 4, 128), f32)
    cp = np.zeros((128, 2, 4, 128), f32)
    for pr in range(4):
        for gi in range(2):
            gl = 2 * pr + gi
            g = G0 + gl
            rows = slice(gi * 64, (gi + 1) * 64)
            col[rows, 0, pr] = lam_re[g]
            col[rows, 1, pr] = lam_im[g]
            col[rows, 2, pr] = log_dt[g]
            cols16 = slice(gl * 16, (gl + 1) * 16)
            bp[cols16, 0, pr, rows] = b_re[g].T
            bp[cols16, 1, pr, rows] = b_im[g].T
            cp[rows, 0, pr, cols16] = c_re[g].T
            cp[rows, 1, pr, cols16] = c_im[g].T
    dcol = np.ascontiguousarray(d_skip[hf * 128:(hf + 1) * 128].reshape(128, 1), dtype=f32)
    return rep, col, bp, cp, dcol


class Stager:
    def __init__(self, p, n=2, queues=("sp", "act"), cast_engs=("pool",), width=256):
        self.p = p
        self.width = width
        self.t = [p.sb([128, 8, width], F32, "stage") for _ in range(n)]
        self.i = 0
        self.queues = queues
        self.cast_engs = cast_engs

    def load(self, dst_fn, src3, kcn, ncols, dst_res):
        p = self.p
        chunk = self.width
        for c0 in range(0, ncols, chunk):
            w = min(chunk, ncols - c0)
            k = self.i % len(self.t)
            q = self.queues[self.i % len(self.queues)]
            ce = self.cast_engs[self.i % len(self.cast_engs)]
            self.i += 1
            st = self.t[k]
            p.dma(q, st[:, 0:kcn, 0:w], src3[:, :, c0:c0 + w], writes=[f"stage{k}"])
            dst = dst_fn(c0, w)
            p.op(ce, lambda e, st=st, dst=dst, kcn=kcn, w=w: e.tensor_copy(out=dst, in_=st[:, 0:kcn, 0:w]),
                 reads=[f"stage{k}"], writes=[dst_res])


def emit_ln_fm(p, r, rres, gcol, bcol, out, ores, ones32, psA, psB, sq, mean, rstd, tmp, tag, sqres=None):
    psa, psares = psA
    sqres = sqres or (sqres)
    psb, psbres = psB
    for f in range(8):
        p.op("pe", lambda e, f=f: e.matmul(psa[:], lhsT=ones32[:], rhs=r[:, f, :], start=(f == 0), stop=(f == 7)),
             reads=["ones32", rres], writes=[psares])
    p.op("act", lambda e: e.activation(out=sq[:], in_=r[:], func=AF.Square), reads=[rres], writes=[sqres])
    for f in range(8):
        p.op("pe", lambda e, f=f: e.matmul(psb[:], lhsT=ones32[:], rhs=sq[:, f, :], start=(f == 0), stop=(f == 7)),
             reads=["ones32", sqres], writes=[psbres])
    p.op("act", lambda e: e.activation(out=mean[:], in_=psa[:], func=AF.Copy, scale=1.0 / D_MODEL),
         reads=[psares], writes=[tag + "mean"])
    p.op("dve", lambda e: e.tensor_tensor(out=tmp[:], in0=mean[:], in1=mean[:], op=ALU.mult),
         reads=[tag + "mean"], writes=[tag + "tmp"])
    p.op("dve", lambda e: e.scalar_tensor_tensor(out=tmp[:], in0=psb[:], scalar=1.0 / D_MODEL, in1=tmp[:], op0=ALU.mult,
                                                 op1=ALU.subtract), reads=[psbres, tag + "tmp"], writes=[tag + "tmp"])
    p.op("act", lambda e: e.activation(out=tmp[:], in_=tmp[:], func=AF.Sqrt, bias=LN_EPS), reads=[tag + "tmp"],
         writes=[tag + "tmp"])
    p.op("dve", lambda e: e.reciprocal(out=rstd[:], in_=tmp[:]), reads=[tag + "tmp"], writes=[tag + "rstd"])
    for f in range(8):
        p.op("dve", lambda e, f=f: e.tensor_tensor(out=sq[:, f, :], in0=r[:, f, :], in1=mean[:], op=ALU.subtract),
             reads=[rres, tag + "mean", sqres], writes=[sqres])
        p.op("pool", lambda e, f=f: e.tensor_tensor(out=sq[:, f, :], in0=sq[:, f, :], in1=rstd[:], op=ALU.mult),
             reads=[sqres, tag + "rstd"], writes=[sqres])
        p.op("act", lambda e, f=f: e.activation(out=out[:, f, :], in_=sq[:, f, :], func=AF.Identity, scale=gcol[:, f:f + 1],
                                                bias=bcol[:, f:f + 1]), reads=[sqres, "lncols"], writes=[ores])


def emit_post(p, io, T):
    NB = T // 512
    v3 = lambda ap: ap.rearrange("(kc p) n -> p kc n", p=128)
    hT_d = v3(io["hT_half"])
    hTb_d = v3(io["hT_hbf"])
    y_d = v3(io["y_own"])
    wg_d, wglu_d, wua_d, wub_d, wuc_d, wo_d, wr_d = (v3(io[k]) for k in ("wg", "wglu", "wua", "wub", "wuc", "wo", "wr"))
    cols_d, rb_d, id_d = io["cols1"], io["rb"], io["ident"]
    h1tok_o, gate_o, rk_o, racc_o = io["h1tok"], io["gate"], io["rk"], io["racc"]

    wg = p.sb([128, 8, 3072], BF16, "wg")
    wglu = p.sb([128, 2, 256], BF16, "wglu")
    wua = p.sb([128, 2, D_MODEL], BF16, "wua")
    wub = p.sb([128, 2, D_MODEL], BF16, "wub")
    wuc = p.sb([128, 4, D_MODEL], BF16, "wuc")
    wo = p.sb([128, 8, D_MODEL], BF16, "wo")
    cols = p.sb([128, 18], F32, "cols")
    wr = p.sb([128, 8, 16], F32, "wr")
    rb = p.sb([128, 16], F32, "rb")
    ones32 = p.sb([128, 128], F32, "ones32")
    p.dma("sp", cols[:], cols_d, writes=["lncols"])
    p.dma("sp", wr[:], wr_d, writes=["wr"])
    p.dma("sp", rb[:], rb_d, writes=["rb"])
    id32 = p.sb([128, 128], F32, "id32")
    p.dma("sp", id32[:], id_d, writes=["id32"])
    p.op("pool", lambda e: e.memset(ones32[:], 1.0), writes=["ones32"])
    stg = Stager(p, n=2)
    stg.load(lambda c0, w: wglu[:, :, c0:c0 + w], wglu_d, 2, 256, "wglu")
    stg.load(lambda c0, w: wua[:, :, c0:c0 + w], wua_d, 2, D_MODEL, "wua")
    stg.load(lambda c0, w: wub[:, :, c0:c0 + w], wub_d, 2, D_MODEL, "wub")
    stg.load(lambda c0, w: wuc[:, :, c0:c0 + w], wuc_d, 4, D_MODEL, "wuc")
    stg.load(lambda c0, w: wg[:, :, c0:c0 + w], wg_d, 8, 3072, "wg")
    stg.load(lambda c0, w: wo[:, :, c0:c0 + w], wo_d, 8, D_MODEL, "wo")

    h32 = p.sb([128, 8, 512], F32, "h32")
    hb = p.sb([128, 8, 512], BF16, "hb")
    y32 = p.sb([128, 8, 512], F32, "y32")
    yb16 = p.sb([128, 8, 512], BF16, "yb16")
    sgm = p.sb([128, 512], F32, "sgm")
    sig = [p.sb([128, 512], F32, "sig") for _ in range(2)]
    macc = p.sb([128, 512], F32, "macc")
    mtmp = p.sb([128, 512], F32, "mtmp")
    mb = p.sb([128, 8, 512], BF16, "mb")
    r = p.sb([128, 8, 512], F32, "r")
    sq = y32
    h1 = r
    mean = p.sb([128, 512], F32, "mean")
    rstd = p.sb([128, 512], F32, "rstd")
    tmp = p.sb([128, 512], F32, "tmp")
    PS = [p.ps([128, 512], F32, "PS") for _ in range(7)]
    rps = p.ps([128, 128], F32, "rps")
    xs_z = io["xs"]
    zt = p.sb([128, D_MODEL], BF16, "zt")
    p.op("pool", lambda e: e.memset(zt[:], 0.0), writes=["zt"])
    for s_ in range(xs_z.shape[0] // 128):
        p.dma("act" if s_ % 2 else "sp", xs_z[s_ * 128:(s_ + 1) * 128, :], zt[:], reads=["zt"], writes=["xs"])
    LT32 = p.sb([128, 128], F32, "LT32")
    p.dma("sp", LT32[:], io["lt"], writes=["LT32"])
    Mt = p.sb([128, 16], F32, "Mt")
    Racc = p.sb([128, 16], F32, "Racc")
    rk_sb = [p.sb([128, 16], F32, "rk") for _ in range(2)]
    h1tok = [p.sb([128, D_MODEL], F32, "h1tok") for _ in range(2)]
    psi = [0]

    def nps():
        k = psi[0] % 7
        psi[0] += 1
        return PS[k], f"PS{k}"

    lg = p.sb([128, 16], F32, "lg")
    mx = p.sb([128, 4], F32, "mx")
    exd = p.sb([128, 4, 8], F32, "exd")
    pr6 = p.sb([128, 4, 6], F32, "pr6")
    gs = p.sb([128, 8], F32, "gs")
    cnt = p.sb([128, 4, 4], F32, "cnt")
    cmp_ = p.sb([128, 4, 4], F32, "cmp")
    gsel = p.sb([128, 16], F32, "gsel")
    gout = [p.sb([128, 16], F32, "gout") for _ in range(2)]
    ybranch = ((wua, "wua", (0, 4)), (wub, "wub", (1, 5)), (wuc, "wuc", (2, 3, 6, 7)))
    YA = (0, 4)
    for blk in range(NB):
        ts_ = slice(blk * 512, (blk + 1) * 512)
        p.dma("sp", h32[:], hT_d[:, :, ts_], reads=["hT_half"], writes=["h32"])
        p.dma("act", yb16[:], y_d[:, :, ts_], reads=["y_own"], writes=["y16"])
        p.dma("act", hb[:], hTb_d[:, :, ts_], reads=["hT_hbf"], writes=["hb"])
        yg = p.sb([128, 2, 512], BF16, "yg") if blk == 0 else yg
        for m in range(2):
            ps_, psr = nps()
            for kc in range(2):
                p.op("pe", lambda e, ps_=ps_, m=m, kc=kc: e.matmul(ps_[:], lhsT=wglu[:, kc, m * 128:(m + 1) * 128],
                                                                   rhs=yb16[:, YA[kc], :], start=(kc == 0), stop=(kc == 1)),
                     reads=["wglu", "y16"], writes=[psr])
            p.op("act", lambda e, ps_=ps_, m=m: e.activation(out=sgm[:], in_=ps_[:], func=AF.Sigmoid, bias=cols[:, 16 + m:17 + m]),
                 reads=[psr, "lncols"], writes=["sgm"])
            p.op("dve", lambda e, m=m: e.tensor_tensor(out=yg[:, m, :], in0=yb16[:, YA[m], :], in1=sgm[:], op=ALU.mult),
                 reads=["sgm", "y16"], writes=["yg"])
        for f in range(8):
            fs = slice(f * 128, (f + 1) * 128)
            for bi, (wup, wres, kcs) in enumerate(ybranch):
                nkc = len(kcs)
                gp, gpr = nps()
                for kc in range(8):
                    p.op("pe", lambda e, gp=gp, bi=bi, f=f, kc=kc: e.matmul(
                        gp[:], lhsT=wg[:, kc, bi * 1024 + f * 128: bi * 1024 + (f + 1) * 128], rhs=hb[:, kc, :],
                        start=(kc == 0), stop=(kc == 7)), reads=["wg", "hb"], writes=[gpr])
                sk = bi % 2
                p.op("act", lambda e, gp=gp, sk=sk: e.activation(out=sig[sk][:], in_=gp[:], func=AF.Sigmoid),
                     reads=[gpr], writes=[f"sig{sk}"])
                up, upr = nps()
                for kc in range(nkc):
                    rhs = yg[:, kc, :] if bi == 0 else yb16[:, kcs[kc], :]
                    p.op("pe", lambda e, up=up, wup=wup, kc=kc, fs=fs, rhs=rhs, nkc=nkc: e.matmul(
                        up[:], lhsT=wup[:, kc, fs], rhs=rhs, start=(kc == 0), stop=(kc == nkc - 1)),
                        reads=[wres, "yg" if bi == 0 else "y16"], writes=[upr])
                if bi == 0:
                    p.op("dve", lambda e, up=up, sk=sk: e.tensor_tensor(out=macc[:], in0=sig[sk][:], in1=up[:], op=ALU.mult),
                         reads=[f"sig{sk}", upr], writes=["macc"])
                else:
                    p.op("dve", lambda e, up=up, sk=sk: e.tensor_tensor(out=mtmp[:], in0=sig[sk][:], in1=up[:], op=ALU.mult),
                         reads=[f"sig{sk}", upr], writes=["mtmp"])
                    if bi == 1:
                        p.op("pool", lambda e: e.tensor_tensor(out=macc[:], in0=macc[:], in1=mtmp[:], op=ALU.add),
                             reads=["macc", "mtmp"], writes=["macc"])
                    else:
                        p.op("pool", lambda e, f=f: e.tensor_tensor(out=mb[:, f, :], in0=macc[:], in1=mtmp[:], op=ALU.add),
                             reads=["macc", "mtmp"], writes=["mb"])
        for f in range(8):
            fs = slice(f * 128, (f + 1) * 128)
            op_, opr = nps()
            for kc in range(8):
                p.op("pe", lambda e, op_=op_, kc=kc, fs=fs: e.matmul(op_[:], lhsT=wo[:, kc, fs], rhs=mb[:, kc, :],
                                                                     start=(kc == 0), stop=(kc == 7)),
                     reads=["wo", "mb"], writes=[opr])
            p.op("dve", lambda e, op_=op_, f=f: e.scalar_tensor_tensor(out=r[:, f, :], in0=h32[:, f, :], scalar=float(ALPHA),
                                                                       in1=op_[:], op0=ALU.mult, op1=ALU.add),
                 reads=[opr, "h32"], writes=["r"])
        emit_ln_fm(p, r, "r", cols[:, 0:8], cols[:, 8:16], h1, "r", ones32, nps(), nps(), sq, mean, rstd, tmp, "ln1", sqres="y32")
        for sub in range(4):
            ss = slice(sub * 128, (sub + 1) * 128)
            for kc in range(8):
                p.op("pe", lambda e, kc=kc, ss=ss: e.matmul(rps[:, 0:16], lhsT=h1[:, kc, ss], rhs=wr[:, kc, :], start=(kc == 0),
                                                            stop=(kc == 7)), reads=["r", "wr"], writes=["rps"])
            RS = "rt"
            p.op("dve", lambda e: e.tensor_tensor(out=lg[:], in0=rps[:, 0:16], in1=rb[:], op=ALU.add), reads=["rps", "rb"], writes=[RS])
            p.op("dve", lambda e: e.tensor_reduce(out=mx[:, 0:1], in_=lg[:], op=ALU.max, axis=AX.X), reads=[RS], writes=[RS])
            p.op("dve", lambda e: e.tensor_scalar(out=mx[:, 1:2], in0=mx[:, 0:1], scalar1=-1.0, scalar2=None, op0=ALU.mult),
                 reads=[RS], writes=[RS])
            lg4 = lg[:].rearrange("p (g k) -> p g k", g=4)
            p.op("act", lambda e: e.activation(out=exd[:, :, 0:4], in_=lg4, func=AF.Exp, bias=mx[:, 1:2]), reads=[RS], writes=[RS])
            p.op("act", lambda e: e.activation(out=exd[:, :, 4:8], in_=lg4, func=AF.Exp, bias=mx[:, 1:2]), reads=[RS], writes=[RS])
            n = 0
            for i in range(4):
                for j in range(i + 1, 4):
                    p.op("dve", lambda e, i=i, j=j, n=n: e.tensor_tensor(out=pr6[:, :, n:n + 1], in0=exd[:, :, i:i + 1],
                                                                         in1=exd[:, :, j:j + 1], op=ALU.add),
                         reads=[RS], writes=[RS])
                    n += 1
            p.op("dve", lambda e: e.tensor_reduce(out=gs[:, 0:4], in_=pr6[:], op=ALU.max, axis=AX.X), reads=[RS], writes=[RS])
            p.op("dve", lambda e: e.tensor_reduce(out=gs[:, 4:5], in_=gs[:, 0:4], op=ALU.max, axis=AX.X), reads=[RS], writes=[RS])
            p.op("dve", lambda e: e.tensor_scalar(out=gs[:, 0:4], in0=gs[:, 0:4], scalar1=gs[:, 4:5], scalar2=None, op0=ALU.is_ge),
                 reads=[RS], writes=[RS])
            for rr in range(1, 4):
                dst = cnt if rr == 1 else cmp_
                p.op("dve", lambda e, rr=rr, dst=dst: e.tensor_tensor(out=dst[:], in0=exd[:, :, rr:rr + 4], in1=exd[:, :, 0:4],
                                                                      op=ALU.is_gt), reads=[RS], writes=[RS])
                if rr > 1:
                    p.op("dve", lambda e: e.tensor_tensor(out=cnt[:], in0=cnt[:], in1=cmp_[:], op=ALU.add), reads=[RS], writes=[RS])
            p.op("dve", lambda e: e.tensor_scalar(out=cnt[:], in0=cnt[:], scalar1=2.0, scalar2=None, op0=ALU.is_lt),
                 reads=[RS], writes=[RS])
            gsel4 = gsel[:].rearrange("p (g k) -> p g k", g=4)
            for g_ in range(4):
                p.op("dve", lambda e, g_=g_: e.scalar_tensor_tensor(out=gsel4[:, g_, :], in0=cnt[:, g_, :], scalar=gs[:, g_:g_ + 1],
                                                                    in1=exd[:, g_, 0:4], op0=ALU.mult, op1=ALU.mult),
                     reads=[RS], writes=[RS])
            p.op("dve", lambda e: e.tensor_reduce(out=mx[:, 2:3], in_=gsel[:], op=ALU.add, axis=AX.X), reads=[RS], writes=[RS])
            p.op("dve", lambda e: e.reciprocal(out=mx[:, 3:4], in_=mx[:, 2:3]), reads=[RS], writes=[RS])
            gk = sub % 2
            p.op("dve", lambda e, gk=gk: e.tensor_scalar(out=gout[gk][:], in0=gsel[:], scalar1=mx[:, 3:4], scalar2=None,
                                                         op0=ALU.mult), reads=[RS], writes=[f"gout{gk}"])
            t0 = blk * 512 + sub * 128
            tile_i = blk * 4 + sub
            p.dma("act", gate_o[t0:t0 + 128, :], gout[gk][:], reads=[f"gout{gk}"], writes=["gate_d"])
            p.op("dve", lambda e, gk=gk: e.tensor_scalar(out=Mt[:], in0=gout[gk][:], scalar1=0.0, scalar2=None, op0=ALU.is_gt),
                 reads=[f"gout{gk}"], writes=["Mt"])
            p.op("pe", lambda e, ti=tile_i: e.matmul(rps[:, 16:32], lhsT=LT32[:], rhs=Mt[:], start=True, stop=(ti == 0)),
                 reads=["LT32", "Mt"], writes=["rps"])
            if tile_i > 0:
                p.op("pe", lambda e: e.matmul(rps[:, 16:32], lhsT=ones32[:], rhs=Racc[:], start=False, stop=True),
                     reads=["ones32", "Racc"], writes=["rps"])
            p.op("act", lambda e, gk=gk: e.activation(out=rk_sb[gk][:], in_=rps[:, 16:32], func=AF.Copy), reads=["rps"],
                 writes=[f"rk{gk}"])
            p.dma("act", rk_o[t0:t0 + 128, :], rk_sb[gk][:], reads=[f"rk{gk}"], writes=["rk_d"])
            if tile_i == 0:
                p.op("dve", lambda e: e.tensor_copy(out=Racc[:], in_=Mt[:]), reads=["Mt"], writes=["Racc"])
            else:
                p.op("dve", lambda e: e.tensor_tensor(out=Racc[:], in0=Racc[:], in1=Mt[:], op=ALU.add), reads=["Mt", "Racc"],
                     writes=["Racc"])
            for half in range(2):
                tp_, tpr = nps()
                for kq in range(4):
                    kc = half * 4 + kq
                    p.op("pe", lambda e, tp_=tp_, kq=kq, kc=kc, ss=ss: e.transpose(tp_[:, kq * 128:(kq + 1) * 128], h1[:, kc, ss],
                                                                                  id32[:]), reads=["r", "id32"], writes=[tpr])
                p.op("act" if half == 0 else "dve",
                     (lambda e, tp_=tp_, gk=gk, half=half: e.activation(out=h1tok[gk][:, half * 512:(half + 1) * 512], in_=tp_[:],
                                                                        func=AF.Copy)) if half == 0 else
                     (lambda e, tp_=tp_, gk=gk, half=half: e.tensor_copy(out=h1tok[gk][:, half * 512:(half + 1) * 512], in_=tp_[:])),
                     reads=[tpr], writes=[f"h1tok{gk}"])
            p.dma("sp", h1tok_o[t0:t0 + 128, :], h1tok[gk][:], reads=[f"h1tok{gk}"], writes=["h1tok_d"])
    p.dma("sp", racc_o, Racc[:], reads=["Racc"], writes=["racc_d"])
    p.end_phase()


def emit_moe(p, io, T, SBT=1024, n_exp=16):
    SBT = min(SBT, T)
    NS = T // SBT
    NH = SBT // 512
    v3 = lambda ap: ap.rearrange("(kc p) n -> p kc n", p=128)
    h1T_d = v3(io["h1T"])
    gT_d = io["gT"]
    w_d = [io["w1"], io["w3"], io["w2"]]
    cols_d = io["cols2"]
    h2T_o = v3(io["h2T"])
    h2res = io["h2res"]

    cols = p.sb([128, 16], F32, "cols")
    ones32 = p.sb([128, 128], F32, "ones32")
    p.dma("sp", cols[:], cols_d, writes=["lncols"])
    p.op("pool", lambda e: e.memset(ones32[:], 1.0), writes=["ones32"])
    stg = Stager(p, n=2, width=256)
    xb = p.sb([128, 8, SBT], BF16, "xb")
    acc = p.sb([128, 8, SBT], F32, "acc")
    wbuf = [p.sb([128, 8, D_MODEL], BF16, "wbuf") for _ in range(4)]
    G = [p.sb([128, SBT], F32, "G") for _ in range(2)]
    hbg = [p.sb([128, 8, 512], BF16, "hbg") for _ in range(NH)]
    sa = [p.sb([128, 512], F32, "sa") for _ in range(2)]
    tt = [p.sb([128, 512], F32, "tt") for _ in range(2)]
    hres = p.sb([128, 8, 512], F32, "hres")
    sq = p.sb([128, 8, 512], F32, "sq")
    mean = p.sb([128, 512], F32, "mean")
    rstd = p.sb([128, 512], F32, "rstd")
    tmp = p.sb([128, 512], F32, "tmp")
    PS = [p.ps([128, 512], F32, "PS") for _ in range(8)]
    psi = [0]

    def nps():
        k = psi[0] % 8
        psi[0] += 1
        return PS[k], f"PS{k}"

    wi = [0]

    def load_w(m, e):
        k = wi[0] % 4
        wi[0] += 1
        src = w_d[m][e].rearrange("(kc p) n -> p kc n", p=128)
        stg.load(lambda c0, w, k=k: wbuf[k][:, :, c0:c0 + w], src, 8, D_MODEL, f"wbuf{k}")
        return wbuf[k], f"wbuf{k}"

    it = 0
    for S in range(NS):
        t0 = S * SBT
        for hf_ in range(NH):
            cs = slice(hf_ * 512, (hf_ + 1) * 512)
            p.dma("sp", hres[:], h1T_d[:, :, t0 + hf_ * 512: t0 + (hf_ + 1) * 512], reads=["h1T"], writes=["hres"])
            p.op("dve", lambda e, cs=cs: e.tensor_copy(out=xb[:, :, cs], in_=hres[:]), reads=["hres"], writes=[("xb", hf_)])
        for ex in range(n_exp):
            gk = ex % 2
            p.dma("pool", G[gk][:], gT_d[ex, t0:t0 + SBT].partition_broadcast(128), reads=["gT"], writes=[f"G{gk}"])
            W1, W1r = load_w(0, ex)
            W3, W3r = load_w(1, ex)
            W2, W2r = load_w(2, ex)
            for f in range(8):
                fs = slice(f * 128, (f + 1) * 128)
                for hf_ in range(NH):
                    cs = slice(hf_ * 512, (hf_ + 1) * 512)
                    k = it % 2
                    it += 1
                    A, Ar = nps()
                    B, Br = nps()
                    for kc in range(8):
                        p.op("pe", lambda e, A=A, W1=W1, kc=kc, fs=fs, cs=cs: e.matmul(A[:], lhsT=W1[:, kc, fs], rhs=xb[:, kc, cs],
                                                                                       start=(kc == 0), stop=(kc == 7)),
                             reads=[W1r, ("xb", hf_)], writes=[Ar])
                    for kc in range(8):
                        p.op("pe", lambda e, B=B, W3=W3, kc=kc, fs=fs, cs=cs: e.matmul(B[:], lhsT=W3[:, kc, fs], rhs=xb[:, kc, cs],
                                                                                       start=(kc == 0), stop=(kc == 7)),
                             reads=[W3r, ("xb", hf_)], writes=[Br])
                    p.op("act", lambda e, A=A, k=k: e.activation(out=sa[k][:], in_=A[:], func=AF.Silu), reads=[Ar], writes=[f"sa{k}"])
                    p.op("dve", lambda e, B=B, k=k: e.tensor_tensor(out=tt[k][:], in0=sa[k][:], in1=B[:], op=ALU.mult),
                         reads=[f"sa{k}", Br], writes=[f"tt{k}"])
                    p.op("pool", lambda e, k=k, f=f, hf_=hf_, gk=gk, cs=cs: e.tensor_tensor(
                        out=hbg[hf_][:, f, :], in0=tt[k][:], in1=G[gk][:, cs], op=ALU.mult),
                        reads=[f"tt{k}", f"G{gk}"], writes=[("hbg", hf_)])
            for o in range(8):
                os_ = slice(o * 128, (o + 1) * 128)
                for hf_ in range(NH):
                    cs = slice(hf_ * 512, (hf_ + 1) * 512)
                    O, Or = nps()
                    for f in range(8):
                        p.op("pe", lambda e, O=O, W2=W2, f=f, os_=os_, hf_=hf_: e.matmul(O[:], lhsT=W2[:, f, os_], rhs=hbg[hf_][:, f, :],
                                                                                         start=(f == 0), stop=(f == 7)),
                             reads=[W2r, ("hbg", hf_)], writes=[Or])
                    if ex == 0:
                        p.op("dve", lambda e, O=O, o=o, cs=cs: e.tensor_copy(out=acc[:, o, cs], in_=O[:]),
                             reads=[Or], writes=[("acc", hf_)])
                    else:
                        p.op("dve", lambda e, O=O, o=o, cs=cs: e.tensor_tensor(out=acc[:, o, cs], in0=acc[:, o, cs], in1=O[:], op=ALU.add),
                             reads=[Or, ("acc", hf_)], writes=[("acc", hf_)])
        for hf_ in range(NH):
            cs = slice(hf_ * 512, (hf_ + 1) * 512)
            tsl = slice(t0 + hf_ * 512, t0 + (hf_ + 1) * 512)
            p.dma("sp", hres[:], h1T_d[:, :, tsl], reads=["h1T"], writes=["hres"])
            rv = acc[:, :, cs]
            rres = ("acc", hf_)
            p.op("dve", lambda e, rv=rv: e.scalar_tensor_tensor(out=rv, in0=hres[:], scalar=float(ALPHA), in1=rv, op0=ALU.mult,
                                                                op1=ALU.add), reads=["hres", rres], writes=[rres])
            emit_ln_fm(p, rv, rres, cols[:, 0:8], cols[:, 8:16], hres, "hres", ones32, nps(), nps(), sq, mean, rstd, tmp, "ln2")
            p.dma("sp", h2T_o[:, :, tsl], hres[:], reads=["hres"], writes=[h2res])
    p.end_phase()


MOE_B = 512


def emit_moe_sparse(p, io, T, last):
    import concourse.bass as _b
    NT = T // 128
    NBLK = (2 * T + 16 * (MOE_B - 1)) // MOE_B
    NR = NBLK * MOE_B
    h1tok_d, gate_d, rk_d, racc_d, xs, ys = io["h1tok"], io["gate"], io["rk"], io["racc"], io["xs"], io["ys"]
    wr_d = [io[k].rearrange("(r q) n -> r (q n)", q=1) for k in ("w1r", "w3r", "w2r")]

    def ind(eng_fn_kwargs):
        return eng_fn_kwargs

    def idma(out, in_, idx_ap, scatter, reads, writes):
        def fn(e):
            off = _b.IndirectOffsetOnAxis(ap=idx_ap, axis=0)
            if scatter:
                return e.indirect_dma_start(out=out, out_offset=off, in_=in_, in_offset=None)
            return e.indirect_dma_start(out=out, out_offset=None, in_=in_, in_offset=off)
        p.ops.append(dict(eng="pool", fn=fn, reads=tuple(reads), writes=tuple(writes), kind="dma"))

    ones32 = p.sb([128, 128], F32, "ones32")
    id32 = p.sb([128, 128], F32, "id32")
    idb = p.sb([128, 128], BF16, "idb")
    ramp = p.sb([128, 16], F32, "ramp")
    iota = p.sb([128, 1], F32, "iota")
    racc = p.sb([128, 16], F32, "racc")
    p.op("pool", lambda e: e.memset(ones32[:], 1.0), writes=["ones32"])
    p.dma("sp", id32[:], io["ident"], writes=["id32"])
    p.dma("sp", ramp[:], io["ramp"], writes=["ramp"])
    p.dma("sp", iota[:], io["iota"], writes=["iota"])
    p.dma("sp", racc[:], racc_d, reads=["racc_d"], writes=["racc"])
    p.op("dve", lambda e: e.tensor_copy(out=idb[:], in_=id32[:]), reads=["id32"], writes=["idb"])
    PS = [p.ps([128, 512], F32, "PS") for _ in range(4)]
    TR = [p.ps([128, 4, 512], BF16, "TR") for _ in range(2)]
    psi = [0]

    def nps():
        k = psi[0] % 4
        psi[0] += 1
        return PS[k], f"PS{k}"

    xblk = [p.sb([128, 4, D_MODEL], BF16, "xblk") for _ in range(2)]

    cnt = p.sb([128, 16], F32, "cnt")
    cmp16 = p.sb([128, 16], F32, "cmp16")
    padded = p.sb([128, 16], F32, "padded")
    pstart = p.sb([128, 16], F32, "pstart")
    pend = p.sb([128, 16], F32, "pend")
    PR = "prol"
    cps_, cpr = nps()
    p.op("pe", lambda e: e.matmul(cps_[:, 0:16], lhsT=ones32[:], rhs=racc[:], start=True, stop=True), reads=["ones32", "racc"],
         writes=[cpr])
    p.op("act", lambda e: e.activation(out=cnt[:], in_=cps_[:, 0:16], func=AF.Copy), reads=[cpr], writes=[PR])
    for ex in range(16):
        p.op("dve", lambda e, ex=ex: e.tensor_scalar(out=cmp16[:], in0=ramp[:], scalar1=cnt[:, ex:ex + 1], scalar2=None, op0=ALU.is_lt),
             reads=[PR, "ramp"], writes=[PR])
        p.op("dve", lambda e, ex=ex: e.tensor_reduce(out=padded[:, ex:ex + 1], in_=cmp16[:], op=ALU.add, axis=AX.X), reads=[PR],
             writes=[PR])
    p.op("dve", lambda e: e.tensor_scalar(out=padded[:], in0=padded[:], scalar1=float(MOE_B), scalar2=None, op0=ALU.mult),
         reads=[PR], writes=[PR])
    p.op("dve", lambda e: e.memset(pstart[:], 0.0), reads=[PR], writes=[PR])
    for ex in range(1, 16):
        p.op("dve", lambda e, ex=ex: e.tensor_tensor(out=pstart[:, ex:ex + 1], in0=pstart[:, ex - 1:ex], in1=padded[:, ex - 1:ex],
                                                     op=ALU.add), reads=[PR], writes=[PR])
    p.op("dve", lambda e: e.tensor_tensor(out=pend[:], in0=pstart[:], in1=padded[:], op=ALU.add), reads=[PR], writes=[PR])
    esf = p.sb([128, NBLK], F32, "esf")
    widf = p.sb([128, NBLK, 4], F32, "widf")
    widx = p.sb([128, NBLK, 4], I32, "widx")
    for s_ in range(NBLK):
        p.op("dve", lambda e, s_=s_: e.tensor_scalar(out=cmp16[:], in0=pend[:], scalar1=float(MOE_B * s_) + 0.5, scalar2=None,
                                                     op0=ALU.is_lt), reads=[PR], writes=[PR])
        p.op("dve", lambda e, s_=s_: e.tensor_reduce(out=esf[:, s_:s_ + 1], in_=cmp16[:], op=ALU.add, axis=AX.X), reads=[PR],
             writes=[PR])
    p.op("dve", lambda e: e.tensor_scalar(out=esf[:], in0=esf[:], scalar1=15.0, scalar2=128.0, op0=ALU.min, op1=ALU.mult),
         reads=[PR], writes=[PR])
    p.op("dve", lambda e: e.tensor_scalar(out=esf[:], in0=esf[:], scalar1=iota[:, 0:1], scalar2=4.0, op0=ALU.add, op1=ALU.mult),
         reads=[PR, "iota"], writes=[PR])
    for q in range(4):
        p.op("dve", lambda e, q=q: e.tensor_scalar(out=widf[:, :, q], in0=esf[:], scalar1=float(q), scalar2=None, op0=ALU.add),
             reads=[PR], writes=[PR])
    p.op("dve", lambda e: e.tensor_copy(out=widx[:], in_=widf[:]), reads=[PR], writes=["widx"])

    idxf = p.sb([128, NT, 2], F32, "idxf")
    idxi = p.sb([128, NT, 2], I32, "idxi")
    gAB = p.sb([128, NT, 2], F32, "gAB")
    gt_ = [p.sb([128, 16], F32, "gt") for _ in range(2)]
    rkt = [p.sb([128, 16], F32, "rkt") for _ in range(2)]
    d1 = p.sb([128, 16], F32, "d1")
    sm = p.sb([128, 8], F32, "sm")
    h32 = [p.sb([128, D_MODEL], F32, "h32") for _ in range(2)]
    xb = [p.sb([128, D_MODEL], BF16, "xb") for _ in range(2)]
    SC = "scat"
    for tt in range(NT):
        k = tt % 2
        rows = slice(tt * 128, (tt + 1) * 128)
        p.dma("sp", h32[k][:], h1tok_d[rows, :], reads=["h1tok_d"], writes=[f"h32{k}"])
        p.dma("act", gt_[k][:], gate_d[rows, :], reads=["gate_d"], writes=[f"gt{k}"])
        p.dma("act", rkt[k][:], rk_d[rows, :], reads=["rk_d"], writes=[f"rkt{k}"])
        p.op("act", lambda e, k=k: e.activation(out=xb[k][:], in_=h32[k][:], func=AF.Copy), reads=[f"h32{k}"], writes=[f"xb{k}"])
        p.op("dve", lambda e, k=k: e.tensor_tensor(out=d1[:], in0=rkt[k][:], in1=pstart[:], op=ALU.add), reads=[f"rkt{k}", PR],
             writes=[SC])
        p.op("dve", lambda e, k=k: e.tensor_scalar(out=cmp16[:], in0=gt_[k][:], scalar1=0.0, scalar2=None, op0=ALU.is_gt),
             reads=[f"gt{k}", PR], writes=[PR])
        p.op("dve", lambda e: e.scalar_tensor_tensor(out=d1[:], in0=d1[:], scalar=1.0, in1=cmp16[:], op0=ALU.add, op1=ALU.mult),
             reads=[SC, PR], writes=[SC])
        p.op("dve", lambda e: e.tensor_reduce(out=sm[:, 0:1], in_=d1[:], op=ALU.max, axis=AX.X), reads=[SC], writes=[SC])
        p.op("dve", lambda e: e.tensor_reduce(out=sm[:, 1:2], in_=d1[:], op=ALU.add, axis=AX.X), reads=[SC], writes=[SC])
        p.op("dve", lambda e, tt=tt: e.tensor_scalar(out=idxf[:, tt, 0:1], in0=sm[:, 0:1], scalar1=-1.0, scalar2=None, op0=ALU.add),
             reads=[SC], writes=["idxf"])
        p.op("dve", lambda e, tt=tt: e.scalar_tensor_tensor(out=idxf[:, tt, 1:2], in0=sm[:, 1:2], scalar=-1.0, in1=sm[:, 0:1],
                                                            op0=ALU.add, op1=ALU.subtract), reads=[SC], writes=["idxf"])
        p.op("dve", lambda e, tt=tt: e.tensor_scalar(out=idxf[:, tt, :], in0=idxf[:, tt, :], scalar1=0.0, scalar2=float(NR - 1),
                                                     op0=ALU.max, op1=ALU.min), reads=["idxf"], writes=["idxf"])
        p.op("dve", lambda e, tt=tt: e.tensor_copy(out=idxi[:, tt, :], in_=idxf[:, tt, :]), reads=["idxf"], writes=[("idxi", tt)])
        p.op("dve", lambda e: e.tensor_scalar(out=cmp16[:], in0=d1[:], scalar1=sm[:, 0:1], scalar2=None, op0=ALU.is_equal),
             reads=[SC, PR], writes=[PR])
        p.op("dve", lambda e, k=k: e.tensor_tensor(out=cmp16[:], in0=cmp16[:], in1=gt_[k][:], op=ALU.mult), reads=[PR, f"gt{k}"],
             writes=[PR])
        p.op("dve", lambda e, tt=tt: e.tensor_reduce(out=gAB[:, tt, 0:1], in_=cmp16[:], op=ALU.add, axis=AX.X), reads=[PR],
             writes=["gAB"])
        p.op("dve", lambda e, k=k: e.tensor_reduce(out=sm[:, 2:3], in_=gt_[k][:], op=ALU.add, axis=AX.X), reads=[f"gt{k}", SC],
             writes=[SC])
        p.op("dve", lambda e, tt=tt: e.tensor_tensor(out=gAB[:, tt, 1:2], in0=sm[:, 2:3], in1=gAB[:, tt, 0:1], op=ALU.subtract),
             reads=[SC, "gAB"], writes=["gAB"])
        for kk in range(2):
            idma(xs, xb[k][:, :], idxi[:, tt, kk:kk + 1], True, [f"xb{k}", ("idxi", tt), "xs"], ["xs"])

    Wm = [[p.sb([128, 8, D_MODEL], BF16, "Wm") for _ in range(3)] for _ in range(2)]
    xT = [p.sb([128, 8, 512], BF16, "xT") for _ in range(2)]
    hbT = p.sb([128, 8, 512], BF16, "hbT")
    sa = [p.sb([128, 512], F32, "sa") for _ in range(2)]
    ysb = [p.sb([128, D_MODEL], F32, "ysb") for _ in range(2)]
    it = 0
    io_i = 0
    for s_ in range(NBLK):
        wb_ = s_ % 2
        for m in range(3):
            for q in range(4):
                idma(Wm[wb_][m][:].rearrange("p (q a) n -> p q (a n)", q=4)[:, q, :], wr_d[m], widx[:, s_, q:q + 1], False,
                     ["widx"], [f"W{wb_}{m}"])
        p.dma("sp", xblk[wb_][:], xs[s_ * MOE_B:(s_ + 1) * MOE_B, :].rearrange("(j p) n -> p j n", p=128), reads=["xs"],
              writes=[f"xblk{wb_}"])
        for half in range(2):
            for kq in range(4):
                kc = half * 4 + kq
                for j in range(4):
                    p.op("pe", lambda e, half=half, kq=kq, kc=kc, j=j, wb_=wb_: e.transpose(
                        TR[half][:, kq, j * 128:(j + 1) * 128], xblk[wb_][:, j, kc * 128:(kc + 1) * 128], idb[:]),
                        reads=[f"xblk{wb_}", "idb"], writes=[f"TR{half}"])
            p.op("act" if half == 0 else "dve",
                 (lambda e, half=half, wb_=wb_: e.activation(out=xT[wb_][:, half * 4:(half + 1) * 4, :], in_=TR[half][:], func=AF.Copy))
                 if half == 0 else
                 (lambda e, half=half, wb_=wb_: e.tensor_copy(out=xT[wb_][:, half * 4:(half + 1) * 4, :], in_=TR[half][:])),
                 reads=[f"TR{half}"], writes=[f"xT{wb_}"])
        W1, W3, W2 = Wm[wb_]
        for f in range(8):
            fs = slice(f * 128, (f + 1) * 128)
            k = it % 2
            it += 1
            A, Ar = nps()
            B, Br = nps()
            for kc in range(8):
                p.op("pe", lambda e, A=A, W1=W1, kc=kc, fs=fs, wb_=wb_: e.matmul(A[:], lhsT=W1[:, kc, fs], rhs=xT[wb_][:, kc, :],
                                                                               start=(kc == 0), stop=(kc == 7)),
                     reads=[f"W{wb_}0", f"xT{wb_}"], writes=[Ar])
            for kc in range(8):
                p.op("pe", lambda e, B=B, W3=W3, kc=kc, fs=fs, wb_=wb_: e.matmul(B[:], lhsT=W3[:, kc, fs], rhs=xT[wb_][:, kc, :],
                                                                               start=(kc == 0), stop=(kc == 7)),
                     reads=[f"W{wb_}1", f"xT{wb_}"], writes=[Br])
            p.op("act", lambda e, A=A, k=k: e.activation(out=sa[k][:], in_=A[:], func=AF.Silu), reads=[Ar], writes=[f"sa{k}"])
            p.op("dve", lambda e, B=B, k=k, f=f: e.tensor_tensor(out=hbT[:, f, :], in0=sa[k][:], in1=B[:], op=ALU.mult),
                 reads=[f"sa{k}", Br], writes=["hbT"])
        for j in range(4):
            yk = io_i % 2
            io_i += 1
            for c in range(2):
                O, Or = nps()
                for f in range(8):
                    p.op("pe", lambda e, O=O, W2=W2, f=f, j=j, c=c: e.matmul(O[:], lhsT=hbT[:, f, j * 128:(j + 1) * 128],
                                                                             rhs=W2[:, f, c * 512:(c + 1) * 512], start=(f == 0),
                                                                             stop=(f == 7)),
                         reads=[f"W{wb_}2", "hbT"], writes=[Or])
                if c == 0:
                    p.op("act", lambda e, O=O, yk=yk: e.activation(out=ysb[yk][:, 0:512], in_=O[:], func=AF.Copy), reads=[Or],
                         writes=[f"ysb{yk}"])
                else:
                    p.op("dve", lambda e, O=O, yk=yk: e.tensor_copy(out=ysb[yk][:, 512:1024], in_=O[:]), reads=[Or],
                         writes=[f"ysb{yk}"])
            r0 = s_ * MOE_B + j * 128
            p.dma("act", ys[r0:r0 + 128, :], ysb[yk][:], reads=[f"ysb{yk}"], writes=["ys"])

    rA = [p.sb([128, D_MODEL], F32, "rA") for _ in range(2)]
    rB = [p.sb([128, D_MODEL], F32, "rB") for _ in range(2)]
    junk = ysb[0]
    stt = p.sb([128, 8], F32, "stt")
    ho = [p.sb([128, D_MODEL], F32, "ho") for _ in range(2)]
    hTt = [p.sb([128, 8, 128], F32, "hTt") for _ in range(2)]
    hT_o = (io["out_fm"] if last else io["hT_half"]).rearrange("(kc p) t -> p kc t", p=128)
    hres_name = "out" if last else "hT_half"
    hTb_o = io["hT_hbf"].rearrange("(kc p) t -> p kc t", p=128)
    cols2 = p.sb([128, 16], F32, "cols2")
    p.dma("sp", cols2[:], io["cols2"], writes=["cols2"])
    for tt in range(NT):
        k = tt % 2
        rows = slice(tt * 128, (tt + 1) * 128)
        idma(rA[k][:, :], ys, idxi[:, tt, 0:1], False, [("idxi", tt), "ys"], [f"rA{k}"])
        idma(rB[k][:, :], ys, idxi[:, tt, 1:2], False, [("idxi", tt), "ys"], [f"rB{k}"])
        p.dma("sp", h32[k][:], h1tok_d[rows, :], reads=["h1tok_d"], writes=[f"h32{k}"])
        p.op("dve", lambda e, k=k, tt=tt: e.tensor_scalar(out=rA[k][:], in0=rA[k][:], scalar1=gAB[:, tt, 0:1], scalar2=None,
                                                          op0=ALU.mult), reads=[f"rA{k}", "gAB"], writes=[f"rA{k}"])
        p.op("dve", lambda e, k=k, tt=tt: e.scalar_tensor_tensor(out=rA[k][:], in0=rB[k][:], scalar=gAB[:, tt, 1:2], in1=rA[k][:],
                                                                 op0=ALU.mult, op1=ALU.add), reads=[f"rA{k}", f"rB{k}", "gAB"],
             writes=[f"rA{k}"])
        p.op("dve", lambda e, k=k: e.scalar_tensor_tensor(out=rA[k][:], in0=h32[k][:], scalar=float(ALPHA), in1=rA[k][:],
                                                          op0=ALU.mult, op1=ALU.add), reads=[f"rA{k}", f"h32{k}"], writes=[f"rA{k}"])
        LS = "ln2s"
        p.op("act", lambda e, k=k: e.activation(out=junk[:], in_=rA[k][:], func=AF.Identity, accum_out=stt[:, 0:1]),
             reads=[f"rA{k}"], writes=["ysb0", LS])
        p.op("act", lambda e, k=k: e.activation(out=junk[:], in_=rA[k][:], func=AF.Square, accum_out=stt[:, 1:2]),
             reads=[f"rA{k}"], writes=["ysb0", LS])
        p.op("dve", lambda e: e.tensor_scalar(out=stt[:, 2:3], in0=stt[:, 0:1], scalar1=1.0 / D_MODEL, scalar2=None, op0=ALU.mult),
             reads=[LS], writes=[LS])
        p.op("dve", lambda e: e.tensor_tensor(out=stt[:, 3:4], in0=stt[:, 2:3], in1=stt[:, 2:3], op=ALU.mult), reads=[LS], writes=[LS])
        p.op("dve", lambda e: e.scalar_tensor_tensor(out=stt[:, 4:5], in0=stt[:, 1:2], scalar=1.0 / D_MODEL, in1=stt[:, 3:4],
                                                     op0=ALU.mult, op1=ALU.subtract), reads=[LS], writes=[LS])
        p.op("act", lambda e: e.activation(out=stt[:, 6:7], in_=stt[:, 4:5], func=AF.Sqrt, bias=LN_EPS), reads=[LS], writes=[LS])
        p.op("dve", lambda e: e.reciprocal(out=stt[:, 5:6], in_=stt[:, 6:7]), reads=[LS], writes=[LS])
        p.op("dve", lambda e, k=k: e.tensor_scalar(out=ho[k][:], in0=rA[k][:], scalar1=stt[:, 2:3], scalar2=stt[:, 5:6],
                                                   op0=ALU.subtract, op1=ALU.mult), reads=[f"rA{k}", LS], writes=[f"ho{k}"])
        for half in range(2):
            tp_, tpr = nps()
            for kq in range(4):
                kc = half * 4 + kq
                p.op("pe", lambda e, tp_=tp_, kq=kq, kc=kc, k=k: e.transpose(tp_[:, kq * 128:(kq + 1) * 128],
                                                                             ho[k][:, kc * 128:(kc + 1) * 128], id32[:]),
                     reads=[f"ho{k}", "id32"], writes=[tpr])
            for kq in range(4):
                kc = half * 4 + kq
                p.op("act", lambda e, tp_=tp_, k=k, kq=kq, kc=kc: e.activation(
                    out=hTt[k][:, kc, :], in_=tp_[:, kq * 128:(kq + 1) * 128], func=AF.Identity, scale=cols2[:, kc:kc + 1],
                    bias=cols2[:, 8 + kc:9 + kc]), reads=[tpr, "cols2"], writes=[f"hTt{k}"])
        p.dma("sp", hT_o[:, :, rows], hTt[k][:], reads=[f"hTt{k}"], writes=[hres_name])
        if not last:
            p.dma("pool", hTb_o[:, :, rows], hTt[k][:], reads=[f"hTt{k}"], writes=["hT_hbf"])
    p.end_phase()


def emit_ln0(p, io, T):
    v3 = lambda ap: ap.rearrange("(kc p) n -> p kc n", p=128)
    x_d = v3(io["xT"])
    h_o = v3(io["hT_half"])
    hb_o = v3(io["hT_hbf"])
    cols = p.sb([128, 16], F32, "cols")
    ones32 = p.sb([128, 128], F32, "ones32")
    p.dma("sp", cols[:], io["cols0"], writes=["lncols"])
    p.op("pool", lambda e: e.memset(ones32[:], 1.0), writes=["ones32"])
    r = [p.sb([128, 8, 512], F32, "r") for _ in range(2)]
    sq = p.sb([128, 8, 512], F32, "sq")
    mean = p.sb([128, 512], F32, "mean")
    rstd = p.sb([128, 512], F32, "rstd")
    tmp = p.sb([128, 512], F32, "tmp")
    PS = [p.ps([128, 512], F32, "PS") for _ in range(4)]
    for blk in range(T // 512):
        k = blk % 2
        ts_ = slice(blk * 512, (blk + 1) * 512)
        p.dma("sp", r[k][:], x_d[:, :, ts_], writes=[f"r{k}"])
        emit_ln_fm(p, r[k], f"r{k}", cols[:, 0:8], cols[:, 8:16], r[k], f"r{k}", ones32, (PS[2 * k], f"PS{2 * k}"),
                   (PS[2 * k + 1], f"PS{2 * k + 1}"), sq, mean, rstd, tmp, "ln0")
        p.dma("act", h_o[:, :, ts_], r[k][:], reads=[f"r{k}"], writes=["hT_half"])
        p.dma("pool", hb_o[:, :, ts_], r[k][:], reads=[f"r{k}"], writes=["hT_hbf"])
    p.end_phase()


PAIRS = [[0, 1], [2, 3], [4, 5], [6, 7]]
LAYER_IN = {"wq": [D_MODEL, 128], "wk": [D_MODEL, 128], "wv": [D_MODEL, 128], "wqk": [D_MODEL, 256], "wvg": [D_MODEL, 512],
            "gn": [128, 256], "wu": [D_MODEL, 128], "lam_rep": [128, 3, 512], "lam_col": [128, 3, 4], "bp": [128, 2, 4, 128],
            "cp": [128, 2, 4, 128], "dcol": [128, 1], "wg": [D_MODEL, 3072], "wglu": [256, 256], "wua": [256, D_MODEL],
            "wub": [256, D_MODEL], "wuc": [512, D_MODEL], "wo": [D_MODEL, D_MODEL], "cols1": [128, 18], "cols2": [128, 16],
            "w1r": [16 * 128 * 4, 2048], "w3r": [16 * 128 * 4, 2048], "w2r": [16 * 128 * 4, 2048],
            "ln2g_bc": [128, D_MODEL], "ln2b_bc": [128, D_MODEL]}


def build_fused(L, depth=DEPTH):
    Tc = L // 2
    NT = L // 128
    p = Prog()
    shared = {"xT": [D_MODEL, Tc], "cols0": [128, 16], "mk": [128, 2], "ident": [128, 128], "tri": [128, 128],
              "masks": [128, 4, 512], "cos": [128, NT, 4, 32], "sin": [128, NT, 4, 32], "dmask": [128, 2, 128], "dec": [128, 6],
              "rb": [128, 16], "wr": [D_MODEL, 16], "lt": [128, 128], "ramp": [128, 16], "iota": [128, 1]}
    g = {k: p.dram_in(k, shp).ap() for k, shp in shared.items()}
    lay = [{k: p.dram_in(f"{k}_{l}", shp).ap() for k, shp in LAYER_IN.items()} for l in range(depth)]
    out = p.dram_out("out", [D_MODEL, Tc]).ap()
    NR = (2 * Tc // MOE_B + 16) * MOE_B
    h1tok = p.dram_tmp("h1tok", [Tc, D_MODEL]).ap()
    gate_t = p.dram_tmp("gate_t", [Tc, 16]).ap()
    rk_t = p.dram_tmp("rk_t", [Tc, 16]).ap()
    racc_t = p.dram_tmp("racc_t", [128, 16]).ap()
    xs_t = p.dram_tmp("xs_t", [NR, D_MODEL], BF16).ap()
    ys_t = p.dram_tmp("ys_t", [NR, D_MODEL]).ap()
    hT_half = p.dram_tmp("hT_half", [D_MODEL, Tc]).ap()
    hT_full = p.dram_tmp("hT_full", [2 * D_MODEL, Tc], BF16).ap()
    hT_hbf = p.dram_tmp("hT_hbf", [D_MODEL, Tc], BF16).ap()
    yin = p.dram_tmp("yin", [2048, Tc], BF16).ap()
    y_own = p.dram_tmp("y_own", [D_MODEL, Tc], BF16).ap()
    yin4 = yin.rearrange("(a b r) t -> a b r t", a=2, b=2)
    nbh = Tc // 512

    hT_full_v = hT_full.rearrange("(kc r p) t -> r p kc t", kc=8, r=2, p=128)

    def hblk(blk):
        rk, lo = blk // nbh, blk % nbh
        return hT_full_v[rk][:, :, lo * 512:(lo + 1) * 512]

    def gather_h():
        for kc in range(8):
            p.coll("AllGather", hT_hbf[kc * 128:(kc + 1) * 128, :], hT_full[kc * 256:(kc + 1) * 256, :], PAIRS,
                   reads=["hT_hbf"], writes=["hT_full"])

    def yw_fn():
        mkt = p.sb([128, 2], F32, "mk")
        p.dma("sp", mkt[:], g["mk"], writes=["mk"])
        return YWriter(p, yin4, mkt, "mk", Tc)

    emit_ln0(p, {"xT": g["xT"], "cols0": g["cols0"], "hT_half": hT_half, "hT_hbf": hT_hbf}, Tc)
    gather_h()
    for l in range(depth):
        io = dict(g)
        io.update(lay[l])
        io.update({"hblk": hblk, "hT_half": hT_half, "hT_hbf": hT_hbf, "y_own": y_own, "h1tok": h1tok, "gate": gate_t, "rk": rk_t,
                   "racc": racc_t, "xs": xs_t, "ys": ys_t, "out_fm": out})
        emit_sb(p, io, L, yw_fn)
        emit_ret(p, io, L, yw_fn)
        emit_s5(p, io, L, yw_fn)
        p.coll("ReduceScatter", yin, y_own, PAIRS, reads=["yin"], writes=["y_own"])
        emit_post(p, io, Tc)
        last = l == depth - 1
        emit_moe_sparse(p, io, Tc, last)
        if not last:
            gather_h()
    return p.build()


def _c(a):
    return np.ascontiguousarray(a, dtype=np.float32)


def _wrows(w):
    return _c(w.reshape(16, 8, 128, 1024).transpose(0, 2, 1, 3).reshape(16 * 128 * 4, 2048))


def fused_in_maps(inp, L, depth=DEPTH):
    Tc = L // 2
    tri, masks = sb_consts()
    retc = [ret_consts(L, hf) for hf in range(2)]
    rb = _c(np.broadcast_to(inp["router_b"][None, :], (128, 16)))
    wr = _c(inp["router_w"])
    cols0 = np.zeros((128, 16), np.float32)
    cols0[:, 0:8] = inp["ln0_g"].reshape(8, 128).T
    cols0[:, 8:16] = inp["ln0_b"].reshape(8, 128).T
    per_layer_common = []
    for l in range(depth):
        cols1 = np.zeros((128, 18), np.float32)
        cols1[:, 0:8] = inp["ln1_g"][l].reshape(8, 128).T
        cols1[:, 8:16] = inp["ln1_b"][l].reshape(8, 128).T
        cols1[:, 16:18] = inp["s5_b_glu"][l].reshape(2, 128).T
        cols2 = np.zeros((128, 16), np.float32)
        cols2[:, 0:8] = inp["ln2_g"][l].reshape(8, 128).T
        cols2[:, 8:16] = inp["ln2_b"][l].reshape(8, 128).T
        per_layer_common.append({"wg": _c(inp["w_in"][l][:, 2560:]), "wglu": _c(inp["s5_w_glu"][l]), "wua": _c(inp["w_up_a"][l]),
                                 "wub": _c(inp["w_up_b"][l]), "wuc": _c(inp["w_up_c"][l]), "wo": _c(inp["w_out"][l]),
                                 "cols1": cols1, "cols2": cols2,
                                 "ln2g_bc": _c(np.broadcast_to(inp["ln2_g"][l][None, :], (128, D_MODEL))),
                                 "ln2b_bc": _c(np.broadcast_to(inp["ln2_b"][l][None, :], (128, D_MODEL))),
                                 "w1r": _wrows(inp["moe_w1"][l]), "w3r": _wrows(inp["moe_w3"][l]), "w2r": _wrows(inp["moe_w2"][l])})
    maps = []
    for c in range(N_CORES):
        bi, hf = c // 2, c % 2
        cos, sin, dmask, dec, ident = retc[hf]
        mk = np.zeros((128, 2), np.float32)
        mk[:, hf] = 1.0
        m = {"xT": _c(inp["x"][bi, hf * Tc:(hf + 1) * Tc, :].T), "cols0": cols0, "mk": mk, "ident": ident, "tri": tri,
             "masks": masks, "cos": cos, "sin": sin, "dmask": dmask, "dec": dec, "rb": rb, "wr": wr,
             "lt": _c(np.arange(128)[:, None] < np.arange(128)[None, :]), "ramp": _c(np.broadcast_to(MOE_B * np.arange(16.0)[None, :],
                                                                                                    (128, 16))),
             "iota": _c(np.arange(128.0).reshape(128, 1))}
        for l in range(depth):
            w_in = inp["w_in"][l]
            q0 = 256 + hf * 128
            qc0 = 1024 + hf * 128
            vc0 = 1536 + hf * 256
            rep, col, bp, cp, dcol = s5_host_layout(hf, inp["s5_lambda_re"][l], inp["s5_lambda_im"][l], inp["s5_log_dt"][l],
                                                    inp["s5_b_re"][l], inp["s5_b_im"][l], inp["s5_c_re"][l], inp["s5_c_im"][l],
                                                    inp["s5_d"][l])
            d = {"wq": _c(w_in[:, q0:q0 + 128]), "wk": _c(w_in[:, q0 + 256:q0 + 384]), "wv": _c(w_in[:, q0 + 512:q0 + 640]),
                 "wqk": _c(np.concatenate([w_in[:, qc0:qc0 + 128], w_in[:, qc0 + 256:qc0 + 384]], axis=1)),
                 "wvg": _c(np.concatenate([w_in[:, vc0:vc0 + 256], w_in[:, vc0 + 512:vc0 + 768]], axis=1)),
                 "gn": _c(np.broadcast_to(inp["ret_gn_g"][l][hf * 256:(hf + 1) * 256][None, :], (128, 256))),
                 "wu": _c(w_in[:, hf * 128:(hf + 1) * 128]), "lam_rep": rep, "lam_col": col, "bp": bp, "cp": cp, "dcol": dcol}
            d.update(per_layer_common[l])
            for k, v in d.items():
                m[f"{k}_{l}"] = v
        maps.append(m)
    return maps


def kernel(**inputs):
    inp = {k: np.asarray(v) for k, v in inputs.items()}
    L = inp["x"].shape[1]
    Tc = L // 2
    nc = build_fused(L)
    res = run_spmd(nc, fused_in_maps(inp, L))
    out = np.zeros((BATCH, L, D_MODEL), np.float32)
    for c in range(N_CORES):
        bi, hf = c // 2, c % 2
        out[bi, hf * Tc:(hf + 1) * Tc, :] = res[c]["out"].T
    return out
```

```python
import math
from contextlib import ExitStack

import numpy as np
import concourse.bass as bass
import concourse.mybir as mybir
from concourse.bass_utils import run_bass_kernel_spmd

F32 = mybir.dt.float32
BF16 = mybir.dt.bfloat16
I32 = mybir.dt.int32
AF = mybir.ActivationFunctionType
ALU = mybir.AluOpType
AX = mybir.AxisListType

N_CORES = 8
D_MODEL = 1024
BATCH = 4
SEQ = 8192
DEPTH = 2
ALPHA = (2 * DEPTH) ** 0.25
LN_EPS = 1e-5


class Prog:
    COMPUTE = ("pe", "act", "dve", "pool")
    QUEUES = ("sp", "act", "pool")
    ENGS = ("pe", "act", "dve", "pool", "sp")
    RING = 12

    def __init__(self, same_engine_sync=True):
        self.nc = bass.Bass("TRN2", target_bir_lowering=False)
        self.ges = ExitStack()
        self.es = ExitStack()
        self.ops = []
        self.same_engine_sync = same_engine_sync
        self._n = 0
        nc = self.nc
        self.semobj = {}
        for e in self.COMPUTE:
            self.semobj[("c", e)] = self.ges.enter_context(nc.semaphore(f"c_{e}"))
        for q in self.QUEUES:
            for i in range(self.RING):
                self.semobj[("r", q, i)] = self.ges.enter_context(nc.semaphore(f"r_{q}{i}"))
        self.semobj[("cc",)] = self.ges.enter_context(nc.semaphore("cc"))
        self.cnt = {e: 0 for e in self.COMPUTE}
        self.ccv = 0
        self.ring_pos = {q: 0 for q in self.QUEUES}
        self.ring_val = {q: [0] * self.RING for q in self.QUEUES}
        self.known = {e: {} for e in self.ENGS}
        self.last_w = {}
        self.readers = {}
        self.pending_barrier = {e: [] for e in self.ENGS}
        self.n_emitted = 0
        self.real = {e: 0 for e in self.COMPUTE}
        self.tokmap = {}

    def _name(self, base):
        self._n += 1
        return f"{base}_{self._n}"

    def sb(self, shape, dt, name="sb"):
        return self.es.enter_context(self.nc.sbuf_tensor(self._name(name), list(shape), dt))

    def ps(self, shape, dt=F32, name="ps"):
        return self.es.enter_context(self.nc.psum_tensor(self._name(name), list(shape), dt))

    def dram_in(self, name, shape, dt=F32):
        return self.nc.dram_tensor(name, list(shape), dt, kind="ExternalInput")

    def dram_out(self, name, shape, dt=F32):
        return self.nc.dram_tensor(name, list(shape), dt, kind="ExternalOutput")

    def dram_tmp(self, name, shape, dt=F32):
        return self.nc.dram_tensor(name, list(shape), dt, kind="Internal")

    def op(self, eng, fn, reads=(), writes=()):
        self.ops.append(dict(eng=eng, fn=fn, reads=tuple(reads), writes=tuple(writes), kind="c"))

    def dma(self, queue, out, in_, reads=(), writes=(), **kw):
        def fn(e, out=out, in_=in_, kw=kw):
            return e.dma_start(out=out, in_=in_, **kw)
        self.ops.append(dict(eng=queue, fn=fn, reads=tuple(reads), writes=tuple(writes), kind="dma"))

    def coll(self, kind, in_ap, out_ap, groups, reads=(), writes=()):
        def fn(e):
            return e.collective_compute(kind, ALU.add if kind in ("AllReduce", "ReduceScatter") else ALU.bypass, replica_groups=groups, ins=[in_ap], outs=[out_ap])
        self.ops.append(dict(eng="pool", fn=fn, reads=tuple(reads), writes=tuple(writes), kind="cc"))

    def end_phase(self, final=False):
        nc = self.nc
        streams = {e: [] for e in self.ENGS}
        for o in self.ops:
            e = o["eng"]
            need = {}

            def req(tok):
                if tok is None:
                    return
                k, v = tok
                if need.get(k, 0) < v:
                    need[k] = v

            for r in o["reads"]:
                req(self.last_w.get(r))
            for w in o["writes"]:
                req(self.last_w.get(w))
                for t in self.readers.get(w, ()):
                    req(t)
            for t in self.pending_barrier[e]:
                req(t)
            self.pending_barrier[e] = []
            if o["kind"] == "dma":
                i = self.ring_pos[e] % self.RING
                self.ring_pos[e] += 1
                key = ("r", e, i)
                if self.ring_val[e][i] > 0:
                    req((key, self.ring_val[e][i]))
                self.ring_val[e][i] += 16
                tok = (key, self.ring_val[e][i])
                inc = (key, 16)
            elif o["kind"] == "cc":
                if self.ccv > 0:
                    req((("cc",), self.ccv))
                self.ccv += CC_INC
                tok = (("cc",), self.ccv)
                inc = (("cc",), CC_INC)
            else:
                self.cnt[e] += 1
                tok = (("c", e), self.cnt[e])
                inc = (("c", e), 1)
            waits = []
            for k, v in need.items():
                if k == ("c", e) and o["kind"] == "c":
                    if e == "pe" or not self.same_engine_sync:
                        continue
                if self.known[e].get(k, 0) >= v:
                    continue
                self.known[e][k] = v
                waits.append((k, v))
            streams[e].append((waits, o["fn"], inc, tok))
            for r in o["reads"]:
                self.readers.setdefault(r, []).append(tok)
            for w in o["writes"]:
                self.last_w[w] = tok
                self.readers[w] = []
        self.n_emitted += len(self.ops)
        self.ops = []
        all_done = []
        for q in self.QUEUES:
            for i in range(self.RING):
                if self.ring_val[q][i] > 0:
                    all_done.append((("r", q, i), self.ring_val[q][i]))
        for e in self.COMPUTE:
            if self.cnt[e] > 0:
                all_done.append((("c", e), self.cnt[e]))
        if self.ccv > 0:
            all_done.append((("cc",), self.ccv))
        for e in self.ENGS:
            self.pending_barrier[e] = list(all_done)
        targets = set(all_done)
        for e in self.ENGS:
            for waits, _, _, _ in streams[e]:
                targets.update(waits)
        for e in self.COMPUTE:
            for waits, fn, inc, tok in streams[e]:
                if inc[0] == ("c", e) and tok in targets:
                    self.real[e] += 1
                    self.tokmap[tok] = self.real[e]
        semobj = self.semobj
        tokmap = self.tokmap

        def real(k, v):
            return tokmap[(k, v)] if k[0] == "c" else v

        def emit(engobj, stream, extra_waits=()):
            for waits, fn, inc, tok in stream:
                for k, v in waits:
                    engobj.wait_ge(semobj[k], real(k, v))
                ins = fn(engobj)
                if inc[0][0] != "c" or tok in tokmap:
                    ins.then_inc(semobj[inc[0]], inc[1])
            for k, v in extra_waits:
                engobj.wait_ge(semobj[k], real(k, v))

        with nc.Block() as block:
            @block.tensor
            def _(eng):
                emit(eng, streams["pe"])

            @block.scalar
            def _(eng):
                emit(eng, streams["act"])

            @block.vector
            def _(eng):
                emit(eng, streams["dve"])

            @block.gpsimd
            def _(eng):
                emit(eng, streams["pool"])

            @block.sync
            def _(eng):
                emit(eng, streams["sp"], all_done if final else ())

        self.es.close()
        self.es = ExitStack()

    def build(self):
        self.end_phase(final=True)
        self.ges.close()
        return self.nc


CC_INC = 1


def run_spmd(nc, in_maps):
    res = run_bass_kernel_spmd(nc, in_maps, core_ids=list(range(N_CORES)))
    return res.results


def load_weight_bf16(p, w_dram_ap, ncols, tag, queue="sp"):
    w32 = p.sb([128, 8, ncols], F32, tag + "32")
    wb = p.sb([128, 8, ncols], BF16, tag + "b")
    p.dma(queue, w32[:], w_dram_ap.rearrange("(kc p) n -> p kc n", p=128), writes=[tag + "32"])
    p.op("pool", lambda e: e.tensor_copy(out=wb[:], in_=w32[:]), reads=[tag + "32"], writes=[tag])
    return wb


class HStream:
    def __init__(self, p, blk_ap, nbuf=2, src_res="hT_full"):
        self.p = p
        self.blk_ap = blk_ap
        self.nbuf = nbuf
        self.src_res = src_res
        self.hb = [p.sb([128, 8, 512], BF16, "hb") for _ in range(nbuf)]
        self.i = 0

    def load(self, blk, cast_eng="dve"):
        k = self.i % self.nbuf
        self.i += 1
        self.p.dma("sp", self.hb[k][:], self.blk_ap(blk), reads=[self.src_res], writes=[f"hb_{k}"])
        return self.hb[k], f"hb_{k}"


class YWriter:
    def __init__(self, p, yin, mk, mk_res, Tc):
        self.p, self.yin, self.mk, self.mk_res, self.Tc = p, yin, mk, mk_res, Tc
        self.tmp = [p.sb([128, 512], BF16, "ywt") for _ in range(4)]
        self.i = 0

    def write(self, row0, psl, src, src_res, blk, engs=("pool", "pool")):
        p = self.p
        nb_half = self.Tc // 512
        th, tl = blk // nb_half, blk % nb_half
        n = psl.stop - psl.start
        for rb in range(2):
            k = self.i % 4
            self.i += 1
            tmp = self.tmp[k]
            p.op(engs[rb], lambda e, tmp=tmp, rb=rb, src=src, psl=psl: e.tensor_scalar(
                out=tmp[psl, :], in0=src, scalar1=self.mk[psl, rb:rb + 1], scalar2=None, op0=ALU.mult),
                reads=[src_res, self.mk_res], writes=[f"ywt{k}"])
            p.dma("act" if rb == 0 else "pool", self.yin[th, rb, row0:row0 + n, tl * 512:(tl + 1) * 512], tmp[psl, :],
                  reads=[f"ywt{k}"], writes=["yin"])


def emit_sb(p, io, L, yw_fn):
    NB = L // 512
    NT = L // 128
    wq_d, wk_d, wv_d, tri_d, msk_d = io["wq"], io["wk"], io["wv"], io["tri"], io["masks"]
    yw = yw_fn()

    wq = load_weight_bf16(p, wq_d, 128, "wq")
    wk = load_weight_bf16(p, wk_d, 128, "wk")
    wv = load_weight_bf16(p, wv_d, 128, "wv")
    tri32 = p.sb([128, 128], F32, "tri32")
    tri = p.sb([128, 128], BF16, "tri")
    ones = p.sb([128, 128], BF16, "ones")
    masks = p.sb([128, 4, 512], F32, "masks")
    p.dma("act", tri32[:], tri_d, writes=["tri32"])
    p.dma("act", masks[:], msk_d, writes=["masks"])
    p.op("pool", lambda e: e.tensor_copy(out=tri[:], in_=tri32[:]), reads=["tri32"], writes=["tri"])
    p.op("pool", lambda e: e.memset(ones[:], 1.0), writes=["ones"])

    QT = p.sb([128, L], BF16, "QT")
    KT = p.sb([128, L], BF16, "KT")
    V = p.sb([128, NT, 128], BF16, "V")

    hs = HStream(p, io["hblk"])
    zps = [p.ps([128, 512], F32, "zps") for _ in range(3)]
    cps = [p.ps([128, 512], F32, "cps") for _ in range(3)]
    pq = zps
    pv = [c[:, 0:128] for c in cps]
    for blk in range(NB):
        hb, hres = hs.load(blk)
        for wi, (w, wres, dst, scale) in enumerate(((wq, "wq", QT, 0.125), (wk, "wk", KT, 1.0))):
            k = wi
            for kc in range(8):
                p.op("pe", lambda e, k=k, w=w, kc=kc, hb=hb: e.matmul(pq[k][:], lhsT=w[:, kc, :], rhs=hb[:, kc, :],
                                                                     start=(kc == 0), stop=(kc == 7)),
                     reads=[wres, hres], writes=[f"z{k}"])
            p.op("act", lambda e, k=k, dst=dst, blk=blk, scale=scale: e.activation(
                out=dst[:, blk * 512:(blk + 1) * 512], in_=pq[k][:], func=AF.Copy, scale=scale),
                reads=[f"z{k}"], writes=[("QK", wi, blk)])
        for sub in range(4):
            k = sub % 2
            t = blk * 4 + sub
            for kc in range(8):
                p.op("pe", lambda e, k=k, kc=kc, hb=hb, sub=sub: e.matmul(
                    pv[k], lhsT=hb[:, kc, sub * 128:(sub + 1) * 128], rhs=wv[:, kc, :],
                    start=(kc == 0), stop=(kc == 7)), reads=["wv", hres], writes=[f"c{k}"])
            p.op("dve", lambda e, k=k, t=t: e.tensor_copy(out=V[:, t, :], in_=pv[k]),
                 reads=[f"c{k}"], writes=[("V", t)])

    _ops1 = [p.ps([128, 512], F32, "ops") for _ in range(2)]
    ops_ = [[_ops1[h], _ops1[h]] for h in range(2)]
    NE = 7
    e32 = [p.sb([128, 512], F32, "e32") for _ in range(NE)]
    spb = [p.sb([128, 512], BF16, "spb") for _ in range(3)]
    wb = [p.sb([128, 512], BF16, "w") for _ in range(3)]
    S32 = [[p.sb([128, 512], F32, "S32") for _ in range(2)] for _ in range(2)]
    Sb = [[p.sb([128, 512], BF16, "Sb") for _ in range(2)] for _ in range(2)]
    yo = [[p.sb([128, 512], F32, "yo") for _ in range(2)] for _ in range(2)]
    tiles = []
    for I in range(NB):
        njb = 4 * I + 4
        for idx, j in enumerate(range(njb - 1, -1, -1)):
            for h in range(2):
                tiles.append(dict(I=I, h=h, j=j, idx=idx, first=(idx == 0), last=(j == 0), r=j - 4 * I))
    n = len(tiles)

    def stA(t, T):
        k = t % 3
        hp = slice(T["h"] * 64, (T["h"] + 1) * 64)
        j, I = T["j"], T["I"]
        p.op("pe", lambda e: e.matmul(zps[k][:], lhsT=KT[hp, j * 128:(j + 1) * 128], rhs=QT[hp, I * 512:(I + 1) * 512],
                                      start=True, stop=True), reads=[("QK", 0, I), ("QK", 1, j // 4)], writes=[f"z{k}"])

    def stB1(t, T):
        k, ke = t % 3, t % NE
        p.op("act", lambda e: e.activation(out=e32[ke][:], in_=zps[k][:], func=AF.Exp), reads=[f"z{k}"], writes=[f"e{ke}"])
        if T["r"] >= 0:
            r = T["r"]
            p.op("dve", lambda e: e.tensor_tensor(out=e32[ke][:], in0=e32[ke][:], in1=masks[:, r, :], op=ALU.mult),
                 reads=[f"e{ke}", "masks"], writes=[f"e{ke}"])

    def stB2(t, T):
        ke, ks, h = t % NE, t % 3, T["h"]
        p.op("act", lambda e: e.activation(out=spb[ks][:], in_=e32[ke][:], func=AF.Ln, bias=1.0),
             reads=[f"e{ke}"], writes=[f"sp{ks}"])
        if not T["last"]:
            ver = (T["idx"] + 1) % 2
            old = T["idx"] % 2
            if T["first"]:
                p.op("dve", lambda e: e.tensor_copy(out=S32[h][ver][:], in_=spb[ks][:]), reads=[f"sp{ks}"], writes=[f"S32{h}{ver}"])
                p.op("pool", lambda e: e.tensor_copy(out=Sb[h][ver][:], in_=spb[ks][:]), reads=[f"sp{ks}"], writes=[f"Sb{h}{ver}"])
            else:
                p.op("dve", lambda e: e.tensor_tensor(out=S32[h][ver][:], in0=S32[h][old][:], in1=spb[ks][:], op=ALU.add),
                     reads=[f"sp{ks}", f"S32{h}{old}"], writes=[f"S32{h}{ver}"])
                p.op("pool", lambda e: e.tensor_tensor(out=Sb[h][ver][:], in0=S32[h][old][:], in1=spb[ks][:], op=ALU.add),
                     reads=[f"sp{ks}", f"S32{h}{old}"], writes=[f"Sb{h}{ver}"])

    def stC1(t, T):
        k, ks, h = t % 3, t % 3, T["h"]
        first = T["first"]
        p.op("pe", lambda e: e.matmul(cps[k][:], lhsT=tri[:], rhs=spb[ks][:], start=True, stop=first),
             reads=["tri", f"sp{ks}"], writes=[f"c{k}"])
        if not first:
            ver = T["idx"] % 2
            p.op("pe", lambda e: e.matmul(cps[k][:], lhsT=ones[:], rhs=Sb[h][ver][:], start=False, stop=True),
                 reads=["ones", f"Sb{h}{ver}"], writes=[f"c{k}"])

    def stC2(t, T):
        k = t % 3
        p.op("act", lambda e: e.activation(out=cps[k][:], in_=cps[k][:], func=AF.Exp, scale=-1.0),
             reads=[f"c{k}"], writes=[f"c{k}"])

    def stD(t, T):
        k, kw, ke, h, I, j = t % 3, t % 3, t % NE, T["h"], T["I"], T["j"]
        par = I % 2
        hp = slice(h * 64, (h + 1) * 64)
        p.op("dve", lambda e: e.tensor_tensor(out=wb[kw][:], in0=e32[ke][:], in1=cps[k][:], op=ALU.mult),
             reads=[f"e{ke}", f"c{k}"], writes=[f"w{kw}"])
        p.op("pe", lambda e: e.matmul(ops_[h][par][hp, :], lhsT=V[:, j, hp], rhs=wb[kw][:], start=T["first"], stop=T["last"]),
             reads=[("V", j), f"w{kw}"], writes=[f"o{h}"])
        if T["last"]:
            p.op("dve", lambda e: e.tensor_copy(out=yo[h][par][hp, :], in_=ops_[h][par][hp, :]),
                 reads=[f"o{h}"], writes=[f"yo{h}{par}"])
            yw.write(128 + h * 64, hp, yo[h][par][hp, :], f"yo{h}{par}", I)

    for step in range(n + 5):
        for off, st in enumerate((stA, stB1, stB2, stC1, stC2, stD)):
            t = step - off
            if 0 <= t < n:
                st(t, tiles[t])
    p.end_phase()


def sb_consts():
    tri = (np.arange(128)[:, None] >= np.arange(128)[None, :]).astype(np.float32)
    masks = np.zeros((128, 4, 512), np.float32)
    for r in range(4):
        masks[:, r, :] = ((128 * r + np.arange(128))[:, None] < np.arange(512)[None, :]).astype(np.float32)
    return tri, masks


RET_GAMMA = [1.0 - 2.0 ** (-5.0 - h) for h in range(4)]


def emit_ret(p, io, L, yw_fn):
    NB = L // 512
    NT = L // 128
    wqk_d, wvg_d, cos_d, sin_d, dm_d, dec_d, gn_d, id_d = (io[k] for k in ("wqk", "wvg", "cos", "sin", "dmask", "dec", "gn",
                                                                          "ident"))
    yw = yw_fn()

    wqk = load_weight_bf16(p, wqk_d, 256, "wqk")
    wvg = load_weight_bf16(p, wvg_d, 512, "wvg", queue="act")
    cos = p.sb([128, NT, 4, 32], F32, "cos")
    sin = p.sb([128, NT, 4, 32], F32, "sin")
    dmask = p.sb([128, 2, 128], F32, "dmask")
    dec = p.sb([128, 6], F32, "dec")
    gn = p.sb([128, 256], F32, "gn")
    id32 = p.sb([128, 128], F32, "id32")
    ident = p.sb([128, 128], BF16, "ident")
    for t_, d_, n_ in ((cos, cos_d, "cos"), (sin, sin_d, "sin"), (dmask, dm_d, "dmask"), (dec, dec_d, "dec"),
                       (gn, gn_d, "gn"), (id32, id_d, "id32")):
        p.dma("act", t_[:], d_, writes=[n_])
    p.op("pool", lambda e: e.tensor_copy(out=ident[:], in_=id32[:]), reads=["id32"], writes=["ident"])

    st32 = p.sb([128, 128], F32, "st32")
    stb = p.sb([128, 128], BF16, "stb")
    p.op("pool", lambda e: e.memset(st32[:], 0.0), writes=["st32"])
    p.op("pool", lambda e: e.memset(stb[:], 0.0), writes=["stb"])

    qkps = p.ps([128, 256], F32, "qkps")
    vgps = p.ps([128, 512], F32, "vgps")
    trps = p.ps([128, 3, 128], BF16, "trps")
    scps = p.ps([128, 128], F32, "scps")
    ops_ = p.ps([128, 128], F32, "ops")
    kvps = p.ps([128, 128], F32, "kvps")

    ta = p.sb([128, 4, 32], F32, "ta")
    tb = p.sb([128, 4, 32], F32, "tb")
    tc_ = p.sb([128, 4, 32], F32, "tc")
    td = p.sb([128, 4, 32], F32, "td")
    R32 = p.sb([128, 4, 2, 32], F32, "R32")
    QKb = p.sb([128, 256], BF16, "QKb")
    QDb = p.sb([128, 128], BF16, "QDb")
    KDb = p.sb([128, 128], BF16, "KDb")
    TR = p.sb([128, 3, 128], BF16, "TR")
    Vb = p.sb([128, 256], BF16, "Vb")
    sg = p.sb([128, 256], F32, "sg")
    scb = p.sb([128, 128], BF16, "scb")
    junk = p.sb([128, 128], F32, "junk")
    stt = p.sb([128, 8], F32, "stt")
    on = p.sb([128, 128], F32, "on")
    yc = [p.sb([128, 256], F32, "yc") for _ in range(2)]
    ycT = [p.sb([128, 512], F32, "ycT") for _ in range(2)]
    typs = p.ps([128, 2, 128], F32, "typs")

    hs = HStream(p, io["hblk"])
    R32f = R32[:].rearrange("p a b c -> p (a b c)")
    for blk in range(NB):
        hb, hres = hs.load(blk)
        for sub in range(4):
            t = blk * 4 + sub
            ts_ = slice(sub * 128, (sub + 1) * 128)
            for kc in range(8):
                p.op("pe", lambda e, kc=kc, hb=hb, ts_=ts_: e.matmul(qkps[:], lhsT=hb[:, kc, ts_], rhs=wqk[:, kc, :],
                                                                     start=(kc == 0), stop=(kc == 7)),
                     reads=["wqk", hres], writes=["qkps"])
            for kc in range(8):
                p.op("pe", lambda e, kc=kc, hb=hb, ts_=ts_: e.matmul(vgps[:], lhsT=hb[:, kc, ts_], rhs=wvg[:, kc, :],
                                                                     start=(kc == 0), stop=(kc == 7)),
                     reads=["wvg", hres], writes=["vgps"])
            qk4 = qkps[:].rearrange("p (a b c) -> p a b c", a=4, b=2)
            t1 = qk4[:, :, 0, :]
            t2 = qk4[:, :, 1, :]
            p.op("dve", lambda e, t=t, t1=t1: e.tensor_tensor(out=ta[:], in0=t1, in1=cos[:, t, :, :], op=ALU.mult),
                 reads=["qkps", "cos"], writes=["ta"])
            p.op("dve", lambda e, t=t, t2=t2: e.tensor_tensor(out=tb[:], in0=t2, in1=sin[:, t, :, :], op=ALU.mult),
                 reads=["qkps", "sin"], writes=["tb"])
            p.op("dve", lambda e, t=t, t1=t1: e.tensor_tensor(out=tc_[:], in0=t1, in1=sin[:, t, :, :], op=ALU.mult),
                 reads=["qkps", "sin"], writes=["tc"])
            p.op("dve", lambda e, t=t, t2=t2: e.tensor_tensor(out=td[:], in0=t2, in1=cos[:, t, :, :], op=ALU.mult),
                 reads=["qkps", "cos"], writes=["td"])
            p.op("pool", lambda e: e.tensor_tensor(out=R32[:, :, 0, :], in0=ta[:], in1=tb[:], op=ALU.subtract),
                 reads=["ta", "tb"], writes=["R32"])
            p.op("pool", lambda e: e.tensor_tensor(out=R32[:, :, 1, :], in0=tc_[:], in1=td[:], op=ALU.add),
                 reads=["tc", "td"], writes=["R32"])
            p.op("act", lambda e: e.activation(out=QKb[:, 0:128], in_=R32f[:, 0:128], func=AF.Copy),
                 reads=["R32"], writes=["QKb"])
            p.op("act", lambda e: e.activation(out=QKb[:, 128:256], in_=R32f[:, 128:256], func=AF.Copy, scale=0.125),
                 reads=["R32"], writes=["QKb"])
            for h in range(2):
                p.op("dve", lambda e, h=h: e.tensor_scalar(out=QDb[:, h * 64:(h + 1) * 64], in0=R32f[:, h * 64:(h + 1) * 64],
                                                           scalar1=dec[:, h:h + 1], scalar2=None, op0=ALU.mult),
                     reads=["R32", "dec"], writes=["QDb"])
                p.op("dve", lambda e, h=h: e.tensor_scalar(out=KDb[:, h * 64:(h + 1) * 64],
                                                           in0=R32f[:, 128 + h * 64:128 + (h + 1) * 64],
                                                           scalar1=dec[:, 2 + h:3 + h], scalar2=None, op0=ALU.mult),
                     reads=["R32", "dec"], writes=["KDb"])
            for i_, (src, sres) in enumerate(((QKb[:, 0:128], "QKb"), (QKb[:, 128:256], "QKb"), (QDb[:], "QDb"))):
                p.op("pe", lambda e, i_=i_, src=src: e.transpose(trps[:, i_, :], src, ident[:]),
                     reads=[sres, "ident"], writes=["trps"])
            p.op("act", lambda e: e.activation(out=TR[:], in_=trps[:], func=AF.Copy), reads=["trps"], writes=["TR"])
            p.op("act", lambda e: e.activation(out=Vb[:], in_=vgps[:, 0:256], func=AF.Copy), reads=["vgps"], writes=["Vb"])
            p.op("act", lambda e: e.activation(out=sg[:], in_=vgps[:, 256:512], func=AF.Silu), reads=["vgps"], writes=["sg"])
            yk = t % 2
            for h in range(2):
                hp = slice(h * 64, (h + 1) * 64)
                vs = slice(h * 128, (h + 1) * 128)
                p.op("pe", lambda e, hp=hp: e.matmul(scps[:], lhsT=TR[hp, 1, :], rhs=TR[hp, 0, :], start=True, stop=True),
                     reads=["TR"], writes=["scps"])
                p.op("dve", lambda e, h=h: e.tensor_tensor(out=scb[:], in0=scps[:], in1=dmask[:, h, :], op=ALU.mult),
                     reads=["scps", "dmask"], writes=["scb"])
                p.op("pe", lambda e, vs=vs, t=t: e.matmul(ops_[:], lhsT=scb[:], rhs=Vb[:, vs], start=True, stop=(t == 0)),
                     reads=["scb", "Vb"], writes=["ops"])
                if t > 0:
                    p.op("pe", lambda e, hp=hp: e.matmul(ops_[:], lhsT=TR[hp, 2, :], rhs=stb[hp, :], start=False, stop=True),
                         reads=["TR", "stb"], writes=["ops"])
                p.op("pe", lambda e, h=h, hp=hp, vs=vs: e.matmul(kvps[hp, :], lhsT=KDb[:, h * 64:(h + 1) * 64], rhs=Vb[:, vs],
                                                                 start=True, stop=True),
                     reads=["KDb", "Vb"], writes=["kvps"])
                p.op("dve", lambda e, h=h, hp=hp: e.scalar_tensor_tensor(out=st32[hp, :], in0=st32[hp, :],
                                                                          scalar=dec[hp, 4 + h:5 + h], in1=kvps[hp, :],
                                                                          op0=ALU.mult, op1=ALU.add),
                     reads=["st32", "kvps", "dec"], writes=["st32"])
                p.op("pool", lambda e, hp=hp: e.tensor_copy(out=stb[hp, :], in_=st32[hp, :]), reads=["st32"], writes=["stb"])
                p.op("act", lambda e: e.activation(out=junk[:], in_=ops_[:], func=AF.Identity, accum_out=stt[:, 0:1]),
                     reads=["ops"], writes=["junk", "stt"])
                p.op("act", lambda e: e.activation(out=junk[:], in_=ops_[:], func=AF.Square, accum_out=stt[:, 1:2]),
                     reads=["ops"], writes=["junk", "stt"])
                p.op("dve", lambda e: e.tensor_scalar(out=stt[:, 2:3], in0=stt[:, 0:1], scalar1=1.0 / 128, scalar2=None,
                                                      op0=ALU.mult), reads=["stt"], writes=["stt"])
                p.op("dve", lambda e: e.tensor_tensor(out=stt[:, 3:4], in0=stt[:, 2:3], in1=stt[:, 2:3], op=ALU.mult),
                     reads=["stt"], writes=["stt"])
                p.op("dve", lambda e: e.scalar_tensor_tensor(out=stt[:, 4:5], in0=stt[:, 1:2], scalar=1.0 / 128,
                                                             in1=stt[:, 3:4], op0=ALU.mult, op1=ALU.subtract),
                     reads=["stt"], writes=["stt"])
                p.op("act", lambda e: e.activation(out=stt[:, 6:7], in_=stt[:, 4:5], func=AF.Sqrt, bias=LN_EPS),
                     reads=["stt"], writes=["stt"])
                p.op("dve", lambda e: e.reciprocal(out=stt[:, 5:6], in_=stt[:, 6:7]), reads=["stt"], writes=["stt"])
                p.op("dve", lambda e: e.tensor_scalar(out=on[:], in0=ops_[:], scalar1=stt[:, 2:3], scalar2=stt[:, 5:6],
                                                      op0=ALU.subtract, op1=ALU.mult), reads=["ops", "stt"], writes=["on"])
                p.op("pool", lambda e, vs=vs: e.tensor_tensor(out=on[:], in0=on[:], in1=gn[:, vs], op=ALU.mult),
                     reads=["on", "gn"], writes=["on"])
                p.op("pool", lambda e, vs=vs, yk=yk: e.tensor_tensor(out=yc[yk][:, vs], in0=on[:], in1=sg[:, vs], op=ALU.mult),
                     reads=["on", "sg"], writes=[f"yc{yk}"])
            for cch in range(2):
                p.op("pe", lambda e, cch=cch, yk=yk: e.transpose(typs[:, cch, :], yc[yk][:, cch * 128:(cch + 1) * 128], id32[:]),
                     reads=[f"yc{yk}", "id32"], writes=["typs"])
            p.op("act", lambda e, sub=sub: e.activation(out=ycT[0][:, sub * 128:(sub + 1) * 128], in_=typs[:, 0, :], func=AF.Copy),
                 reads=["typs"], writes=["ycT0"])
            p.op("act", lambda e, sub=sub: e.activation(out=ycT[1][:, sub * 128:(sub + 1) * 128], in_=typs[:, 1, :], func=AF.Copy),
                 reads=["typs"], writes=["ycT1"])
        for cch in range(2):
            yw.write(256 + cch * 128, slice(0, 128), ycT[cch][:], f"ycT{cch}", blk)
    p.end_phase()


def ret_consts(L, hf):
    NT = L // 128
    half = 32
    inv_freq = (10000.0 ** (-np.arange(half, dtype=np.float32) / half)).astype(np.float32)
    pos = np.arange(L, dtype=np.float32)
    ang = (pos[:, None] * inv_freq[None, :]).astype(np.float32)
    c = np.cos(ang).astype(np.float32).reshape(NT, 128, 32).transpose(1, 0, 2)
    s = np.sin(ang).astype(np.float32).reshape(NT, 128, 32).transpose(1, 0, 2)
    cos = np.ascontiguousarray(np.broadcast_to(c[:, :, None, :], (128, NT, 4, 32)))
    sin = np.ascontiguousarray(np.broadcast_to(s[:, :, None, :], (128, NT, 4, 32)))
    idx = np.arange(128)
    dmask = np.zeros((128, 2, 128), np.float32)
    dec = np.zeros((128, 6), np.float32)
    for h in range(2):
        g = RET_GAMMA[2 * hf + h]
        lg = math.log(g)
        sI = idx[:, None]
        cI = idx[None, :]
        same = (sI // 64) == (cI // 64)
        m = np.where(same, np.exp(lg * np.abs(cI - sI)), np.where(cI > sI, np.exp(lg * (cI - sI)), 0.0))
        dmask[:, h, :] = m
        dec[:, h] = np.exp(lg * (idx + 1.0))
        dec[:, 2 + h] = 0.125 * np.exp(lg * (127.0 - idx))
        dec[:, 4 + h] = math.exp(lg * 128.0)
    return cos, sin, dmask, dec, np.eye(128, dtype=np.float32)

def emit_lambar(p, lre, lim, ldt, shape, tag):
    def T(n):
        return p.sb(shape, F32, tag + n)
    dt, a, th, mag, c, s, t1, t2, c2, s2, lbr, lbi = (T(n) for n in
                                                     ("dt", "a", "th", "mag", "c", "s", "t1", "t2", "c2", "s2", "lbr", "lbi"))
    R = tag + "res"
    p.op("act", lambda e: e.activation(out=dt[:], in_=ldt[:], func=AF.Exp), reads=[tag + "in"], writes=[R])
    p.op("dve", lambda e: e.tensor_tensor(out=a[:], in0=lre[:], in1=dt[:], op=ALU.mult), reads=[tag + "in", R], writes=[R])
    p.op("dve", lambda e: e.tensor_tensor(out=th[:], in0=lim[:], in1=dt[:], op=ALU.mult), reads=[tag + "in", R], writes=[R])
    p.op("act", lambda e: e.activation(out=mag[:], in_=a[:], func=AF.Exp), reads=[R], writes=[R])
    p.op("act", lambda e: e.activation(out=s[:], in_=th[:], func=AF.Sin, scale=1.0 / 16), reads=[R], writes=[R])
    p.op("dve", lambda e: e.tensor_scalar(out=t1[:], in0=th[:], scalar1=-1.0 / 16, scalar2=math.pi / 2, op0=ALU.mult,
                                          op1=ALU.add), reads=[R], writes=[R])
    p.op("act", lambda e: e.activation(out=c[:], in_=t1[:], func=AF.Sin), reads=[R], writes=[R])
    cur = (c, s)
    nxt = (c2, s2)
    for _ in range(4):
        cc, ss = cur
        nc_, ns_ = nxt
        p.op("dve", lambda e, cc=cc: e.tensor_tensor(out=t1[:], in0=cc[:], in1=cc[:], op=ALU.mult), reads=[R], writes=[R])
        p.op("dve", lambda e, ss=ss: e.tensor_tensor(out=t2[:], in0=ss[:], in1=ss[:], op=ALU.mult), reads=[R], writes=[R])
        p.op("dve", lambda e, cc=cc, ss=ss, ns_=ns_: e.scalar_tensor_tensor(out=ns_[:], in0=cc[:], scalar=2.0, in1=ss[:],
                                                                            op0=ALU.mult, op1=ALU.mult), reads=[R], writes=[R])
        p.op("dve", lambda e, nc_=nc_: e.tensor_tensor(out=nc_[:], in0=t1[:], in1=t2[:], op=ALU.subtract), reads=[R], writes=[R])
        cur, nxt = nxt, cur
    cc, ss = cur
    p.op("dve", lambda e: e.tensor_tensor(out=lbr[:], in0=mag[:], in1=cc[:], op=ALU.mult), reads=[R], writes=[R])
    p.op("dve", lambda e: e.tensor_tensor(out=lbi[:], in0=mag[:], in1=ss[:], op=ALU.mult), reads=[R], writes=[R])
    return lbr, lbi, R, (t1, t2, a, th, c, s)


def emit_s5(p, io, L, yw_fn, SEG=1024):
    stop = 0
    SEG = min(SEG, L)
    NSEG = L // SEG
    NBS = SEG // 512
    NK = int(math.log2(SEG))
    wu_d, rep_d, col_d, bp_d, cp_d, dcol_d = (io[k] for k in ("wu", "lam_rep", "lam_col", "bp", "cp", "dcol"))
    yw = yw_fn()

    wu = load_weight_bf16(p, wu_d, 128, "wu")
    rep = p.sb([128, 3, 512], F32, "rep")
    col = p.sb([128, 3, 4], F32, "col")
    bp = p.sb([128, 2, 4, 128], F32, "bp")
    cp = p.sb([128, 2, 4, 128], F32, "cp")
    dcol = p.sb([128, 1], F32, "dcol")
    p.dma("act", rep[:], rep_d, writes=["repin"])
    p.dma("act", col[:], col_d, writes=["colin"])
    p.dma("act", bp[:], bp_d, writes=["bp"])
    p.dma("act", cp[:], cp_d, writes=["cp"])
    p.dma("act", dcol[:], dcol_d, writes=["dcol"])

    lbr, lbi, R, (t1, t2, t3, t4, t5, t6) = emit_lambar(p, rep[:, 0, :], rep[:, 1, :], rep[:, 2, :], [128, 512], "rep")
    lr, li = rep[:, 0, :], rep[:, 1, :]
    zr = p.sb([128, 512], F32, "zr")
    zi = p.sb([128, 512], F32, "zi")
    nr = p.sb([128, 512], F32, "nr")
    p.op("dve", lambda e: e.tensor_scalar(out=nr[:], in0=lbr[:], scalar1=-1.0, scalar2=None, op0=ALU.add), reads=[R], writes=[R])
    p.op("dve", lambda e: e.tensor_tensor(out=t1[:], in0=lr, in1=lr, op=ALU.mult), reads=["repin", R], writes=[R])
    p.op("dve", lambda e: e.tensor_tensor(out=t2[:], in0=li, in1=li, op=ALU.mult), reads=["repin", R], writes=[R])
    p.op("dve", lambda e: e.tensor_tensor(out=t3[:], in0=t1[:], in1=t2[:], op=ALU.add), reads=[R], writes=[R])
    p.op("dve", lambda e: e.reciprocal(out=t4[:], in_=t3[:]), reads=[R], writes=[R])
    p.op("dve", lambda e: e.tensor_tensor(out=t1[:], in0=nr[:], in1=lr, op=ALU.mult), reads=[R], writes=[R])
    p.op("dve", lambda e: e.tensor_tensor(out=t2[:], in0=lbi[:], in1=li, op=ALU.mult), reads=[R], writes=[R])
    p.op("dve", lambda e: e.tensor_tensor(out=t1[:], in0=t1[:], in1=t2[:], op=ALU.add), reads=[R], writes=[R])
    p.op("dve", lambda e: e.tensor_tensor(out=zr[:], in0=t1[:], in1=t4[:], op=ALU.mult), reads=[R], writes=[R])
    p.op("dve", lambda e: e.tensor_tensor(out=t1[:], in0=lbi[:], in1=lr, op=ALU.mult), reads=[R], writes=[R])
    p.op("dve", lambda e: e.tensor_tensor(out=t2[:], in0=nr[:], in1=li, op=ALU.mult), reads=[R], writes=[R])
    p.op("dve", lambda e: e.tensor_tensor(out=t1[:], in0=t1[:], in1=t2[:], op=ALU.subtract), reads=[R], writes=[R])
    p.op("dve", lambda e: e.tensor_tensor(out=zi[:], in0=t1[:], in1=t4[:], op=ALU.mult), reads=[R], writes=[R])
    bb = p.sb([128, 2, 4, 128], BF16, "bb")
    ncp = p.sb([128, 2, 4, 128], BF16, "cpb")
    for pr in range(4):
        ps_ = slice(pr * 128, (pr + 1) * 128)
        u1 = t5[:, 0:128]
        u2 = t6[:, 0:128]
        p.op("dve", lambda e, pr=pr, ps_=ps_: e.tensor_tensor(out=u1, in0=bp[:, 0, pr, :], in1=zr[:, ps_], op=ALU.mult),
             reads=["bp", R], writes=[R])
        p.op("dve", lambda e, pr=pr, ps_=ps_: e.tensor_tensor(out=u2, in0=bp[:, 1, pr, :], in1=zi[:, ps_], op=ALU.mult),
             reads=["bp", R], writes=[R])
        p.op("dve", lambda e, pr=pr: e.tensor_tensor(out=bb[:, 0, pr, :], in0=u1, in1=u2, op=ALU.subtract),
             reads=[R], writes=[R, "bb"])
        p.op("dve", lambda e, pr=pr, ps_=ps_: e.tensor_tensor(out=u1, in0=bp[:, 0, pr, :], in1=zi[:, ps_], op=ALU.mult),
             reads=["bp", R], writes=[R])
        p.op("dve", lambda e, pr=pr, ps_=ps_: e.tensor_tensor(out=u2, in0=bp[:, 1, pr, :], in1=zr[:, ps_], op=ALU.mult),
             reads=["bp", R], writes=[R])
        p.op("dve", lambda e, pr=pr: e.tensor_tensor(out=bb[:, 1, pr, :], in0=u1, in1=u2, op=ALU.add),
             reads=[R], writes=[R, "bb"])
    p.op("act", lambda e: e.activation(out=ncp[:, 0, :, :], in_=cp[:, 0, :, :], func=AF.Copy), reads=["cp"], writes=["cpb"])
    p.op("act", lambda e: e.activation(out=ncp[:, 1, :, :], in_=cp[:, 1, :, :], func=AF.Copy, scale=-1.0),
         reads=["cp"], writes=["cpb"])

    cbr, cbi, RC, (c1, c2, _, _, _, _) = emit_lambar(p, col[:, 0, :], col[:, 1, :], col[:, 2, :], [128, 4], "col")
    AR = p.sb([128, NK + 1, 4], F32, "AR")
    AI = p.sb([128, NK + 1, 4], F32, "AI")
    NAI = p.sb([128, NK + 1, 4], F32, "NAI")
    p.op("dve", lambda e: e.tensor_copy(out=AR[:, 0, :], in_=cbr[:]), reads=[RC], writes=["A"])
    p.op("dve", lambda e: e.tensor_copy(out=AI[:, 0, :], in_=cbi[:]), reads=[RC], writes=["A"])
    for k in range(1, NK + 1):
        p.op("dve", lambda e, k=k: e.tensor_tensor(out=c1[:], in0=AR[:, k - 1, :], in1=AR[:, k - 1, :], op=ALU.mult),
             reads=["A", RC], writes=[RC])
        p.op("dve", lambda e, k=k: e.tensor_tensor(out=c2[:], in0=AI[:, k - 1, :], in1=AI[:, k - 1, :], op=ALU.mult),
             reads=["A", RC], writes=[RC])
        p.op("dve", lambda e, k=k: e.tensor_tensor(out=AR[:, k, :], in0=c1[:], in1=c2[:], op=ALU.subtract),
             reads=[RC, "A"], writes=["A"])
        p.op("dve", lambda e, k=k: e.scalar_tensor_tensor(out=AI[:, k, :], in0=AR[:, k - 1, :], scalar=2.0, in1=AI[:, k - 1, :],
                                                          op0=ALU.mult, op1=ALU.mult), reads=["A"], writes=["A"])
    p.op("dve", lambda e: e.tensor_scalar(out=NAI[:], in0=AI[:], scalar1=-1.0, scalar2=None, op0=ALU.mult),
         reads=["A"], writes=["A"])

    UT = p.sb([128, SEG], BF16, "UT")
    U32 = p.sb([128, SEG], F32, "U32")
    RE = [[p.sb([128, SEG], F32, "RE") for _ in range(2)] for _ in range(2)]
    IM = [[p.sb([128, SEG], F32, "IM") for _ in range(2)] for _ in range(2)]
    REb = p.sb([128, 4, SEG], BF16, "REb")
    IMb = p.sb([128, 4, SEG], BF16, "IMb")
    carry = p.sb([128, 4, 2], F32, "carry")
    tmpB = [p.sb([128, SEG], F32, "tmpB") for _ in range(2)]
    ctmp = p.sb([128, 4], F32, "ctmp")
    ups = [p.ps([128, 512], F32, "ups") for _ in range(2)]
    bps = [p.ps([128, 512], F32, "bps") for _ in range(2)]
    yps = [p.ps([128, 512], F32, "yps") for _ in range(2)]
    g1 = p.sb([128, 512], F32, "g1")
    g2 = p.sb([128, 512], F32, "g2")
    g3 = p.sb([128, 512], F32, "g3")
    yo = [p.sb([128, 512], F32, "yo") for _ in range(2)]
    hs = HStream(p, io["hblk"])
    for sg_ in range(NSEG):
        for b_ in range(NBS):
            blk = sg_ * NBS + b_
            hb, hres = hs.load(blk)
            k = b_ % 2
            for kc in range(8):
                p.op("pe", lambda e, k=k, kc=kc, hb=hb: e.matmul(ups[k][:], lhsT=wu[:, kc, :], rhs=hb[:, kc, :],
                                                                 start=(kc == 0), stop=(kc == 7)),
                     reads=["wu", hres], writes=[f"ups{k}"])
            cs = slice(b_ * 512, (b_ + 1) * 512)
            p.op("dve", lambda e, k=k, cs=cs: e.tensor_copy(out=U32[:, cs], in_=ups[k][:]),
                 reads=[f"ups{k}"], writes=[("U32", b_)])
            p.op("act", lambda e, cs=cs: e.activation(out=UT[:, cs], in_=U32[:, cs], func=AF.Copy),
                 reads=[("U32", b_)], writes=[("UT", b_)])
        for pg in range(2):
            lanes = ((0, 2 * pg), (1, 2 * pg + 1))
            for ln, pr in lanes:
                for b_ in range(NBS):
                    cs = slice(b_ * 512, (b_ + 1) * 512)
                    for ri, dst in enumerate((RE[ln][0], IM[ln][0])):
                        k = (b_ * 2 + ri) % 2
                        p.op("pe", lambda e, k=k, ri=ri, pr=pr, cs=cs: e.matmul(bps[k][:], lhsT=bb[:, ri, pr, :], rhs=UT[:, cs],
                                                                                start=True, stop=True),
                             reads=["bb", ("UT", b_)], writes=[f"bps{k}"])
                        p.op("act", lambda e, k=k, dst=dst, cs=cs: e.activation(out=dst[:, cs], in_=bps[k][:], func=AF.Copy),
                             reads=[f"bps{k}"], writes=[("RI", ln, 0, ri)])
                if sg_ > 0:
                    cr = carry[:, pr, 0:1]
                    ci = carry[:, pr, 1:2]
                    R0, I0 = RE[ln][0], IM[ln][0]
                    for dstT, ri, pairA, pairB in ((R0, 0, (AR, cr), (NAI, ci)), (I0, 1, (AI, cr), (AR, ci))):
                        for sc, cc in (pairA, pairB):
                            p.op("dve", lambda e, pr=pr, dstT=dstT, sc=sc, cc=cc: e.scalar_tensor_tensor(
                                out=dstT[:, 0:1], in0=cc, scalar=sc[:, 0, pr:pr + 1], in1=dstT[:, 0:1], op0=ALU.mult, op1=ALU.add),
                                reads=["carry", "A", ("RI", ln, 0, ri)], writes=[("RI", ln, 0, ri)])
            src = 0
            for k in range(NK):
                d = 1 << k
                dst = 1 - src
                for ln, pr in lanes:
                    sR, sI, dR, dI = RE[ln][src], IM[ln][src], RE[ln][dst], IM[ln][dst]
                    ar = AR[:, k, pr:pr + 1]
                    ai = AI[:, k, pr:pr + 1]
                    nai = NAI[:, k, pr:pr + 1]
                    tB = tmpB[ln]
                    rs = [("RI", ln, src, 0), ("RI", ln, src, 1), "A"]
                    p.op("act", lambda e, sR=sR, dR=dR, d=d: e.activation(out=dR[:, 0:d], in_=sR[:, 0:d], func=AF.Copy),
                         reads=rs, writes=[("RI", ln, dst, 0)])
                    p.op("act", lambda e, sI=sI, dI=dI, d=d: e.activation(out=dI[:, 0:d], in_=sI[:, 0:d], func=AF.Copy),
                         reads=rs, writes=[("RI", ln, dst, 1)])
                    p.op("dve", lambda e, sR=sR, dR=dR, d=d, ar=ar: e.scalar_tensor_tensor(
                        out=dR[:, d:SEG], in0=sR[:, 0:SEG - d], scalar=ar, in1=sR[:, d:SEG], op0=ALU.mult, op1=ALU.add),
                        reads=rs, writes=[("RI", ln, dst, 0)])
                    p.op("act", lambda e, sI=sI, d=d, ar=ar, tB=tB: e.activation(out=tB[:, d:SEG], in_=sI[:, 0:SEG - d],
                                                                                 func=AF.Identity, scale=ar),
                         reads=rs, writes=[f"tmpB{ln}"])
                    p.op("dve", lambda e, sR=sR, sI=sI, dI=dI, d=d, ai=ai: e.scalar_tensor_tensor(
                        out=dI[:, d:SEG], in0=sR[:, 0:SEG - d], scalar=ai, in1=sI[:, d:SEG], op0=ALU.mult, op1=ALU.add),
                        reads=rs, writes=[("RI", ln, dst, 1)])
                    p.op("dve", lambda e, sI=sI, dR=dR, d=d, nai=nai: e.scalar_tensor_tensor(
                        out=dR[:, d:SEG], in0=sI[:, 0:SEG - d], scalar=nai, in1=dR[:, d:SEG], op0=ALU.mult, op1=ALU.add),
                        reads=rs + [("RI", ln, dst, 0)], writes=[("RI", ln, dst, 0)])
                    p.op("pool", lambda e, dI=dI, d=d, tB=tB: e.tensor_tensor(out=dI[:, d:SEG], in0=tB[:, d:SEG], in1=dI[:, d:SEG],
                                                                              op=ALU.add),
                         reads=[f"tmpB{ln}", ("RI", ln, dst, 1)], writes=[("RI", ln, dst, 1)])
                src = dst
            for ln, pr in lanes:
                fR, fI = RE[ln][src], IM[ln][src]
                p.op("act", lambda e, fR=fR, pr=pr: e.activation(out=REb[:, pr, :], in_=fR[:], func=AF.Copy),
                     reads=[("RI", ln, src, 0)], writes=[("Sb", pr)])
                p.op("act", lambda e, fI=fI, pr=pr: e.activation(out=IMb[:, pr, :], in_=fI[:], func=AF.Copy),
                     reads=[("RI", ln, src, 1)], writes=[("Sb", pr)])
                p.op("dve", lambda e, fR=fR, pr=pr: e.tensor_copy(out=carry[:, pr, 0:1], in_=fR[:, SEG - 1:SEG]),
                     reads=[("RI", ln, src, 0)], writes=["carry"])
                p.op("dve", lambda e, fI=fI, pr=pr: e.tensor_copy(out=carry[:, pr, 1:2], in_=fI[:, SEG - 1:SEG]),
                     reads=[("RI", ln, src, 1)], writes=["carry"])
        for b_ in range(NBS):
            blk = sg_ * NBS + b_
            k = b_ % 2
            cs = slice(b_ * 512, (b_ + 1) * 512)
            n = 0
            for pr in range(4):
                for ri, Sx in enumerate((REb, IMb)):
                    p.op("pe", lambda e, k=k, pr=pr, ri=ri, Sx=Sx, cs=cs, n=n: e.matmul(
                        yps[k][:], lhsT=ncp[:, ri, pr, :], rhs=Sx[:, pr, cs], start=(n == 0), stop=(n == 7)),
                        reads=["cpb", ("Sb", pr)], writes=[f"yps{k}"])
                    n += 1
            p.op("dve", lambda e, k=k, cs=cs: e.scalar_tensor_tensor(out=g1[:], in0=U32[:, cs], scalar=dcol[:, 0:1], in1=yps[k][:],
                                                                     op0=ALU.mult, op1=ALU.add),
                 reads=[f"yps{k}", ("U32", b_), "dcol"], writes=["g1"])
            p.op("act", lambda e: e.activation(out=g2[:], in_=g1[:], func=AF.Square), reads=["g1"], writes=["g2"])
            p.op("dve", lambda e: e.tensor_scalar(out=g2[:], in0=g2[:], scalar1=0.044715, scalar2=1.0, op0=ALU.mult, op1=ALU.add),
                 reads=["g2"], writes=["g2"])
            p.op("dve", lambda e: e.tensor_tensor(out=g2[:], in0=g2[:], in1=g1[:], op=ALU.mult), reads=["g2", "g1"], writes=["g2"])
            p.op("act", lambda e: e.activation(out=g3[:], in_=g2[:], func=AF.Tanh, scale=math.sqrt(2.0 / math.pi)),
                 reads=["g2"], writes=["g3"])
            p.op("pool", lambda e: e.tensor_scalar(out=g1[:], in0=g1[:], scalar1=0.5, scalar2=None, op0=ALU.mult),
                 reads=["g1", "g2"], writes=["g1"])
            p.op("dve", lambda e, k=k: e.scalar_tensor_tensor(out=yo[k][:], in0=g3[:], scalar=1.0, in1=g1[:], op0=ALU.add,
                                                              op1=ALU.mult), reads=["g3", "g1"], writes=[f"yo{k}"])
            yw.write(0, slice(0, 128), yo[k][:], f"yo{k}", blk)
    p.end_phase()


def s5_host_layout(hf, lam_re, lam_im, log_dt, b_re, b_im, c_re, c_im, d_skip):
    G0 = 8 * hf
    f32 = np.float32
    rep = np.zeros((128, 3, 512), f32)
    rep[:, 0, :] = lam_re[G0:G0 + 8].reshape(1, 512)
    rep[:, 1, :] = lam_im[G0:G0 + 8].reshape(1, 512)
    rep[:, 2, :] = np.repeat(log_dt[G0:G0 + 8], 64)[None, :]
    col = np.zeros((128, 3, 4), f32)
    bp = np.zeros((128, 2, 4, 128), f32)
    cp = np.zeros((128, 2, 4, 128), f32)
    for pr in range(4):
        for gi in range(2):
            gl = 2 * pr + gi
            g = G0 + gl
            rows = slice(gi * 64, (gi + 1) * 64)
            col[rows, 0, pr] = lam_re[g]
            col[rows, 1, pr] = lam_im[g]
            col[rows, 2, pr] = log_dt[g]
            cols16 = slice(gl * 16, (gl + 1) * 16)
            bp[cols16, 0, pr, rows] = b_re[g].T
            bp[cols16, 1, pr, rows] = b_im[g].T
            cp[rows, 0, pr, cols16] = c_re[g].T
            cp[rows, 1, pr, cols16] = c_im[g].T
    dcol = np.ascontiguousarray(d_skip[hf * 128:(hf + 1) * 128].reshape(128, 1), dtype=f32)
    return rep, col, bp, cp, dcol


class Stager:
    def __init__(self, p, n=2, queues=("sp", "act"), cast_engs=("pool",), width=256):
        self.p = p
        self.width = width
        self.t = [p.sb([128, 8, width], F32, "stage") for _ in range(n)]
        self.i = 0
        self.queues = queues
        self.cast_engs = cast_engs

    def load(self, dst_fn, src3, kcn, ncols, dst_res):
        p = self.p
        chunk = self.width
        for c0 in range(0, ncols, chunk):
            w = min(chunk, ncols - c0)
            k = self.i % len(self.t)
            q = self.queues[self.i % len(self.queues)]
            ce = self.cast_engs[self.i % len(self.cast_engs)]
            self.i += 1
            st = self.t[k]
            p.dma(q, st[:, 0:kcn, 0:w], src3[:, :, c0:c0 + w], writes=[f"stage{k}"])
            dst = dst_fn(c0, w)
            p.op(ce, lambda e, st=st, dst=dst, kcn=kcn, w=w: e.tensor_copy(out=dst, in_=st[:, 0:kcn, 0:w]),
                 reads=[f"stage{k}"], writes=[dst_res])


def emit_ln_fm(p, r, rres, gcol, bcol, out, ores, ones32, psA, psB, sq, mean, rstd, tmp, tag, sqres=None):
    psa, psares = psA
    sqres = sqres or (sqres)
    psb, psbres = psB
    for f in range(8):
        p.op("pe", lambda e, f=f: e.matmul(psa[:], lhsT=ones32[:], rhs=r[:, f, :], start=(f == 0), stop=(f == 7)),
             reads=["ones32", rres], writes=[psares])
    p.op("act", lambda e: e.activation(out=sq[:], in_=r[:], func=AF.Square), reads=[rres], writes=[sqres])
    for f in range(8):
        p.op("pe", lambda e, f=f: e.matmul(psb[:], lhsT=ones32[:], rhs=sq[:, f, :], start=(f == 0), stop=(f == 7)),
             reads=["ones32", sqres], writes=[psbres])
    p.op("act", lambda e: e.activation(out=mean[:], in_=psa[:], func=AF.Copy, scale=1.0 / D_MODEL),
         reads=[psares], writes=[tag + "mean"])
    p.op("dve", lambda e: e.tensor_tensor(out=tmp[:], in0=mean[:], in1=mean[:], op=ALU.mult),
         reads=[tag + "mean"], writes=[tag + "tmp"])
    p.op("dve", lambda e: e.scalar_tensor_tensor(out=tmp[:], in0=psb[:], scalar=1.0 / D_MODEL, in1=tmp[:], op0=ALU.mult,
                                                 op1=ALU.subtract), reads=[psbres, tag + "tmp"], writes=[tag + "tmp"])
    p.op("act", lambda e: e.activation(out=tmp[:], in_=tmp[:], func=AF.Sqrt, bias=LN_EPS), reads=[tag + "tmp"],
         writes=[tag + "tmp"])
    p.op("dve", lambda e: e.reciprocal(out=rstd[:], in_=tmp[:]), reads=[tag + "tmp"], writes=[tag + "rstd"])
    for f in range(8):
        p.op("dve", lambda e, f=f: e.tensor_tensor(out=sq[:, f, :], in0=r[:, f, :], in1=mean[:], op=ALU.subtract),
             reads=[rres, tag + "mean", sqres], writes=[sqres])
        p.op("pool", lambda e, f=f: e.tensor_tensor(out=sq[:, f, :], in0=sq[:, f, :], in1=rstd[:], op=ALU.mult),
             reads=[sqres, tag + "rstd"], writes=[sqres])
        p.op("act", lambda e, f=f: e.activation(out=out[:, f, :], in_=sq[:, f, :], func=AF.Identity, scale=gcol[:, f:f + 1],
                                                bias=bcol[:, f:f + 1]), reads=[sqres, "lncols"], writes=[ores])


def emit_post(p, io, T):
    NB = T // 512
    v3 = lambda ap: ap.rearrange("(kc p) n -> p kc n", p=128)
    hT_d = v3(io["hT_half"])
    hTb_d = v3(io["hT_hbf"])
    y_d = v3(io["y_own"])
    wg_d, wglu_d, wua_d, wub_d, wuc_d, wo_d, wr_d = (v3(io[k]) for k in ("wg", "wglu", "wua", "wub", "wuc", "wo", "wr"))
    cols_d, rb_d, id_d = io["cols1"], io["rb"], io["ident"]
    h1tok_o, gate_o, rk_o, racc_o = io["h1tok"], io["gate"], io["rk"], io["racc"]

    wg = p.sb([128, 8, 3072], BF16, "wg")
    wglu = p.sb([128, 2, 256], BF16, "wglu")
    wua = p.sb([128, 2, D_MODEL], BF16, "wua")
    wub = p.sb([128, 2, D_MODEL], BF16, "wub")
    wuc = p.sb([128, 4, D_MODEL], BF16, "wuc")
    wo = p.sb([128, 8, D_MODEL], BF16, "wo")
    cols = p.sb([128, 18], F32, "cols")
    wr = p.sb([128, 8, 16], F32, "wr")
    rb = p.sb([128, 16], F32, "rb")
    ones32 = p.sb([128, 128], F32, "ones32")
    p.dma("sp", cols[:], cols_d, writes=["lncols"])
    p.dma("sp", wr[:], wr_d, writes=["wr"])
    p.dma("sp", rb[:], rb_d, writes=["rb"])
    id32 = p.sb([128, 128], F32, "id32")
    p.dma("sp", id32[:], id_d, writes=["id32"])
    p.op("pool", lambda e: e.memset(ones32[:], 1.0), writes=["ones32"])
    stg = Stager(p, n=2)
    stg.load(lambda c0, w: wglu[:, :, c0:c0 + w], wglu_d, 2, 256, "wglu")
    stg.load(lambda c0, w: wua[:, :, c0:c0 + w], wua_d, 2, D_MODEL, "wua")
    stg.load(lambda c0, w: wub[:, :, c0:c0 + w], wub_d, 2, D_MODEL, "wub")
    stg.load(lambda c0, w: wuc[:, :, c0:c0 + w], wuc_d, 4, D_MODEL, "wuc")
    stg.load(lambda c0, w: wg[:, :, c0:c0 + w], wg_d, 8, 3072, "wg")
    stg.load(lambda c0, w: wo[:, :, c0:c0 + w], wo_d, 8, D_MODEL, "wo")

    h32 = p.sb([128, 8, 512], F32, "h32")
    hb = p.sb([128, 8, 512], BF16, "hb")
    y32 = p.sb([128, 8, 512], F32, "y32")
    yb16 = p.sb([128, 8, 512], BF16, "yb16")
    sgm = p.sb([128, 512], F32, "sgm")
    sig = [p.sb([128, 512], F32, "sig") for _ in range(2)]
    macc = p.sb([128, 512], F32, "macc")
    mtmp = p.sb([128, 512], F32, "mtmp")
    mb = p.sb([128, 8, 512], BF16, "mb")
    r = p.sb([128, 8, 512], F32, "r")
    sq = y32
    h1 = r
    mean = p.sb([128, 512], F32, "mean")
    rstd = p.sb([128, 512], F32, "rstd")
    tmp = p.sb([128, 512], F32, "tmp")
    PS = [p.ps([128, 512], F32, "PS") for _ in range(7)]
    rps = p.ps([128, 128], F32, "rps")
    xs_z = io["xs"]
    zt = p.sb([128, D_MODEL], BF16, "zt")
    p.op("pool", lambda e: e.memset(zt[:], 0.0), writes=["zt"])
    for s_ in range(xs_z.shape[0] // 128):
        p.dma("act" if s_ % 2 else "sp", xs_z[s_ * 128:(s_ + 1) * 128, :], zt[:], reads=["zt"], writes=["xs"])
    LT32 = p.sb([128, 128], F32, "LT32")
    p.dma("sp", LT32[:], io["lt"], writes=["LT32"])
    Mt = p.sb([128, 16], F32, "Mt")
    Racc = p.sb([128, 16], F32, "Racc")
    rk_sb = [p.sb([128, 16], F32, "rk") for _ in range(2)]
    h1tok = [p.sb([128, D_MODEL], F32, "h1tok") for _ in range(2)]
    psi = [0]

    def nps():
        k = psi[0] % 7
        psi[0] += 1
        return PS[k], f"PS{k}"

    lg = p.sb([128, 16], F32, "lg")
    mx = p.sb([128, 4], F32, "mx")
    exd = p.sb([128, 4, 8], F32, "exd")
    pr6 = p.sb([128, 4, 6], F32, "pr6")
    gs = p.sb([128, 8], F32, "gs")
    cnt = p.sb([128, 4, 4], F32, "cnt")
    cmp_ = p.sb([128, 4, 4], F32, "cmp")
    gsel = p.sb([128, 16], F32, "gsel")
    gout = [p.sb([128, 16], F32, "gout") for _ in range(2)]
    ybranch = ((wua, "wua", (0, 4)), (wub, "wub", (1, 5)), (wuc, "wuc", (2, 3, 6, 7)))
    YA = (0, 4)
    for blk in range(NB):
        ts_ = slice(blk * 512, (blk + 1) * 512)
        p.dma("sp", h32[:], hT_d[:, :, ts_], reads=["hT_half"], writes=["h32"])
        p.dma("act", yb16[:], y_d[:, :, ts_], reads=["y_own"], writes=["y16"])
        p.dma("act", hb[:], hTb_d[:, :, ts_], reads=["hT_hbf"], writes=["hb"])
        yg = p.sb([128, 2, 512], BF16, "yg") if blk == 0 else yg
        for m in range(2):
            ps_, psr = nps()
            for kc in range(2):
                p.op("pe", lambda e, ps_=ps_, m=m, kc=kc: e.matmul(ps_[:], lhsT=wglu[:, kc, m * 128:(m + 1) * 128],
                                                                   rhs=yb16[:, YA[kc], :], start=(kc == 0), stop=(kc == 1)),
                     reads=["wglu", "y16"], writes=[psr])
            p.op("act", lambda e, ps_=ps_, m=m: e.activation(out=sgm[:], in_=ps_[:], func=AF.Sigmoid, bias=cols[:, 16 + m:17 + m]),
                 reads=[psr, "lncols"], writes=["sgm"])
            p.op("dve", lambda e, m=m: e.tensor_tensor(out=yg[:, m, :], in0=yb16[:, YA[m], :], in1=sgm[:], op=ALU.mult),
                 reads=["sgm", "y16"], writes=["yg"])
        for f in range(8):
            fs = slice(f * 128, (f + 1) * 128)
            for bi, (wup, wres, kcs) in enumerate(ybranch):
                nkc = len(kcs)
                gp, gpr = nps()
                for kc in range(8):
                    p.op("pe", lambda e, gp=gp, bi=bi, f=f, kc=kc: e.matmul(
                        gp[:], lhsT=wg[:, kc, bi * 1024 + f * 128: bi * 1024 + (f + 1) * 128], rhs=hb[:, kc, :],
                        start=(kc == 0), stop=(kc == 7)), reads=["wg", "hb"], writes=[gpr])
                sk = bi % 2
                p.op("act", lambda e, gp=gp, sk=sk: e.activation(out=sig[sk][:], in_=gp[:], func=AF.Sigmoid),
                     reads=[gpr], writes=[f"sig{sk}"])
                up, upr = nps()
                for kc in range(nkc):
                    rhs = yg[:, kc, :] if bi == 0 else yb16[:, kcs[kc], :]
                    p.op("pe", lambda e, up=up, wup=wup, kc=kc, fs=fs, rhs=rhs, nkc=nkc: e.matmul(
                        up[:], lhsT=wup[:, kc, fs], rhs=rhs, start=(kc == 0), stop=(kc == nkc - 1)),
                        reads=[wres, "yg" if bi == 0 else "y16"], writes=[upr])
                if bi == 0:
                    p.op("dve", lambda e, up=up, sk=sk: e.tensor_tensor(out=macc[:], in0=sig[sk][:], in1=up[:], op=ALU.mult),
                         reads=[f"sig{sk}", upr], writes=["macc"])
                else:
                    p.op("dve", lambda e, up=up, sk=sk: e.tensor_tensor(out=mtmp[:], in0=sig[sk][:], in1=up[:], op=ALU.mult),
                         reads=[f"sig{sk}", upr], writes=["mtmp"])
                    if bi == 1:
                        p.op("pool", lambda e: e.tensor_tensor(out=macc[:], in0=macc[:], in1=mtmp[:], op=ALU.add),
                             reads=["macc", "mtmp"], writes=["macc"])
                    else:
                        p.op("pool", lambda e, f=f: e.tensor_tensor(out=mb[:, f, :], in0=macc[:], in1=mtmp[:], op=ALU.add),
                             reads=["macc", "mtmp"], writes=["mb"])
        for f in range(8):
            fs = slice(f * 128, (f + 1) * 128)
            op_, opr = nps()
            for kc in range(8):
                p.op("pe", lambda e, op_=op_, kc=kc, fs=fs: e.matmul(op_[:], lhsT=wo[:, kc, fs], rhs=mb[:, kc, :],
                                                                     start=(kc == 0), stop=(kc == 7)),
                     reads=["wo", "mb"], writes=[opr])
            p.op("dve", lambda e, op_=op_, f=f: e.scalar_tensor_tensor(out=r[:, f, :], in0=h32[:, f, :], scalar=float(ALPHA),
                                                                       in1=op_[:], op0=ALU.mult, op1=ALU.add),
                 reads=[opr, "h32"], writes=["r"])
        emit_ln_fm(p, r, "r", cols[:, 0:8], cols[:, 8:16], h1, "r", ones32, nps(), nps(), sq, mean, rstd, tmp, "ln1", sqres="y32")
        for sub in range(4):
            ss = slice(sub * 128, (sub + 1) * 128)
            for kc in range(8):
                p.op("pe", lambda e, kc=kc, ss=ss: e.matmul(rps[:, 0:16], lhsT=h1[:, kc, ss], rhs=wr[:, kc, :], start=(kc == 0),
                                                            stop=(kc == 7)), reads=["r", "wr"], writes=["rps"])
            RS = "rt"
            p.op("dve", lambda e: e.tensor_tensor(out=lg[:], in0=rps[:, 0:16], in1=rb[:], op=ALU.add), reads=["rps", "rb"], writes=[RS])
            p.op("dve", lambda e: e.tensor_reduce(out=mx[:, 0:1], in_=lg[:], op=ALU.max, axis=AX.X), reads=[RS], writes=[RS])
            p.op("dve", lambda e: e.tensor_scalar(out=mx[:, 1:2], in0=mx[:, 0:1], scalar1=-1.0, scalar2=None, op0=ALU.mult),
                 reads=[RS], writes=[RS])
            lg4 = lg[:].rearrange("p (g k) -> p g k", g=4)
            p.op("act", lambda e: e.activation(out=exd[:, :, 0:4], in_=lg4, func=AF.Exp, bias=mx[:, 1:2]), reads=[RS], writes=[RS])
            p.op("act", lambda e: e.activation(out=exd[:, :, 4:8], in_=lg4, func=AF.Exp, bias=mx[:, 1:2]), reads=[RS], writes=[RS])
            n = 0
            for i in range(4):
                for j in range(i + 1, 4):
                    p.op("dve", lambda e, i=i, j=j, n=n: e.tensor_tensor(out=pr6[:, :, n:n + 1], in0=exd[:, :, i:i + 1],
                                                                         in1=exd[:, :, j:j + 1], op=ALU.add),
                         reads=[RS], writes=[RS])
                    n += 1
            p.op("dve", lambda e: e.tensor_reduce(out=gs[:, 0:4], in_=pr6[:], op=ALU.max, axis=AX.X), reads=[RS], writes=[RS])
            p.op("dve", lambda e: e.tensor_reduce(out=gs[:, 4:5], in_=gs[:, 0:4], op=ALU.max, axis=AX.X), reads=[RS], writes=[RS])
            p.op("dve", lambda e: e.tensor_scalar(out=gs[:, 0:4], in0=gs[:, 0:4], scalar1=gs[:, 4:5], scalar2=None, op0=ALU.is_ge),
                 reads=[RS], writes=[RS])
            for rr in range(1, 4):
                dst = cnt if rr == 1 else cmp_
                p.op("dve", lambda e, rr=rr, dst=dst: e.tensor_tensor(out=dst[:], in0=exd[:, :, rr:rr + 4], in1=exd[:, :, 0:4],
                                                                      op=ALU.is_gt), reads=[RS], writes=[RS])
                if rr > 1:
                    p.op("dve", lambda e: e.tensor_tensor(out=cnt[:], in0=cnt[:], in1=cmp_[:], op=ALU.add), reads=[RS], writes=[RS])
            p.op("dve", lambda e: e.tensor_scalar(out=cnt[:], in0=cnt[:], scalar1=2.0, scalar2=None, op0=ALU.is_lt),
                 reads=[RS], writes=[RS])
            gsel4 = gsel[:].rearrange("p (g k) -> p g k", g=4)
            for g_ in range(4):
                p.op("dve", lambda e, g_=g_: e.scalar_tensor_tensor(out=gsel4[:, g_, :], in0=cnt[:, g_, :], scalar=gs[:, g_:g_ + 1],
                                                                    in1=exd[:, g_, 0:4], op0=ALU.mult, op1=ALU.mult),
                     reads=[RS], writes=[RS])
            p.op("dve", lambda e: e.tensor_reduce(out=mx[:, 2:3], in_=gsel[:], op=ALU.add, axis=AX.X), reads=[RS], writes=[RS])
            p.op("dve", lambda e: e.reciprocal(out=mx[:, 3:4], in_=mx[:, 2:3]), reads=[RS], writes=[RS])
            gk = sub % 2
            p.op("dve", lambda e, gk=gk: e.tensor_scalar(out=gout[gk][:], in0=gsel[:], scalar1=mx[:, 3:4], scalar2=None,
                                                         op0=ALU.mult), reads=[RS], writes=[f"gout{gk}"])
            t0 = blk * 512 + sub * 128
            tile_i = blk * 4 + sub
            p.dma("act", gate_o[t0:t0 + 128, :], gout[gk][:], reads=[f"gout{gk}"], writes=["gate_d"])
            p.op("dve", lambda e, gk=gk: e.tensor_scalar(out=Mt[:], in0=gout[gk][:], scalar1=0.0, scalar2=None, op0=ALU.is_gt),
                 reads=[f"gout{gk}"], writes=["Mt"])
            p.op("pe", lambda e, ti=tile_i: e.matmul(rps[:, 16:32], lhsT=LT32[:], rhs=Mt[:], start=True, stop=(ti == 0)),
                 reads=["LT32", "Mt"], writes=["rps"])
            if tile_i > 0:
                p.op("pe", lambda e: e.matmul(rps[:, 16:32], lhsT=ones32[:], rhs=Racc[:], start=False, stop=True),
                     reads=["ones32", "Racc"], writes=["rps"])
            p.op("act", lambda e, gk=gk: e.activation(out=rk_sb[gk][:], in_=rps[:, 16:32], func=AF.Copy), reads=["rps"],
                 writes=[f"rk{gk}"])
            p.dma("act", rk_o[t0:t0 + 128, :], rk_sb[gk][:], reads=[f"rk{gk}"], writes=["rk_d"])
            if tile_i == 0:
                p.op("dve", lambda e: e.tensor_copy(out=Racc[:], in_=Mt[:]), reads=["Mt"], writes=["Racc"])
            else:
                p.op("dve", lambda e: e.tensor_tensor(out=Racc[:], in0=Racc[:], in1=Mt[:], op=ALU.add), reads=["Mt", "Racc"],
                     writes=["Racc"])
            for half in range(2):
                tp_, tpr = nps()
                for kq in range(4):
                    kc = half * 4 + kq
                    p.op("pe", lambda e, tp_=tp_, kq=kq, kc=kc, ss=ss: e.transpose(tp_[:, kq * 128:(kq + 1) * 128], h1[:, kc, ss],
                                                                                  id32[:]), reads=["r", "id32"], writes=[tpr])
                p.op("act" if half == 0 else "dve",
                     (lambda e, tp_=tp_, gk=gk, half=half: e.activation(out=h1tok[gk][:, half * 512:(half + 1) * 512], in_=tp_[:],
                                                                        func=AF.Copy)) if half == 0 else
                     (lambda e, tp_=tp_, gk=gk, half=half: e.tensor_copy(out=h1tok[gk][:, half * 512:(half + 1) * 512], in_=tp_[:])),
                     reads=[tpr], writes=[f"h1tok{gk}"])
            p.dma("sp", h1tok_o[t0:t0 + 128, :], h1tok[gk][:], reads=[f"h1tok{gk}"], writes=["h1tok_d"])
    p.dma("sp", racc_o, Racc[:], reads=["Racc"], writes=["racc_d"])
    p.end_phase()


def emit_moe(p, io, T, SBT=1024, n_exp=16):
    SBT = min(SBT, T)
    NS = T // SBT
    NH = SBT // 512
    v3 = lambda ap: ap.rearrange("(kc p) n -> p kc n", p=128)
    h1T_d = v3(io["h1T"])
    gT_d = io["gT"]
    w_d = [io["w1"], io["w3"], io["w2"]]
    cols_d = io["cols2"]
    h2T_o = v3(io["h2T"])
    h2res = io["h2res"]

    cols = p.sb([128, 16], F32, "cols")
    ones32 = p.sb([128, 128], F32, "ones32")
    p.dma("sp", cols[:], cols_d, writes=["lncols"])
    p.op("pool", lambda e: e.memset(ones32[:], 1.0), writes=["ones32"])
    stg = Stager(p, n=2, width=256)
    xb = p.sb([128, 8, SBT], BF16, "xb")
    acc = p.sb([128, 8, SBT], F32, "acc")
    wbuf = [p.sb([128, 8, D_MODEL], BF16, "wbuf") for _ in range(4)]
    G = [p.sb([128, SBT], F32, "G") for _ in range(2)]
    hbg = [p.sb([128, 8, 512], BF16, "hbg") for _ in range(NH)]
    sa = [p.sb([128, 512], F32, "sa") for _ in range(2)]
    tt = [p.sb([128, 512], F32, "tt") for _ in range(2)]
    hres = p.sb([128, 8, 512], F32, "hres")
    sq = p.sb([128, 8, 512], F32, "sq")
    mean = p.sb([128, 512], F32, "mean")
    rstd = p.sb([128, 512], F32, "rstd")
    tmp = p.sb([128, 512], F32, "tmp")
    PS = [p.ps([128, 512], F32, "PS") for _ in range(8)]
    psi = [0]

    def nps():
        k = psi[0] % 8
        psi[0] += 1
        return PS[k], f"PS{k}"

    wi = [0]

    def load_w(m, e):
        k = wi[0] % 4
        wi[0] += 1
        src = w_d[m][e].rearrange("(kc p) n -> p kc n", p=128)
        stg.load(lambda c0, w, k=k: wbuf[k][:, :, c0:c0 + w], src, 8, D_MODEL, f"wbuf{k}")
        return wbuf[k], f"wbuf{k}"

    it = 0
    for S in range(NS):
        t0 = S * SBT
        for hf_ in range(NH):
            cs = slice(hf_ * 512, (hf_ + 1) * 512)
            p.dma("sp", hres[:], h1T_d[:, :, t0 + hf_ * 512: t0 + (hf_ + 1) * 512], reads=["h1T"], writes=["hres"])
            p.op("dve", lambda e, cs=cs: e.tensor_copy(out=xb[:, :, cs], in_=hres[:]), reads=["hres"], writes=[("xb", hf_)])
        for ex in range(n_exp):
            gk = ex % 2
            p.dma("pool", G[gk][:], gT_d[ex, t0:t0 + SBT].partition_broadcast(128), reads=["gT"], writes=[f"G{gk}"])
            W1, W1r = load_w(0, ex)
            W3, W3r = load_w(1, ex)
            W2, W2r = load_w(2, ex)
            for f in range(8):
                fs = slice(f * 128, (f + 1) * 128)
                for hf_ in range(NH):
                    cs = slice(hf_ * 512, (hf_ + 1) * 512)
                    k = it % 2
                    it += 1
                    A, Ar = nps()
                    B, Br = nps()
                    for kc in range(8):
                        p.op("pe", lambda e, A=A, W1=W1, kc=kc, fs=fs, cs=cs: e.matmul(A[:], lhsT=W1[:, kc, fs], rhs=xb[:, kc, cs],
                                                                                       start=(kc == 0), stop=(kc == 7)),
                             reads=[W1r, ("xb", hf_)], writes=[Ar])
                    for kc in range(8):
                        p.op("pe", lambda e, B=B, W3=W3, kc=kc, fs=fs, cs=cs: e.matmul(B[:], lhsT=W3[:, kc, fs], rhs=xb[:, kc, cs],
                                                                                       start=(kc == 0), stop=(kc == 7)),
                             reads=[W3r, ("xb", hf_)], writes=[Br])
                    p.op("act", lambda e, A=A, k=k: e.activation(out=sa[k][:], in_=A[:], func=AF.Silu), reads=[Ar], writes=[f"sa{k}"])
                    p.op("dve", lambda e, B=B, k=k: e.tensor_tensor(out=tt[k][:], in0=sa[k][:], in1=B[:], op=ALU.mult),
                         reads=[f"sa{k}", Br], writes=[f"tt{k}"])
                    p.op("pool", lambda e, k=k, f=f, hf_=hf_, gk=gk, cs=cs: e.tensor_tensor(
                        out=hbg[hf_][:, f, :], in0=tt[k][:], in1=G[gk][:, cs], op=ALU.mult),
                        reads=[f"tt{k}", f"G{gk}"], writes=[("hbg", hf_)])
            for o in range(8):
                os_ = slice(o * 128, (o + 1) * 128)
                for hf_ in range(NH):
                    cs = slice(hf_ * 512, (hf_ + 1) * 512)
                    O, Or = nps()
                    for f in range(8):
                        p.op("pe", lambda e, O=O, W2=W2, f=f, os_=os_, hf_=hf_: e.matmul(O[:], lhsT=W2[:, f, os_], rhs=hbg[hf_][:, f, :],
                                                                                         start=(f == 0), stop=(f == 7)),
                             reads=[W2r, ("hbg", hf_)], writes=[Or])
                    if ex == 0:
                        p.op("dve", lambda e, O=O, o=o, cs=cs: e.tensor_copy(out=acc[:, o, cs], in_=O[:]),
                             reads=[Or], writes=[("acc", hf_)])
                    else:
                        p.op("dve", lambda e, O=O, o=o, cs=cs: e.tensor_tensor(out=acc[:, o, cs], in0=acc[:, o, cs], in1=O[:], op=ALU.add),
                             reads=[Or, ("acc", hf_)], writes=[("acc", hf_)])
        for hf_ in range(NH):
            cs = slice(hf_ * 512, (hf_ + 1) * 512)
            tsl = slice(t0 + hf_ * 512, t0 + (hf_ + 1) * 512)
            p.dma("sp", hres[:], h1T_d[:, :, tsl], reads=["h1T"], writes=["hres"])
            rv = acc[:, :, cs]
            rres = ("acc", hf_)
            p.op("dve", lambda e, rv=rv: e.scalar_tensor_tensor(out=rv, in0=hres[:], scalar=float(ALPHA), in1=rv, op0=ALU.mult,
                                                                op1=ALU.add), reads=["hres", rres], writes=[rres])
            emit_ln_fm(p, rv, rres, cols[:, 0:8], cols[:, 8:16], hres, "hres", ones32, nps(), nps(), sq, mean, rstd, tmp, "ln2")
            p.dma("sp", h2T_o[:, :, tsl], hres[:], reads=["hres"], writes=[h2res])
    p.end_phase()


MOE_B = 512


def emit_moe_sparse(p, io, T, last):
    import concourse.bass as _b
    NT = T // 128
    NBLK = (2 * T + 16 * (MOE_B - 1)) // MOE_B
    NR = NBLK * MOE_B
    h1tok_d, gate_d, rk_d, racc_d, xs, ys = io["h1tok"], io["gate"], io["rk"], io["racc"], io["xs"], io["ys"]
    wr_d = [io[k].rearrange("(r q) n -> r (q n)", q=1) for k in ("w1r", "w3r", "w2r")]

    def ind(eng_fn_kwargs):
        return eng_fn_kwargs

    def idma(out, in_, idx_ap, scatter, reads, writes):
        def fn(e):
            off = _b.IndirectOffsetOnAxis(ap=idx_ap, axis=0)
            if scatter:
                return e.indirect_dma_start(out=out, out_offset=off, in_=in_, in_offset=None)
            return e.indirect_dma_start(out=out, out_offset=None, in_=in_, in_offset=off)
        p.ops.append(dict(eng="pool", fn=fn, reads=tuple(reads), writes=tuple(writes), kind="dma"))

    ones32 = p.sb([128, 128], F32, "ones32")
    id32 = p.sb([128, 128], F32, "id32")
    idb = p.sb([128, 128], BF16, "idb")
    ramp = p.sb([128, 16], F32, "ramp")
    iota = p.sb([128, 1], F32, "iota")
    racc = p.sb([128, 16], F32, "racc")
    p.op("pool", lambda e: e.memset(ones32[:], 1.0), writes=["ones32"])
    p.dma("sp", id32[:], io["ident"], writes=["id32"])
    p.dma("sp", ramp[:], io["ramp"], writes=["ramp"])
    p.dma("sp", iota[:], io["iota"], writes=["iota"])
    p.dma("sp", racc[:], racc_d, reads=["racc_d"], writes=["racc"])
    p.op("dve", lambda e: e.tensor_copy(out=idb[:], in_=id32[:]), reads=["id32"], writes=["idb"])
    PS = [p.ps([128, 512], F32, "PS") for _ in range(4)]
    TR = [p.ps([128, 4, 512], BF16, "TR") for _ in range(2)]
    psi = [0]

    def nps():
        k = psi[0] % 4
        psi[0] += 1
        return PS[k], f"PS{k}"

    xblk = [p.sb([128, 4, D_MODEL], BF16, "xblk") for _ in range(2)]

    cnt = p.sb([128, 16], F32, "cnt")
    cmp16 = p.sb([128, 16], F32, "cmp16")
    padded = p.sb([128, 16], F32, "padded")
    pstart = p.sb([128, 16], F32, "pstart")
    pend = p.sb([128, 16], F32, "pend")
    PR = "prol"
    cps_, cpr = nps()
    p.op("pe", lambda e: e.matmul(cps_[:, 0:16], lhsT=ones32[:], rhs=racc[:], start=True, stop=True), reads=["ones32", "racc"],
         writes=[cpr])
    p.op("act", lambda e: e.activation(out=cnt[:], in_=cps_[:, 0:16], func=AF.Copy), reads=[cpr], writes=[PR])
    for ex in range(16):
        p.op("dve", lambda e, ex=ex: e.tensor_scalar(out=cmp16[:], in0=ramp[:], scalar1=cnt[:, ex:ex + 1], scalar2=None, op0=ALU.is_lt),
             reads=[PR, "ramp"], writes=[PR])
        p.op("dve", lambda e, ex=ex: e.tensor_reduce(out=padded[:, ex:ex + 1], in_=cmp16[:], op=ALU.add, axis=AX.X), reads=[PR],
             writes=[PR])
    p.op("dve", lambda e: e.tensor_scalar(out=padded[:], in0=padded[:], scalar1=float(MOE_B), scalar2=None, op0=ALU.mult),
         reads=[PR], writes=[PR])
    p.op("dve", lambda e: e.memset(pstart[:], 0.0), reads=[PR], writes=[PR])
    for ex in range(1, 16):
        p.op("dve", lambda e, ex=ex: e.tensor_tensor(out=pstart[:, ex:ex + 1], in0=pstart[:, ex - 1:ex], in1=padded[:, ex - 1:ex],
                                                     op=ALU.add), reads=[PR], writes=[PR])
    p.op("dve", lambda e: e.tensor_tensor(out=pend[:], in0=pstart[:], in1=padded[:], op=ALU.add), reads=[PR], writes=[PR])
    esf = p.sb([128, NBLK], F32, "esf")
    widf = p.sb([128, NBLK, 4], F32, "widf")
    widx = p.sb([128, NBLK, 4], I32, "widx")
    for s_ in range(NBLK):
        p.op("dve", lambda e, s_=s_: e.tensor_scalar(out=cmp16[:], in0=pend[:], scalar1=float(MOE_B * s_) + 0.5, scalar2=None,
                                                     op0=ALU.is_lt), reads=[PR], writes=[PR])
        p.op("dve", lambda e, s_=s_: e.tensor_reduce(out=esf[:, s_:s_ + 1], in_=cmp16[:], op=ALU.add, axis=AX.X), reads=[PR],
             writes=[PR])
    p.op("dve", lambda e: e.tensor_scalar(out=esf[:], in0=esf[:], scalar1=15.0, scalar2=128.0, op0=ALU.min, op1=ALU.mult),
         reads=[PR], writes=[PR])
    p.op("dve", lambda e: e.tensor_scalar(out=esf[:], in0=esf[:], scalar1=iota[:, 0:1], scalar2=4.0, op0=ALU.add, op1=ALU.mult),
         reads=[PR, "iota"], writes=[PR])
    for q in range(4):
        p.op("dve", lambda e, q=q: e.tensor_scalar(out=widf[:, :, q], in0=esf[:], scalar1=float(q), scalar2=None, op0=ALU.add),
             reads=[PR], writes=[PR])
    p.op("dve", lambda e: e.tensor_copy(out=widx[:], in_=widf[:]), reads=[PR], writes=["widx"])

    idxf = p.sb([128, NT, 2], F32, "idxf")
    idxi = p.sb([128, NT, 2], I32, "idxi")
    gAB = p.sb([128, NT, 2], F32, "gAB")
    gt_ = [p.sb([128, 16], F32, "gt") for _ in range(2)]
    rkt = [p.sb([128, 16], F32, "rkt") for _ in range(2)]
    d1 = p.sb([128, 16], F32, "d1")
    sm = p.sb([128, 8], F32, "sm")
    h32 = [p.sb([128, D_MODEL], F32, "h32") for _ in range(2)]
    xb = [p.sb([128, D_MODEL], BF16, "xb") for _ in range(2)]
    SC = "scat"
    for tt in range(NT):
        k = tt % 2
        rows = slice(tt * 128, (tt + 1) * 128)
        p.dma("sp", h32[k][:], h1tok_d[rows, :], reads=["h1tok_d"], writes=[f"h32{k}"])
        p.dma("act", gt_[k][:], gate_d[rows, :], reads=["gate_d"], writes=[f"gt{k}"])
        p.dma("act", rkt[k][:], rk_d[rows, :], reads=["rk_d"], writes=[f"rkt{k}"])
        p.op("act", lambda e, k=k: e.activation(out=xb[k][:], in_=h32[k][:], func=AF.Copy), reads=[f"h32{k}"], writes=[f"xb{k}"])
        p.op("dve", lambda e, k=k: e.tensor_tensor(out=d1[:], in0=rkt[k][:], in1=pstart[:], op=ALU.add), reads=[f"rkt{k}", PR],
             writes=[SC])
        p.op("dve", lambda e, k=k: e.tensor_scalar(out=cmp16[:], in0=gt_[k][:], scalar1=0.0, scalar2=None, op0=ALU.is_gt),
             reads=[f"gt{k}", PR], writes=[PR])
        p.op("dve", lambda e: e.scalar_tensor_tensor(out=d1[:], in0=d1[:], scalar=1.0, in1=cmp16[:], op0=ALU.add, op1=ALU.mult),
             reads=[SC, PR], writes=[SC])
        p.op("dve", lambda e: e.tensor_reduce(out=sm[:, 0:1], in_=d1[:], op=ALU.max, axis=AX.X), reads=[SC], writes=[SC])
        p.op("dve", lambda e: e.tensor_reduce(out=sm[:, 1:2], in_=d1[:], op=ALU.add, axis=AX.X), reads=[SC], writes=[SC])
        p.op("dve", lambda e, tt=tt: e.tensor_scalar(out=idxf[:, tt, 0:1], in0=sm[:, 0:1], scalar1=-1.0, scalar2=None, op0=ALU.add),
             reads=[SC], writes=["idxf"])
        p.op("dve", lambda e, tt=tt: e.scalar_tensor_tensor(out=idxf[:, tt, 1:2], in0=sm[:, 1:2], scalar=-1.0, in1=sm[:, 0:1],
                                                            op0=ALU.add, op1=ALU.subtract), reads=[SC], writes=["idxf"])
        p.op("dve", lambda e, tt=tt: e.tensor_scalar(out=idxf[:, tt, :], in0=idxf[:, tt, :], scalar1=0.0, scalar2=float(NR - 1),
                                                     op0=ALU.max, op1=ALU.min), reads=["idxf"], writes=["idxf"])
        p.op("dve", lambda e, tt=tt: e.tensor_copy(out=idxi[:, tt, :], in_=idxf[:, tt, :]), reads=["idxf"], writes=[("idxi", tt)])
        p.op("dve", lambda e: e.tensor_scalar(out=cmp16[:], in0=d1[:], scalar1=sm[:, 0:1], scalar2=None, op0=ALU.is_equal),
             reads=[SC, PR], writes=[PR])
        p.op("dve", lambda e, k=k: e.tensor_tensor(out=cmp16[:], in0=cmp16[:], in1=gt_[k][:], op=ALU.mult), reads=[PR, f"gt{k}"],
             writes=[PR])
        p.op("dve", lambda e, tt=tt: e.tensor_reduce(out=gAB[:, tt, 0:1], in_=cmp16[:], op=ALU.add, axis=AX.X), reads=[PR],
             writes=["gAB"])
        p.op("dve", lambda e, k=k: e.tensor_reduce(out=sm[:, 2:3], in_=gt_[k][:], op=ALU.add, axis=AX.X), reads=[f"gt{k}", SC],
             writes=[SC])
        p.op("dve", lambda e, tt=tt: e.tensor_tensor(out=gAB[:, tt, 1:2], in0=sm[:, 2:3], in1=gAB[:, tt, 0:1], op=ALU.subtract),
             reads=[SC, "gAB"], writes=["gAB"])
        for kk in range(2):
            idma(xs, xb[k][:, :], idxi[:, tt, kk:kk + 1], True, [f"xb{k}", ("idxi", tt), "xs"], ["xs"])

    Wm = [[p.sb([128, 8, D_MODEL], BF16, "Wm") for _ in range(3)] for _ in range(2)]
    xT = [p.sb([128, 8, 512], BF16, "xT") for _ in range(2)]
    hbT = p.sb([128, 8, 512], BF16, "hbT")
    sa = [p.sb([128, 512], F32, "sa") for _ in range(2)]
    ysb = [p.sb([128, D_MODEL], F32, "ysb") for _ in range(2)]
    it = 0
    io_i = 0
    for s_ in range(NBLK):
        wb_ = s_ % 2
        for m in range(3):
            for q in range(4):
                idma(Wm[wb_][m][:].rearrange("p (q a) n -> p q (a n)", q=4)[:, q, :], wr_d[m], widx[:, s_, q:q + 1], False,
                     ["widx"], [f"W{wb_}{m}"])
        p.dma("sp", xblk[wb_][:], xs[s_ * MOE_B:(s_ + 1) * MOE_B, :].rearrange("(j p) n -> p j n", p=128), reads=["xs"],
              writes=[f"xblk{wb_}"])
        for half in range(2):
            for kq in range(4):
                kc = half * 4 + kq
                for j in range(4):
                    p.op("pe", lambda e, half=half, kq=kq, kc=kc, j=j, wb_=wb_: e.transpose(
                        TR[half][:, kq, j * 128:(j + 1) * 128], xblk[wb_][:, j, kc * 128:(kc + 1) * 128], idb[:]),
                        reads=[f"xblk{wb_}", "idb"], writes=[f"TR{half}"])
            p.op("act" if half == 0 else "dve",
                 (lambda e, half=half, wb_=wb_: e.activation(out=xT[wb_][:, half * 4:(half + 1) * 4, :], in_=TR[half][:], func=AF.Copy))
                 if half == 0 else
                 (lambda e, half=half, wb_=wb_: e.tensor_copy(out=xT[wb_][:, half * 4:(half + 1) * 4, :], in_=TR[half][:])),
                 reads=[f"TR{half}"], writes=[f"xT{wb_}"])
        W1, W3, W2 = Wm[wb_]
        for f in range(8):
            fs = slice(f * 128, (f + 1) * 128)
            k = it % 2
            it += 1
            A, Ar = nps()
            B, Br = nps()
            for kc in range(8):
                p.op("pe", lambda e, A=A, W1=W1, kc=kc, fs=fs, wb_=wb_: e.matmul(A[:], lhsT=W1[:, kc, fs], rhs=xT[wb_][:, kc, :],
                                                                               start=(kc == 0), stop=(kc == 7)),
                     reads=[f"W{wb_}0", f"xT{wb_}"], writes=[Ar])
            for kc in range(8):
                p.op("pe", lambda e, B=B, W3=W3, kc=kc, fs=fs, wb_=wb_: e.matmul(B[:], lhsT=W3[:, kc, fs], rhs=xT[wb_][:, kc, :],
                                                                               start=(kc == 0), stop=(kc == 7)),
                     reads=[f"W{wb_}1", f"xT{wb_}"], writes=[Br])
            p.op("act", lambda e, A=A, k=k: e.activation(out=sa[k][:], in_=A[:], func=AF.Silu), reads=[Ar], writes=[f"sa{k}"])
            p.op("dve", lambda e, B=B, k=k, f=f: e.tensor_tensor(out=hbT[:, f, :], in0=sa[k][:], in1=B[:], op=ALU.mult),
                 reads=[f"sa{k}", Br], writes=["hbT"])
        for j in range(4):
            yk = io_i % 2
            io_i += 1
            for c in range(2):
                O, Or = nps()
                for f in range(8):
                    p.op("pe", lambda e, O=O, W2=W2, f=f, j=j, c=c: e.matmul(O[:], lhsT=hbT[:, f, j * 128:(j + 1) * 128],
                                                                             rhs=W2[:, f, c * 512:(c + 1) * 512], start=(f == 0),
                                                                             stop=(f == 7)),
                         reads=[f"W{wb_}2", "hbT"], writes=[Or])
                if c == 0:
                    p.op("act", lambda e, O=O, yk=yk: e.activation(out=ysb[yk][:, 0:512], in_=O[:], func=AF.Copy), reads=[Or],
                         writes=[f"ysb{yk}"])
                else:
                    p.op("dve", lambda e, O=O, yk=yk: e.tensor_copy(out=ysb[yk][:, 512:1024], in_=O[:]), reads=[Or],
                         writes=[f"ysb{yk}"])
            r0 = s_ * MOE_B + j * 128
            p.dma("act", ys[r0:r0 + 128, :], ysb[yk][:], reads=[f"ysb{yk}"], writes=["ys"])

    rA = [p.sb([128, D_MODEL], F32, "rA") for _ in range(2)]
    rB = [p.sb([128, D_MODEL], F32, "rB") for _ in range(2)]
    junk = ysb[0]
    stt = p.sb([128, 8], F32, "stt")
    ho = [p.sb([128, D_MODEL], F32, "ho") for _ in range(2)]
    hTt = [p.sb([128, 8, 128], F32, "hTt") for _ in range(2)]
    hT_o = (io["out_fm"] if last else io["hT_half"]).rearrange("(kc p) t -> p kc t", p=128)
    hres_name = "out" if last else "hT_half"
    hTb_o = io["hT_hbf"].rearrange("(kc p) t -> p kc t", p=128)
    cols2 = p.sb([128, 16], F32, "cols2")
    p.dma("sp", cols2[:], io["cols2"], writes=["cols2"])
    for tt in range(NT):
        k = tt % 2
        rows = slice(tt * 128, (tt + 1) * 128)
        idma(rA[k][:, :], ys, idxi[:, tt, 0:1], False, [("idxi", tt), "ys"], [f"rA{k}"])
        idma(rB[k][:, :], ys, idxi[:, tt, 1:2], False, [("idxi", tt), "ys"], [f"rB{k}"])
        p.dma("sp", h32[k][:], h1tok_d[rows, :], reads=["h1tok_d"], writes=[f"h32{k}"])
        p.op("dve", lambda e, k=k, tt=tt: e.tensor_scalar(out=rA[k][:], in0=rA[k][:], scalar1=gAB[:, tt, 0:1], scalar2=None,
                                                          op0=ALU.mult), reads=[f"rA{k}", "gAB"], writes=[f"rA{k}"])
        p.op("dve", lambda e, k=k, tt=tt: e.scalar_tensor_tensor(out=rA[k][:], in0=rB[k][:], scalar=gAB[:, tt, 1:2], in1=rA[k][:],
                                                                 op0=ALU.mult, op1=ALU.add), reads=[f"rA{k}", f"rB{k}", "gAB"],
             writes=[f"rA{k}"])
        p.op("dve", lambda e, k=k: e.scalar_tensor_tensor(out=rA[k][:], in0=h32[k][:], scalar=float(ALPHA), in1=rA[k][:],
                                                          op0=ALU.mult, op1=ALU.add), reads=[f"rA{k}", f"h32{k}"], writes=[f"rA{k}"])
        LS = "ln2s"
        p.op("act", lambda e, k=k: e.activation(out=junk[:], in_=rA[k][:], func=AF.Identity, accum_out=stt[:, 0:1]),
             reads=[f"rA{k}"], writes=["ysb0", LS])
        p.op("act", lambda e, k=k: e.activation(out=junk[:], in_=rA[k][:], func=AF.Square, accum_out=stt[:, 1:2]),
             reads=[f"rA{k}"], writes=["ysb0", LS])
        p.op("dve", lambda e: e.tensor_scalar(out=stt[:, 2:3], in0=stt[:, 0:1], scalar1=1.0 / D_MODEL, scalar2=None, op0=ALU.mult),
             reads=[LS], writes=[LS])
        p.op("dve", lambda e: e.tensor_tensor(out=stt[:, 3:4], in0=stt[:, 2:3], in1=stt[:, 2:3], op=ALU.mult), reads=[LS], writes=[LS])
        p.op("dve", lambda e: e.scalar_tensor_tensor(out=stt[:, 4:5], in0=stt[:, 1:2], scalar=1.0 / D_MODEL, in1=stt[:, 3:4],
                                                     op0=ALU.mult, op1=ALU.subtract), reads=[LS], writes=[LS])
        p.op("act", lambda e: e.activation(out=stt[:, 6:7], in_=stt[:, 4:5], func=AF.Sqrt, bias=LN_EPS), reads=[LS], writes=[LS])
        p.op("dve", lambda e: e.reciprocal(out=stt[:, 5:6], in_=stt[:, 6:7]), reads=[LS], writes=[LS])
        p.op("dve", lambda e, k=k: e.tensor_scalar(out=ho[k][:], in0=rA[k][:], scalar1=stt[:, 2:3], scalar2=stt[:, 5:6],
                                                   op0=ALU.subtract, op1=ALU.mult), reads=[f"rA{k}", LS], writes=[f"ho{k}"])
        for half in range(2):
            tp_, tpr = nps()
            for kq in range(4):
                kc = half * 4 + kq
                p.op("pe", lambda e, tp_=tp_, kq=kq, kc=kc, k=k: e.transpose(tp_[:, kq * 128:(kq + 1) * 128],
                                                                             ho[k][:, kc * 128:(kc + 1) * 128], id32[:]),
                     reads=[f"ho{k}", "id32"], writes=[tpr])
            for kq in range(4):
                kc = half * 4 + kq
                p.op("act", lambda e, tp_=tp_, k=k, kq=kq, kc=kc: e.activation(
                    out=hTt[k][:, kc, :], in_=tp_[:, kq * 128:(kq + 1) * 128], func=AF.Identity, scale=cols2[:, kc:kc + 1],
                    bias=cols2[:, 8 + kc:9 + kc]), reads=[tpr, "cols2"], writes=[f"hTt{k}"])
        p.dma("sp", hT_o[:, :, rows], hTt[k][:], reads=[f"hTt{k}"], writes=[hres_name])
        if not last:
            p.dma("pool", hTb_o[:, :, rows], hTt[k][:], reads=[f"hTt{k}"], writes=["hT_hbf"])
    p.end_phase()


def emit_ln0(p, io, T):
    v3 = lambda ap: ap.rearrange("(kc p) n -> p kc n", p=128)
    x_d = v3(io["xT"])
    h_o = v3(io["hT_half"])
    hb_o = v3(io["hT_hbf"])
    cols = p.sb([128, 16], F32, "cols")
    ones32 = p.sb([128, 128], F32, "ones32")
    p.dma("sp", cols[:], io["cols0"], writes=["lncols"])
    p.op("pool", lambda e: e.memset(ones32[:], 1.0), writes=["ones32"])
    r = [p.sb([128, 8, 512], F32, "r") for _ in range(2)]
    sq = p.sb([128, 8, 512], F32, "sq")
    mean = p.sb([128, 512], F32, "mean")
    rstd = p.sb([128, 512], F32, "rstd")
    tmp = p.sb([128, 512], F32, "tmp")
    PS = [p.ps([128, 512], F32, "PS") for _ in range(4)]
    for blk in range(T // 512):
        k = blk % 2
        ts_ = slice(blk * 512, (blk + 1) * 512)
        p.dma("sp", r[k][:], x_d[:, :, ts_], writes=[f"r{k}"])
        emit_ln_fm(p, r[k], f"r{k}", cols[:, 0:8], cols[:, 8:16], r[k], f"r{k}", ones32, (PS[2 * k], f"PS{2 * k}"),
                   (PS[2 * k + 1], f"PS{2 * k + 1}"), sq, mean, rstd, tmp, "ln0")
        p.dma("act", h_o[:, :, ts_], r[k][:], reads=[f"r{k}"], writes=["hT_half"])
        p.dma("pool", hb_o[:, :, ts_], r[k][:], reads=[f"r{k}"], writes=["hT_hbf"])
    p.end_phase()


PAIRS = [[0, 1], [2, 3], [4, 5], [6, 7]]
LAYER_IN = {"wq": [D_MODEL, 128], "wk": [D_MODEL, 128], "wv": [D_MODEL, 128], "wqk": [D_MODEL, 256], "wvg": [D_MODEL, 512],
            "gn": [128, 256], "wu": [D_MODEL, 128], "lam_rep": [128, 3, 512], "lam_col": [128, 3, 4], "bp": [128, 2, 4, 128],
            "cp": [128, 2, 4, 128], "dcol": [128, 1], "wg": [D_MODEL, 3072], "wglu": [256, 256], "wua": [256, D_MODEL],
            "wub": [256, D_MODEL], "wuc": [512, D_MODEL], "wo": [D_MODEL, D_MODEL], "cols1": [128, 18], "cols2": [128, 16],
            "w1r": [16 * 128 * 4, 2048], "w3r": [16 * 128 * 4, 2048], "w2r": [16 * 128 * 4, 2048],
            "ln2g_bc": [128, D_MODEL], "ln2b_bc": [128, D_MODEL]}


def build_fused(L, depth=DEPTH):
    Tc = L // 2
    NT = L // 128
    p = Prog()
    shared = {"xT": [D_MODEL, Tc], "cols0": [128, 16], "mk": [128, 2], "ident": [128, 128], "tri": [128, 128],
              "masks": [128, 4, 512], "cos": [128, NT, 4, 32], "sin": [128, NT, 4, 32], "dmask": [128, 2, 128], "dec": [128, 6],
              "rb": [128, 16], "wr": [D_MODEL, 16], "lt": [128, 128], "ramp": [128, 16], "iota": [128, 1]}
    g = {k: p.dram_in(k, shp).ap() for k, shp in shared.items()}
    lay = [{k: p.dram_in(f"{k}_{l}", shp).ap() for k, shp in LAYER_IN.items()} for l in range(depth)]
    out = p.dram_out("out", [D_MODEL, Tc]).ap()
    NR = (2 * Tc // MOE_B + 16) * MOE_B
    h1tok = p.dram_tmp("h1tok", [Tc, D_MODEL]).ap()
    gate_t = p.dram_tmp("gate_t", [Tc, 16]).ap()
    rk_t = p.dram_tmp("rk_t", [Tc, 16]).ap()
    racc_t = p.dram_tmp("racc_t", [128, 16]).ap()
    xs_t = p.dram_tmp("xs_t", [NR, D_MODEL], BF16).ap()
    ys_t = p.dram_tmp("ys_t", [NR, D_MODEL]).ap()
    hT_half = p.dram_tmp("hT_half", [D_MODEL, Tc]).ap()
    hT_full = p.dram_tmp("hT_full", [2 * D_MODEL, Tc], BF16).ap()
    hT_hbf = p.dram_tmp("hT_hbf", [D_MODEL, Tc], BF16).ap()
    yin = p.dram_tmp("yin", [2048, Tc], BF16).ap()
    y_own = p.dram_tmp("y_own", [D_MODEL, Tc], BF16).ap()
    yin4 = yin.rearrange("(a b r) t -> a b r t", a=2, b=2)
    nbh = Tc // 512

    hT_full_v = hT_full.rearrange("(kc r p) t -> r p kc t", kc=8, r=2, p=128)

    def hblk(blk):
        rk, lo = blk // nbh, blk % nbh
        return hT_full_v[rk][:, :, lo * 512:(lo + 1) * 512]

    def gather_h():
        for kc in range(8):
            p.coll("AllGather", hT_hbf[kc * 128:(kc + 1) * 128, :], hT_full[kc * 256:(kc + 1) * 256, :], PAIRS,
                   reads=["hT_hbf"], writes=["hT_full"])

    def yw_fn():
        mkt = p.sb([128, 2], F32, "mk")
        p.dma("sp", mkt[:], g["mk"], writes=["mk"])
        return YWriter(p, yin4, mkt, "mk", Tc)

    emit_ln0(p, {"xT": g["xT"], "cols0": g["cols0"], "hT_half": hT_half, "hT_hbf": hT_hbf}, Tc)
    gather_h()
    for l in range(depth):
        io = dict(g)
        io.update(lay[l])
        io.update({"hblk": hblk, "hT_half": hT_half, "hT_hbf": hT_hbf, "y_own": y_own, "h1tok": h1tok, "gate": gate_t, "rk": rk_t,
                   "racc": racc_t, "xs": xs_t, "ys": ys_t, "out_fm": out})
        emit_sb(p, io, L, yw_fn)
        emit_ret(p, io, L, yw_fn)
        emit_s5(p, io, L, yw_fn)
        p.coll("ReduceScatter", yin, y_own, PAIRS, reads=["yin"], writes=["y_own"])
        emit_post(p, io, Tc)
        last = l == depth - 1
        emit_moe_sparse(p, io, Tc, last)
        if not last:
            gather_h()
    return p.build()


def _c(a):
    return np.ascontiguousarray(a, dtype=np.float32)


def _wrows(w):
    return _c(w.reshape(16, 8, 128, 1024).transpose(0, 2, 1, 3).reshape(16 * 128 * 4, 2048))


def fused_in_maps(inp, L, depth=DEPTH):
    Tc = L // 2
    tri, masks = sb_consts()
    retc = [ret_consts(L, hf) for hf in range(2)]
    rb = _c(np.broadcast_to(inp["router_b"][None, :], (128, 16)))
    wr = _c(inp["router_w"])
    cols0 = np.zeros((128, 16), np.float32)
    cols0[:, 0:8] = inp["ln0_g"].reshape(8, 128).T
    cols0[:, 8:16] = inp["ln0_b"].reshape(8, 128).T
    per_layer_common = []
    for l in range(depth):
        cols1 = np.zeros((128, 18), np.float32)
        cols1[:, 0:8] = inp["ln1_g"][l].reshape(8, 128).T
        cols1[:, 8:16] = inp["ln1_b"][l].reshape(8, 128).T
        cols1[:, 16:18] = inp["s5_b_glu"][l].reshape(2, 128).T
        cols2 = np.zeros((128, 16), np.float32)
        cols2[:, 0:8] = inp["ln2_g"][l].reshape(8, 128).T
        cols2[:, 8:16] = inp["ln2_b"][l].reshape(8, 128).T
        per_layer_common.append({"wg": _c(inp["w_in"][l][:, 2560:]), "wglu": _c(inp["s5_w_glu"][l]), "wua": _c(inp["w_up_a"][l]),
                                 "wub": _c(inp["w_up_b"][l]), "wuc": _c(inp["w_up_c"][l]), "wo": _c(inp["w_out"][l]),
                                 "cols1": cols1, "cols2": cols2,
                                 "ln2g_bc": _c(np.broadcast_to(inp["ln2_g"][l][None, :], (128, D_MODEL))),
                                 "ln2b_bc": _c(np.broadcast_to(inp["ln2_b"][l][None, :], (128, D_MODEL))),
                                 "w1r": _wrows(inp["moe_w1"][l]), "w3r": _wrows(inp["moe_w3"][l]), "w2r": _wrows(inp["moe_w2"][l])})
    maps = []
    for c in range(N_CORES):
        bi, hf = c // 2, c % 2
        cos, sin, dmask, dec, ident = retc[hf]
        mk = np.zeros((128, 2), np.float32)
        mk[:, hf] = 1.0
        m = {"xT": _c(inp["x"][bi, hf * Tc:(hf + 1) * Tc, :].T), "cols0": cols0, "mk": mk, "ident": ident, "tri": tri,
             "masks": masks, "cos": cos, "sin": sin, "dmask": dmask, "dec": dec, "rb": rb, "wr": wr,
             "lt": _c(np.arange(128)[:, None] < np.arange(128)[None, :]), "ramp": _c(np.broadcast_to(MOE_B * np.arange(16.0)[None, :],
                                                                                                    (128, 16))),
             "iota": _c(np.arange(128.0).reshape(128, 1))}
        for l in range(depth):
            w_in = inp["w_in"][l]
            q0 = 256 + hf * 128
            qc0 = 1024 + hf * 128
            vc0 = 1536 + hf * 256
            rep, col, bp, cp, dcol = s5_host_layout(hf, inp["s5_lambda_re"][l], inp["s5_lambda_im"][l], inp["s5_log_dt"][l],
                                                    inp["s5_b_re"][l], inp["s5_b_im"][l], inp["s5_c_re"][l], inp["s5_c_im"][l],
                                                    inp["s5_d"][l])
            d = {"wq": _c(w_in[:, q0:q0 + 128]), "wk": _c(w_in[:, q0 + 256:q0 + 384]), "wv": _c(w_in[:, q0 + 512:q0 + 640]),
                 "wqk": _c(np.concatenate([w_in[:, qc0:qc0 + 128], w_in[:, qc0 + 256:qc0 + 384]], axis=1)),
                 "wvg": _c(np.concatenate([w_in[:, vc0:vc0 + 256], w_in[:, vc0 + 512:vc0 + 768]], axis=1)),
                 "gn": _c(np.broadcast_to(inp["ret_gn_g"][l][hf * 256:(hf + 1) * 256][None, :], (128, 256))),
                 "wu": _c(w_in[:, hf * 128:(hf + 1) * 128]), "lam_rep": rep, "lam_col": col, "bp": bp, "cp": cp, "dcol": dcol}
            d.update(per_layer_common[l])
            for k, v in d.items():
                m[f"{k}_{l}"] = v
        maps.append(m)
    return maps


def kernel(**inputs):
    inp = {k: np.asarray(v) for k, v in inputs.items()}
    L = inp["x"].shape[1]
    Tc = L // 2
    nc = build_fused(L)
    res = run_spmd(nc, fused_in_maps(inp, L))
    out = np.zeros((BATCH, L, D_MODEL), np.float32)
    for c in range(N_CORES):
        bi, hf = c // 2, c % 2
        out[bi, hf * Tc:(hf + 1) * Tc, :] = res[c]["out"].T
    return out
```
